# Optimizing a Trainium2 kernel written in Bass

```python
import math
import jax
import jax.numpy as jnp
from jax import lax
import numpy as np

D_MODEL = 2048
BATCH = 4
SEQ = 4096
DEPTH = 2

CTX_LEN = 256
GRID_W = 64
MIX_WIDTH = D_MODEL
N_GROUPS = 4
GROUP_WIDTH = MIX_WIDTH // N_GROUPS
HEAD_DIM = 128
GROUP_HEADS = GROUP_WIDTH // HEAD_DIM
GLA_HEADS = GROUP_HEADS
GLA_DK = HEAD_DIM // 2
GLA_GATE_RANK = 16
GLA_GATE_TAU = 16.0
GLA_CHUNK = 16
DN_HEADS = GROUP_HEADS
DN_CHUNK = 64
SHORT_CONV = 5
GQA_HEADS = GROUP_HEADS
GQA_KV_HEADS = 2
DIFF_HEADS = GROUP_HEADS
DIFF_DH = HEAD_DIM // 2
Q_BLOCK = 128
ROPE_THETA = 10000.0
N_EXPERTS = 16
EC_CAPACITY = 2
EXPERT_FF = D_MODEL // 2
RMS_EPS = 1e-6

IN_SPLITS = (
    ('gla_q', GLA_HEADS * GLA_DK),
    ('gla_k', GLA_HEADS * GLA_DK),
    ('gla_v', GLA_HEADS * HEAD_DIM),
    ('gla_g', GLA_HEADS * HEAD_DIM),
    ('gla_lr', 2 * GLA_GATE_RANK),
    ('dn_qkv', 3 * DN_HEADS * HEAD_DIM),
    ('dn_g', DN_HEADS * HEAD_DIM),
    ('dn_a', 2 * DN_HEADS),
    ('dn_b', 2 * DN_HEADS),
    ('gqa_q', GQA_HEADS * HEAD_DIM),
    ('gqa_kv', 2 * GQA_KV_HEADS * HEAD_DIM),
    ('diff_q', DIFF_HEADS * 2 * DIFF_DH),
    ('diff_k', DIFF_HEADS * 2 * DIFF_DH),
    ('diff_v', DIFF_HEADS * HEAD_DIM),
)

kernel_name = "hybrid_parallel_groups_ec_moe_dit"


def rms_norm(x, w, eps=RMS_EPS):
    xf = x.astype(jnp.float32)
    y = xf * lax.rsqrt(jnp.mean(xf * xf, axis=-1, keepdims=True) + eps)
    return y.astype(x.dtype) * w


def l2_norm(x, eps=RMS_EPS):
    xf = x.astype(jnp.float32)
    return xf * lax.rsqrt(jnp.sum(xf * xf, axis=-1, keepdims=True) + eps)


def split_columns(z):
    sizes = [s for _, s in IN_SPLITS]
    cuts = [int(v) for v in np.cumsum(sizes)[:-1]]
    return dict(zip([n for n, _ in IN_SPLITS], jnp.split(z, cuts, axis=-1)))


def axial_rope(x, pos_r, pos_c):
    d = x.shape[-1]
    half = d // 2
    inv = ROPE_THETA ** (-jnp.arange(0, half, 2, dtype=jnp.float32) / half)

    def rot(xa, pos):
        ang = pos.astype(jnp.float32)[:, None] * inv
        ang = ang.reshape((ang.shape[0],) + (1,) * (x.ndim - 3) + (ang.shape[1],))
        cos, sin = jnp.cos(ang).astype(x.dtype), jnp.sin(ang).astype(x.dtype)
        x1, x2 = jnp.split(xa, 2, axis=-1)
        return jnp.concatenate([x1 * cos - x2 * sin, x1 * sin + x2 * cos], axis=-1)

    return jnp.concatenate([rot(x[..., :half], pos_r), rot(x[..., half:], pos_c)], axis=-1)


def short_conv(x, w):
    k, ch = w.shape
    return lax.conv_general_dilated(x, w[:, None, :], window_strides=(1,),
                                    padding=[(k // 2, k // 2)],
                                    dimension_numbers=('NWC', 'WIO', 'NWC'),
                                    feature_group_count=ch)


def gla_chunked(q, k, v, log_a, s0):
    out_dtype = v.dtype
    q, k, v, log_a = (t.astype(jnp.float32) for t in (q, k, v, log_a))
    B, T, H, dk = q.shape
    dv = v.shape[-1]
    n = T // GLA_CHUNK
    q, k, log_a = (t.reshape(B, n, GLA_CHUNK, H, dk) for t in (q, k, log_a))
    v = v.reshape(B, n, GLA_CHUNK, H, dv)
    b = jnp.cumsum(log_a, axis=2)
    idx = jnp.arange(GLA_CHUNK)
    incl = (idx[:, None] >= idx[None, :])[:, :, None, None]
    decay = jnp.exp(jnp.where(incl, b[:, :, :, None] - b[:, :, None], -jnp.inf))
    scores = jnp.einsum('bnihd,bnijhd,bnjhd->bnhij', q, decay, k)
    o_intra = jnp.einsum('bnhij,bnjhe->bnihe', scores, v)
    b_last = b[:, :, -1]
    qg = q * jnp.exp(b)
    kd = k * jnp.exp(b_last[:, :, None] - b)
    dl = jnp.exp(b_last)

    def step(S, inp):
        qg_c, kd_c, v_c, dl_c = inp
        o = jnp.einsum('bihd,bhde->bihe', qg_c, S)
        S = S * dl_c[..., None] + jnp.einsum('bjhd,bjhe->bhde', kd_c, v_c)
        return S, o

    xs = tuple(jnp.moveaxis(t, 1, 0) for t in (qg, kd, v, dl))
    S, o_inter = lax.scan(step, s0, xs)
    o = o_intra + jnp.moveaxis(o_inter, 0, 1)
    return o.reshape(B, T, H, dv).astype(out_dtype), S


def gated_delta_chunked(q, k, v, g, beta, s0):
    out_dtype = v.dtype
    q, k, v, g, beta = (t.astype(jnp.float32) for t in (q, k, v, g, beta))
    B, T, H, dk = q.shape
    dv = v.shape[-1]
    C = DN_CHUNK
    n = T // C
    q, k = (t.reshape(B, n, C, H, dk) for t in (q, k))
    v = v.reshape(B, n, C, H, dv)
    g, beta = (t.reshape(B, n, C, H) for t in (g, beta))
    gc = jnp.cumsum(g, axis=2)
    gh = jnp.moveaxis(gc, 3, 2)
    idx = jnp.arange(C)
    incl = idx[:, None] >= idx[None, :]
    strict = idx[:, None] > idx[None, :]
    decay = jnp.exp(jnp.where(incl, gh[..., :, None] - gh[..., None, :], -jnp.inf))
    kb = k * beta[..., None]
    lower = jnp.where(strict, jnp.einsum('bnihd,bnjhd->bnhij', kb, k) * decay, 0.0)
    eye = jnp.broadcast_to(jnp.eye(C, dtype=jnp.float32), lower.shape)
    tmat = lax.linalg.triangular_solve(lower + eye, eye, left_side=True, lower=True)
    u = jnp.einsum('bnhij,bnjhe->bnihe', tmat, v * beta[..., None])
    w = jnp.einsum('bnhij,bnjhd->bnihd', tmat, kb * jnp.exp(gc)[..., None])
    attn = jnp.einsum('bnihd,bnjhd->bnhij', q, k) * decay
    qg = q * jnp.exp(gc)[..., None]
    g_last = gc[:, :, -1]
    kd = k * jnp.exp(g_last[:, :, None] - gc)[..., None]
    dl = jnp.exp(g_last)

    def step(S, inp):
        u_c, w_c, qg_c, a_c, kd_c, dl_c = inp
        v_new = u_c - jnp.einsum('bihd,bhde->bihe', w_c, S)
        o = jnp.einsum('bihd,bhde->bihe', qg_c, S) + jnp.einsum('bhij,bjhe->bihe', a_c, v_new)
        S = S * dl_c[:, :, None, None] + jnp.einsum('bjhd,bjhe->bhde', kd_c, v_new)
        return S, o

    xs = tuple(jnp.moveaxis(t, 1, 0) for t in (u, w, qg, attn, kd, dl))
    S, o = lax.scan(step, s0, xs)
    return jnp.moveaxis(o, 0, 1).reshape(B, T, H, dv).astype(out_dtype), S


def run_direction(scan_fn, ctx_in, lat_in, s0, reverse):
    flip = (lambda t: jnp.flip(t, axis=1)) if reverse else (lambda t: t)
    o_ctx, s_ctx = scan_fn(*[flip(t) for t in ctx_in], s0)
    o_lat, _ = scan_fn(*[flip(t) for t in lat_in], s_ctx)
    return flip(o_ctx), flip(o_lat)


def blocked_queries(attend, q, *kv):
    B, T = q.shape[:2]
    qb = q.reshape((B, T // Q_BLOCK, Q_BLOCK) + q.shape[2:]).swapaxes(0, 1)
    ob = lax.map(lambda qi: attend(qi, *kv), qb)
    return ob.swapaxes(0, 1).reshape((B, T) + ob.shape[3:])


def gqa_attend(q, k, v):
    B, Tq, H, d = q.shape
    G = k.shape[2]
    qg = q.reshape(B, Tq, G, H // G, d)
    s = jnp.einsum('bqgrd,bkgd->bgrqk', qg, k).astype(jnp.float32) * d ** -0.5
    p = jax.nn.softmax(s, axis=-1).astype(v.dtype)
    return jnp.einsum('bgrqk,bkgd->bqgrd', p, v).reshape(B, Tq, H, d)


def diff_attend(q, k, v, lam):
    s = jnp.einsum('bqhcd,bkhcd->bhcqk', q, k).astype(jnp.float32) * DIFF_DH ** -0.5
    p = jax.nn.softmax(s, axis=-1)
    a = (p[:, :, 0] - lam * p[:, :, 1]).astype(v.dtype)
    return jnp.einsum('bhqk,bkhe->bqhe', a, v)


def gla_mixer(zc, zl, lp, need_ctx):
    def prep(z):
        B, T, _ = z['gla_q'].shape
        q = z['gla_q'].reshape(B, T, GLA_HEADS, GLA_DK) * GLA_DK ** -0.5
        k = z['gla_k'].reshape(B, T, GLA_HEADS, GLA_DK)
        v = z['gla_v'].reshape(B, T, GLA_HEADS, HEAD_DIM)
        lr = z['gla_lr'].reshape(B, T, 2, GLA_GATE_RANK)
        logit = jnp.einsum('btrl,rlk->btrk', lr, lp['gla_gate_w2']) + lp['gla_gate_b']
        log_a = jax.nn.log_sigmoid(logit.astype(jnp.float32)) / GLA_GATE_TAU
        return q, k, v, log_a.reshape(B, T, 2, GLA_HEADS, GLA_DK)

    qc, kc, vc, ac = prep(zc)
    ql, kl, vl, al = prep(zl)
    s0 = jnp.zeros((ql.shape[0], GLA_HEADS, GLA_DK, HEAD_DIM), jnp.float32)
    dirs = [run_direction(gla_chunked, (qc, kc, vc, ac[:, :, d]), (ql, kl, vl, al[:, :, d]), s0, d == 1)
            for d in range(2)]

    def post(o, z):
        B, T = o.shape[:2]
        return rms_norm(o, lp['gla_norm_w']).reshape(B, T, GROUP_WIDTH) * jax.nn.silu(z['gla_g'])

    o_lat = post(dirs[0][1] + dirs[1][1], zl)
    o_ctx = post(dirs[0][0] + dirs[1][0], zc) if need_ctx else None
    return o_ctx, o_lat


def deltanet_mixer(zc, zl, lp, need_ctx):
    def prep(z):
        B, T, _ = z['dn_qkv'].shape
        qkv = jax.nn.silu(short_conv(z['dn_qkv'], lp['dn_conv_w']))
        q, k, v = jnp.split(qkv, 3, axis=-1)
        q = l2_norm(q.reshape(B, T, DN_HEADS, HEAD_DIM)) * HEAD_DIM ** -0.5
        k = l2_norm(k.reshape(B, T, DN_HEADS, HEAD_DIM))
        v = v.reshape(B, T, DN_HEADS, HEAD_DIM)
        a = z['dn_a'].reshape(B, T, 2, DN_HEADS).astype(jnp.float32)
        g = -jnp.exp(lp['dn_a_log'].astype(jnp.float32)) * jax.nn.softplus(
            a + lp['dn_dt_bias'].astype(jnp.float32))
        beta = jax.nn.sigmoid(z['dn_b'].reshape(B, T, 2, DN_HEADS).astype(jnp.float32))
        return q, k, v, g, beta

    qc, kc, vc, gc, bc = prep(zc)
    ql, kl, vl, gl, bl = prep(zl)
    s0 = jnp.zeros((ql.shape[0], DN_HEADS, HEAD_DIM, HEAD_DIM), jnp.float32)
    dirs = [run_direction(gated_delta_chunked, (qc, kc, vc, gc[:, :, d], bc[:, :, d]),
                          (ql, kl, vl, gl[:, :, d], bl[:, :, d]), s0, d == 1)
            for d in range(2)]

    def post(o, z):
        B, T = o.shape[:2]
        return rms_norm(o, lp['dn_norm_w']).reshape(B, T, GROUP_WIDTH) * jax.nn.silu(z['dn_g'])

    o_lat = post(dirs[0][1] + dirs[1][1], zl)
    o_ctx = post(dirs[0][0] + dirs[1][0], zc) if need_ctx else None
    return o_ctx, o_lat


def gqa_mixer(zc, zl, lp, pos_r, pos_c, need_ctx):
    def prep(z):
        B, T, _ = z['gqa_q'].shape
        q = rms_norm(z['gqa_q'].reshape(B, T, GQA_HEADS, HEAD_DIM), lp['gqa_q_norm'])
        kv = z['gqa_kv'].reshape(B, T, 2, GQA_KV_HEADS, HEAD_DIM)
        return q, rms_norm(kv[:, :, 0], lp['gqa_k_norm']), kv[:, :, 1]

    qc, kc, vc = prep(zc)
    ql, kl, vl = prep(zl)
    ql = axial_rope(ql, pos_r, pos_c)
    kl = axial_rope(kl, pos_r, pos_c)
    k_all = jnp.concatenate([kc, kl], axis=1)
    v_all = jnp.concatenate([vc, vl], axis=1)
    o_lat = blocked_queries(gqa_attend, ql, k_all, v_all)
    B, N = o_lat.shape[:2]
    o_lat = o_lat.reshape(B, N, GROUP_WIDTH)
    o_ctx = gqa_attend(qc, kc, vc).reshape(B, qc.shape[1], GROUP_WIDTH) if need_ctx else None
    return o_ctx, o_lat


def diff_mixer(zc, zl, lp, lam_init, pos_r, pos_c, need_ctx):
    def prep(z):
        B, T, _ = z['diff_q'].shape
        q = z['diff_q'].reshape(B, T, DIFF_HEADS, 2, DIFF_DH)
        k = z['diff_k'].reshape(B, T, DIFF_HEADS, 2, DIFF_DH)
        return q, k, z['diff_v'].reshape(B, T, DIFF_HEADS, HEAD_DIM)

    qc, kc, vc = prep(zc)
    ql, kl, vl = prep(zl)
    ql = axial_rope(ql, pos_r, pos_c)
    kl = axial_rope(kl, pos_r, pos_c)
    lq1, lk1, lq2, lk2 = lp['diff_lambda'].astype(jnp.float32)
    lam = jnp.exp(jnp.sum(lq1 * lk1)) - jnp.exp(jnp.sum(lq2 * lk2)) + lam_init
    k_all = jnp.concatenate([kc, kl], axis=1)
    v_all = jnp.concatenate([vc, vl], axis=1)
    attend = lambda qi, kk, vv: diff_attend(qi, kk, vv, lam)

    def post(o):
        B, T = o.shape[:2]
        return (rms_norm(o, lp['diff_norm_w']) * (1.0 - lam_init)).reshape(B, T, GROUP_WIDTH)

    o_lat = post(blocked_queries(attend, ql, k_all, v_all))
    o_ctx = post(attend(qc, kc, vc)) if need_ctx else None
    return o_ctx, o_lat


def token_mixers(h_ctx, h_lat, lp, lam_init, need_ctx):
    zc = split_columns(h_ctx @ lp['w_in'])
    zl = split_columns(h_lat @ lp['w_in'])
    n_lat = h_lat.shape[1]
    rows = n_lat // GRID_W
    pos_r = jnp.repeat(jnp.arange(rows), GRID_W)
    pos_c = jnp.tile(jnp.arange(GRID_W), rows)
    parts = [gla_mixer(zc, zl, lp, need_ctx),
             deltanet_mixer(zc, zl, lp, need_ctx),
             gqa_mixer(zc, zl, lp, pos_r, pos_c, need_ctx),
             diff_mixer(zc, zl, lp, lam_init, pos_r, pos_c, need_ctx)]
    o_lat = jnp.concatenate([p[1] for p in parts], axis=-1) @ lp['w_out']
    o_ctx = jnp.concatenate([p[0] for p in parts], axis=-1) @ lp['w_out'] if need_ctx else None
    return o_ctx, o_lat


def expert_choice_moe(h, router_w, w_gate, w_up, w_down):
    B, N, D = h.shape
    cap = EC_CAPACITY * N // N_EXPERTS
    aff = jax.nn.softmax((h @ router_w).astype(jnp.float32), axis=-1)
    weight, idx = lax.top_k(jnp.swapaxes(aff, 1, 2), cap)
    xs = jax.vmap(lambda hb, ib: hb[ib])(h, idx)
    hid = jax.nn.silu(jnp.einsum('becd,edf->becf', xs, w_gate)) * jnp.einsum('becd,edf->becf', xs, w_up)
    y = jnp.einsum('becf,efd->becd', hid, w_down) * weight[..., None].astype(h.dtype)
    return jax.vmap(lambda yb, ib: jnp.zeros((N, D), h.dtype).at[ib].add(yb))(y, idx)


def setup_inputs(seed: int = 0) -> dict:
    key = jax.random.key(seed)
    ks = jax.random.split(key, 26)
    f32 = jnp.float32
    nrm = lambda k, shape, scale: jax.random.normal(k, shape, f32) * scale
    D = D_MODEL
    L = DEPTH
    d_in = sum(s for _, s in IN_SPLITS)
    dt = jnp.exp(jax.random.uniform(ks[15], (L, 2, DN_HEADS), f32, math.log(1e-3), math.log(1e-1)))
    return {
        'x': nrm(ks[0], (BATCH, SEQ, D), 1.0),
        'c': nrm(ks[1], (BATCH, D), 1.0),
        'ctx': nrm(ks[2], (BATCH, CTX_LEN, D), 1.0),
        'c_ctx': nrm(ks[3], (D,), 1.0),
        'mod_w': nrm(ks[4], (L, D, 6 * D), 0.5 * D ** -0.5),
        'mod_b': nrm(ks[5], (L, 6 * D), 0.02),
        'norm1_w': 1.0 + nrm(ks[6], (L, D), 0.05),
        'norm2_w': 1.0 + nrm(ks[7], (L, D), 0.05),
        'w_in': nrm(ks[8], (L, D, d_in), D ** -0.5),
        'w_out': nrm(ks[9], (L, MIX_WIDTH, D), MIX_WIDTH ** -0.5),
        'gla_gate_w2': nrm(ks[10], (L, 2, GLA_GATE_RANK, GLA_HEADS * GLA_DK), GLA_GATE_RANK ** -0.5),
        'gla_gate_b': nrm(ks[11], (L, 2, GLA_HEADS * GLA_DK), 0.1),
        'gla_norm_w': 1.0 + nrm(ks[12], (L, HEAD_DIM), 0.05),
        'dn_conv_w': nrm(ks[13], (L, SHORT_CONV, 3 * DN_HEADS * HEAD_DIM), SHORT_CONV ** -0.5),
        'dn_a_log': jnp.log(jax.random.uniform(ks[14], (L, 2, DN_HEADS), f32, 1.0, 16.0)),
        'dn_dt_bias': dt + jnp.log(-jnp.expm1(-dt)),
        'dn_norm_w': 1.0 + nrm(ks[16], (L, HEAD_DIM), 0.05),
        'gqa_q_norm': 1.0 + nrm(ks[17], (L, HEAD_DIM), 0.05),
        'gqa_k_norm': 1.0 + nrm(ks[18], (L, HEAD_DIM), 0.05),
        'diff_lambda': nrm(ks[19], (L, 4, DIFF_DH), 0.1),
        'diff_norm_w': 1.0 + nrm(ks[20], (L, HEAD_DIM), 0.05),
        'router_w': nrm(ks[21], (L, D, N_EXPERTS), D ** -0.5),
        'exp_w_gate': nrm(ks[22], (L, N_EXPERTS, D, EXPERT_FF), D ** -0.5),
        'exp_w_up': nrm(ks[23], (L, N_EXPERTS, D, EXPERT_FF), D ** -0.5),
        'exp_w_down': nrm(ks[24], (L, N_EXPERTS, EXPERT_FF, D), EXPERT_FF ** -0.5),
        'final_norm_w': 1.0 + nrm(ks[25], (D,), 0.05),
    }


def reference(x, c, ctx, c_ctx, mod_w, mod_b, norm1_w, norm2_w, w_in, w_out,
              gla_gate_w2, gla_gate_b, gla_norm_w, dn_conv_w, dn_a_log, dn_dt_bias, dn_norm_w,
              gqa_q_norm, gqa_k_norm, diff_lambda, diff_norm_w,
              router_w, exp_w_gate, exp_w_up, exp_w_down, final_norm_w):
    ctx_h = ctx
    for l in range(DEPTH):
        last = l == DEPTH - 1
        lp = {'w_in': w_in[l], 'w_out': w_out[l],
              'gla_gate_w2': gla_gate_w2[l], 'gla_gate_b': gla_gate_b[l], 'gla_norm_w': gla_norm_w[l],
              'dn_conv_w': dn_conv_w[l], 'dn_a_log': dn_a_log[l], 'dn_dt_bias': dn_dt_bias[l],
              'dn_norm_w': dn_norm_w[l], 'gqa_q_norm': gqa_q_norm[l], 'gqa_k_norm': gqa_k_norm[l],
              'diff_lambda': diff_lambda[l], 'diff_norm_w': diff_norm_w[l]}
        lam_init = 0.8 - 0.6 * math.exp(-0.3 * l)
        sh1, sc1, g1, sh2, sc2, g2 = [m[:, None, :] for m in
                                      jnp.split(jax.nn.silu(c) @ mod_w[l] + mod_b[l], 6, axis=-1)]
        csh1, csc1, cg1, csh2, csc2, cg2 = jnp.split(jax.nn.silu(c_ctx) @ mod_w[l] + mod_b[l], 6, axis=-1)
        h_lat = rms_norm(x, norm1_w[l]) * (1.0 + sc1) + sh1
        h_ctx = rms_norm(ctx_h, norm1_w[l]) * (1.0 + csc1) + csh1
        o_ctx, o_lat = token_mixers(h_ctx, h_lat, lp, lam_init, not last)
        x = x + g1 * o_lat
        h2 = rms_norm(x, norm2_w[l]) * (1.0 + sc2) + sh2
        x = x + g2 * expert_choice_moe(h2, router_w[l], exp_w_gate[l], exp_w_up[l], exp_w_down[l])
        if not last:
            ctx_h = ctx_h + cg1 * o_ctx
            hc2 = rms_norm(ctx_h, norm2_w[l]) * (1.0 + csc2) + csh2
            ctx_h = ctx_h + cg2 * expert_choice_moe(hc2, router_w[l], exp_w_gate[l], exp_w_up[l], exp_w_down[l])
    return rms_norm(x, final_norm_w)
```

```python
import numpy as np
import concourse.bass as bass
import concourse.mybir as mybir
from concourse.bass_utils import run_bass_kernel_spmd

F32 = mybir.dt.float32
BF16 = mybir.dt.bfloat16
ALU = mybir.AluOpType
AF = mybir.ActivationFunctionType
AX = mybir.AxisListType

SEM_LIMIT = 24000


class Buf:
    def __init__(self, t, name):
        self.t = t
        self.name = name
        self.last_w = None
        self.readers = []
        self.dma_sem = None
        self.is_dram = False
        self.is_psum = False
        self.slot = None

    def __getitem__(self, idx):
        return View(self, self.t[idx])

    def sub(self, key):
        return Buf(self.t, f"{self.name}.{key}")

    def v(self, ap):
        return View(self, ap)


class View:
    def __init__(self, buf, ap):
        self.buf = buf
        self.ap = ap


class Op:
    __slots__ = ("eng", "fn", "waits", "inc", "idx", "is_dma", "dma_buf", "dma_val")

    def __init__(self, eng, fn):
        self.eng = eng
        self.fn = fn
        self.waits = []
        self.inc = False
        self.idx = None
        self.is_dma = False
        self.dma_buf = None
        self.dma_val = None


ENGS = ("pe", "act", "dve", "pool", "sp")


class _Scope:
    def __init__(self, S):
        self.S = S

    def __enter__(self):
        from contextlib import ExitStack
        self.old = self.S.stack
        self.S.scope_bufs.append([])
        self.st = ExitStack()
        self.st.__enter__()
        self.S.stack = self.st
        return self

    def __exit__(self, *a):
        self.S.barrier()
        for b in self.S.scope_bufs.pop():
            if b.slot is not None:
                self.S.free_slots.append(b.slot)
                b.slot = None
        self.S.stack = self.old
        return self.st.__exit__(*a)


class Sched:
    def __init__(self, nc, stack):
        self.nc = nc
        self.stack = stack
        self.ops = {e: [] for e in ENGS}
        self.all_ops = []
        self.nbuf = 0
        self.dma_counts = {}
        self._bar_pos = 0
        self._bar = {}
        self.free_slots = []
        self.nslots = 0
        self.scope_bufs = [[]]

    def sbuf(self, shape, dt=F32, name=None):
        self.nbuf += 1
        name = f"{name}_{self.nbuf}" if name else f"sb{self.nbuf}"
        t = self.stack.enter_context(self.nc.sbuf_tensor(name, list(shape), dt))
        b = Buf(t, name)
        self.scope_bufs[-1].append(b)
        return b

    def psum(self, shape, dt=F32, name=None):
        self.nbuf += 1
        name = name or f"ps{self.nbuf}"
        t = self.stack.enter_context(self.nc.psum_tensor(name, list(shape), dt))
        b = Buf(t, name)
        b.is_psum = True
        return b

    def scope(self):
        return _Scope(self)

    def dram(self, name, shape, dt=F32, kind="Internal"):
        t = self.nc.dram_tensor(name, list(shape), dt, kind=kind)
        b = Buf(t.ap(), name)
        b.is_dram = True
        return b

    def barrier(self):
        lasts = []
        for e in ENGS:
            if e in ("sp", "pool"):
                continue
            if self.ops[e]:
                lasts.append(self.ops[e][-1])
        dmas = [o for o in self.all_ops[self._bar_pos:] if o.is_dma]
        self._bar_pos = len(self.all_ops)
        lastd = {}
        for o in dmas:
            lastd[o.dma_val] = o
        for e in ("sp", "pool"):
            nd = [o for o in self.ops[e] if not o.is_dma]
            if nd:
                lasts.append(nd[-1])
        self._bar = {e: lasts + list(lastd.values()) for e in ENGS}

    def _dep(self, op, reads, writes):
        for b in reads:
            w = b.last_w
            if w is not None:
                if not (w.eng == "pe" and op.eng == "pe"):
                    op.waits.append(w)
            if b.is_psum:
                for r in b.readers:
                    if r.eng != op.eng:
                        op.waits.append(r)
            b.readers.append(op)
        for b in writes:
            w = b.last_w
            if w is not None and not (w.is_dma and op.is_dma and w.eng == op.eng) \
                    and not (w.eng == "pe" and op.eng == "pe"):
                op.waits.append(w)
            for r in b.readers:
                if r is op:
                    continue
                if not (r.eng == "pe" and op.eng == "pe"):
                    op.waits.append(r)
            b.last_w = op
            b.readers = []

    def I(self, eng, meth, *args, r=(), w=(), **kw):
        reads = list(r)
        writes = list(w)
        kw2 = {}
        for k, val in kw.items():
            if isinstance(val, View):
                if k in ("out", "accum_out"):
                    writes.append(val.buf)
                else:
                    reads.append(val.buf)
                kw2[k] = val.ap
            else:
                kw2[k] = val
        is_dma = meth in ("dma_start", "collective_compute")
        op = Op(eng, None)
        op.is_dma = is_dma
        if is_dma:
            sb = None
            if meth == "collective_compute":
                sb = writes[0]
            for k in ("in_", "out"):
                if isinstance(kw.get(k), View):
                    if sb is None or not kw[k].buf.is_dram:
                        sb = kw[k].buf
            if sb.slot is None:
                if self.free_slots:
                    sb.slot = self.free_slots.pop()
                else:
                    sb.slot = self.nslots
                    self.nslots += 1
            op.dma_buf = sb
            op.dma_val = sb.slot
        self._dep(op, reads, writes)
        if self._bar.get(eng):
            op.waits.extend(o for o in self._bar.pop(eng) if o is not op)
        op.fn = (meth, args, kw2)
        self.ops[eng].append(op)
        self.all_ops.append(op)
        return op

    def pe(self, meth, **kw):
        return self.I("pe", meth, **kw)

    def act(self, meth, **kw):
        return self.I("act", meth, **kw)

    def dve(self, meth, **kw):
        return self.I("dve", meth, **kw)

    def pool(self, meth, **kw):
        return self.I("pool", meth, **kw)

    def dma(self, out, in_, eng="sp", **kw):
        return self.I(eng, "dma_start", out=out, in_=in_, **kw)

    def emit(self, final_waits=()):
        nc = self.nc
        for op in self.all_ops:
            for wop in op.waits:
                wop.inc = True
        for op in final_waits:
            op.inc = True
        for op in self.all_ops:
            if op.is_dma:
                op.inc = True
        eng_cnt = {e: 0 for e in ENGS}
        slot_state = {}
        for op in self.all_ops:
            if not op.inc:
                continue
            if op.is_dma:
                b = op.dma_val
                stt = slot_state.setdefault(b, [0, 0])
                iv = 1 if op.fn[0] == "collective_compute" else 16
                if stt[1] + iv > SEM_LIMIT:
                    stt[0] += 1
                    stt[1] = 0
                stt[1] += iv
                op.idx = (stt[0], stt[1], iv)
            else:
                eng_cnt[op.eng] += 1
                op.idx = eng_cnt[op.eng]
        self.eng_sems = {}
        for e in ENGS:
            n = (eng_cnt[e] + SEM_LIMIT - 1) // SEM_LIMIT
            self.eng_sems[e] = [
                self.stack.enter_context(nc.semaphore(f"s_{e}_{i}")) for i in range(n)
            ]
        self.dma_sems = {}
        for b, stt in slot_state.items():
            self.dma_sems[b] = [
                self.stack.enter_context(nc.semaphore(f"d_slot{b}_{i}"))
                for i in range(stt[0] + 1)
            ]
        nsem = sum(len(v) for v in self.eng_sems.values()) + sum(
            len(v) for v in self.dma_sems.values())
        self.nsem = nsem

        def sem_of(op):
            if op.is_dma:
                k, val, iv = op.idx
                return self.dma_sems[op.dma_val][k], val
            k = (op.idx - 1) // SEM_LIMIT
            return self.eng_sems[op.eng][k], op.idx - k * SEM_LIMIT

        block = self.stack.enter_context(nc.Block())
        engmap = {"pe": block.tensor, "act": block.scalar, "dve": block.vector,
                  "pool": block.gpsimd, "sp": block.sync}

        def make(ename):
            ops = self.ops[ename]
            fw = [o for o in final_waits]

            def body(eng):
                known = {}
                for op in ops:
                    need = {}
                    for wop in op.waits:
                        sem, val = sem_of(wop)
                        key = id(sem)
                        if key not in need or need[key][1] < val:
                            need[key] = (sem, val)
                    for key, (sem, val) in need.items():
                        if known.get(key, 0) >= val:
                            continue
                        known[key] = val
                        eng.wait_ge(sem, val)
                    meth, args, kw = op.fn
                    ins = getattr(eng, meth)(*args, **kw)
                    if op.inc:
                        sem, val = sem_of(op)
                        ins.then_inc(sem, op.idx[2] if op.is_dma else 1)
                if ename == "sp":
                    need = {}
                    for wop in fw:
                        sem, val = sem_of(wop)
                        key = id(sem)
                        if key not in need or need[key][1] < val:
                            need[key] = (sem, val)
                    for key, (sem, val) in need.items():
                        eng.wait_ge(sem, val)
            return body

        for e in ENGS:
            if self.ops[e] or (e == "sp" and final_waits):
                engmap[e](make(e))


from contextlib import ExitStack

T = 4352
NT = 34
D = 2048
KC = 16
NCTX = 256
FAM = [('gla_q', 128), ('gla_k', 128), ('gla_v', 256), ('gla_g', 256), ('gla_lr', 32),
       ('dn_q', 256), ('dn_k', 256), ('dn_v', 256), ('dn_g', 256), ('dn_a', 4), ('dn_b', 4),
       ('gqa_q', 256), ('gqa_k', 128), ('gqa_v', 128),
       ('diff_q', 256), ('diff_k', 256), ('diff_v', 256)]
ZOFF = {}
_o = 0
for _n, _w in FAM:
    ZOFF[_n] = (_o, _w)
    _o += _w
ZC = _o


def wcols(h):
    r = np.arange
    c = []
    c += list(r(0 + 128 * h, 0 + 128 * h + 128))
    c += list(r(256 + 128 * h, 256 + 128 * h + 128))
    c += list(r(512 + 256 * h, 512 + 256 * h + 256))
    c += list(r(1024 + 256 * h, 1024 + 256 * h + 256))
    c += list(r(1536, 1568))
    c += list(r(1568 + 256 * h, 1568 + 256 * h + 256))
    c += list(r(1568 + 512 + 256 * h, 1568 + 512 + 256 * h + 256))
    c += list(r(1568 + 1024 + 256 * h, 1568 + 1024 + 256 * h + 256))
    c += list(r(3104 + 256 * h, 3104 + 256 * h + 256))
    c += [3616 + d * 4 + 2 * h + j for d in range(2) for j in range(2)]
    c += [3624 + d * 4 + 2 * h + j for d in range(2) for j in range(2)]
    c += list(r(3632 + 256 * h, 3632 + 256 * h + 256))
    c += list(r(4144 + 128 * h, 4144 + 128 * h + 128))
    c += list(r(4144 + 256 + 128 * h, 4144 + 256 + 128 * h + 128))
    c += list(r(4656 + 256 * h, 4656 + 256 * h + 256))
    c += list(r(5168 + 256 * h, 5168 + 256 * h + 256))
    c += list(r(5680 + 256 * h, 5680 + 256 * h + 256))
    assert len(c) == ZC
    return np.array(c)


def bc(ap, n=128):
    return ap.partition_broadcast(n)


def phase_mod(S, c2, mod_w, mod_b, modv):
    with S.scope():
        cT = S.sbuf([128, 16, 2])
        for r in range(2):
            S.dma(out=cT[:, :, r:r + 1], in_=c2.v(c2.t[r:r + 1, :].rearrange("r (k p) -> p k r", p=128)),
                  allow_slow_non_contiguous=True)
        sT = S.sbuf([128, 16, 2])
        S.act("activation", out=sT[:], in_=cT[:], func=AF.Silu)
        mb = S.sbuf([2, 12288])
        S.dma(out=mb[:], in_=mod_b.v(bc(mod_b.t[0:1, :], 2)))
        mv = S.sbuf([2, 12288])
        wr = [S.sbuf([128, 16, 512]) for _ in range(2)]
        ps = [S.psum([2, 512]) for _ in range(2)]
        mwv = mod_w.t.rearrange("(k p) n -> p k n", p=128)
        for ct in range(24):
            wb = wr[ct % 2]
            cs = slice(ct * 512, (ct + 1) * 512)
            S.dma(out=wb[:, 0:8, :], in_=mod_w.v(mwv[:, 0:8, cs]))
            S.dma(out=wb[:, 8:16, :], in_=mod_w.v(mwv[:, 8:16, cs]))
            for k in range(16):
                S.pe("matmul", out=ps[ct % 2][:], lhsT=sT[:, k, :], rhs=wb[:, k, :],
                     start=(k == 0), stop=(k == 15))
            S.dve("tensor_tensor", out=mv[:, cs], in0=ps[ct % 2][:], in1=mb[:, cs], op=ALU.add)
        S.dma(out=modv.v(modv.t), in_=mv[:], eng="pool")


def load_w_bf16(S, Wb, w_dram, stages, nk, ncols):
    for k in range(nk):
        st = stages[k % len(stages)]
        S.dma(out=st[:, :ncols], in_=w_dram.v(w_dram.t[k * 128:(k + 1) * 128, :]))
        S.I("dve" if k % 2 == 0 else "pool", "tensor_copy", out=Wb[:, k, :], in_=st[:, :ncols])


def rsqrt_col(S, out, in_, scale, eps):
    S.dve("tensor_scalar", out=out[:], in0=in_[:], scalar1=scale, scalar2=eps,
          op0=ALU.mult, op1=ALU.add)
    S.act("activation", out=out[:], in_=out[:], func=AF.Sqrt)
    S.dve("reciprocal", out=out[:], in_=out[:])


def rms_mod(S, x, A, Bv, hb, junk, ssq, rstd, width=2048):
    S.act("activation", out=junk[:], in_=x[:], func=AF.Square, accum_out=ssq[:])
    rsqrt_col(S, rstd, ssq, 1.0 / width, 1e-6)
    S.dve("scalar_tensor_tensor", out=hb[:], in0=x[:], scalar=rstd[:], in1=A[:],
          op0=ALU.mult, op1=ALU.mult)
    S.pool("tensor_tensor", out=hb[:], in0=hb[:], in1=Bv[:], op=ALU.add)


def transpose_tile(S, src, dstT, idt, pst, nk=16, base=0):
    ng = (nk + 3) // 4
    for g in range(ng):
        p = pst[(base + g) % len(pst)]
        n = min(4, nk - g * 4)
        for j in range(n):
            k = g * 4 + j
            S.pe("transpose", out=p[:, j * 128:(j + 1) * 128], in_=src[:, k * 128:(k + 1) * 128],
                 identity=idt[:])
        eng = "act" if g % 2 else "dve"
        dv = dstT.v(dstT.t[:, g * 4:g * 4 + n, :].rearrange("p a b -> p (a b)"))
        if eng == "act":
            S.act("activation", out=dv, in_=p[:, :n * 128], func=AF.Copy)
        else:
            S.dve("tensor_copy", out=dv, in_=p[:, :n * 128])


def phase_in(S, xin, modv, nw, w_in, z, ident):
    with S.scope():
        idt = S.sbuf([128, 128])
        S.dma(out=idt[:], in_=ident.v(ident.t))
        Wb = S.sbuf([128, 16, ZC], BF16)
        zst = [S.sbuf([128, ZC]) for _ in range(2)]
        load_w_bf16(S, Wb, w_in, zst, 16, ZC)
        nwb = S.sbuf([128, 2048])
        S.dma(out=nwb[:], in_=nw.v(bc(nw.t[0:1, :])))
        A = S.sbuf([128, 2048])
        Bv = S.sbuf([128, 2048])
        xr = [S.sbuf([128, 2048]) for _ in range(2)]
        hb = S.sbuf([128, 2048])
        junk = S.sbuf([128, 2048], BF16)
        hT = [S.sbuf([128, 16, 128], BF16) for _ in range(2)]
        ssq = S.sbuf([128, 1])
        rstd = S.sbuf([128, 1])
        pst = [S.psum([128, 512]) for _ in range(2)]
        psz = [S.psum([128, 512]) for _ in range(3)]
        for t in range(NT):
            if t == 0 or t == 2:
                r = 1 if t == 0 else 0
                S.dma(out=A[:], in_=modv.v(bc(modv.t[r:r + 1, 2048:4096])))
                S.dma(out=Bv[:], in_=modv.v(bc(modv.t[r:r + 1, 0:2048])))
                S.dve("scalar_tensor_tensor", out=A[:], in0=A[:], scalar=1.0, in1=nwb[:],
                      op0=ALU.add, op1=ALU.mult)
            x = xr[t % 2]
            S.dma(out=x[:], in_=xin.v(xin.t[t * 128:(t + 1) * 128, :]))
            rms_mod(S, x, A, Bv, hb, junk, ssq, rstd)
            transpose_tile(S, hb, hT[t % 2], idt, pst)
            zs = zst[t % 2]
            for ct in range((ZC + 511) // 512):
                w = min(512, ZC - ct * 512)
                p = psz[ct % 3]
                for k in range(16):
                    S.pe("matmul", out=p[:, :w], lhsT=hT[t % 2][:, k, :],
                         rhs=Wb[:, k, ct * 512:ct * 512 + w], start=(k == 0), stop=(k == 15))
                if ct % 2:
                    S.act("activation", out=zs[:, ct * 512:ct * 512 + w], in_=p[:, :w], func=AF.Copy)
                else:
                    S.dve("tensor_copy", out=zs[:, ct * 512:ct * 512 + w], in_=p[:, :w])
            S.dma(out=z.v(z.t[t * 128:(t + 1) * 128, :]), in_=zs[:], eng="pool")


def rope_tables():
    out = {}
    t = np.arange(4096)
    pos = {0: (t // 64).astype(np.float64), 1: (t % 64).astype(np.float64)}
    for d in (128, 64):
        half = d // 2
        nf = half // 2
        inv = 10000.0 ** (-np.arange(0, half, 2, dtype=np.float64) / half)
        C = np.ones((T, d), np.float32)
        Sg = np.zeros((T, d), np.float32)
        for ax in range(2):
            ang = (pos[ax][:, None].astype(np.float32) * inv[None, :].astype(np.float32)).astype(np.float32)
            cs, sn = np.cos(ang), np.sin(ang)
            o = ax * half
            C[NCTX:, o:o + nf] = cs
            C[NCTX:, o + nf:o + 2 * nf] = cs
            Sg[NCTX:, o:o + nf] = -sn
            Sg[NCTX:, o + nf:o + 2 * nf] = sn
        out[f'ropeC{d}'] = C
        out[f'ropeS{d}'] = Sg
    return out


def rope_apply(S, x, xo, t1, Ct, St, nvec, d, c0):
    nf = d // 4
    cs = slice(c0, c0 + nvec * d)
    v3 = lambda b: b.v(b.t[:, cs].rearrange("p (v d) -> p v d", d=d))
    cb = Ct.v(Ct.t[:, :].unsqueeze(1).to_broadcast([128, nvec, d]))
    S.dve("tensor_tensor", out=v3(t1), in0=v3(x), in1=cb, op=ALU.mult)
    v4 = lambda b: b.t[:, cs].rearrange("p (v a h f) -> p v a h f", a=2, h=2, f=nf)
    s4 = St.t[:, :].rearrange("p (a h f) -> p a h f", a=2, h=2)
    for hf in range(2):
        sb = St.v(s4[:, :, hf, :].unsqueeze(1).to_broadcast([128, nvec, 2, nf]))
        S.dve("tensor_tensor", out=xo.v(v4(xo)[:, :, :, hf, :]), in0=x.v(v4(x)[:, :, :, 1 - hf, :]),
               in1=sb, op=ALU.mult)
    S.dve("tensor_tensor", out=xo[:, cs], in0=xo[:, cs], in1=t1[:, cs], op=ALU.add)


DEBUG = {}


def phase_attn(S, z, consts, prm, ocat, need_ctx=True):
    gq0 = ZOFF['gqa_q'][0]
    df0 = ZOFF['diff_q'][0]
    with S.scope():
        idt = S.sbuf([128, 128])
        S.dma(out=idt[:], in_=consts['ident'].v(consts['ident'].t))
        ones = S.sbuf([128, 128], BF16)
        S.dve("memset", out=ones[:], constant=1.0) if False else S.I("dve", "memset", ones.t[:], 1.0, w=[ones])
        wn = S.sbuf([128, 3, 128])
        S.dma(out=wn[:, 0, :], in_=prm['gqa_qn'].v(bc(prm['gqa_qn'].t[0:1, :])))
        S.dma(out=wn[:, 1, :], in_=prm['gqa_qn'].v(bc(prm['gqa_qn'].t[0:1, :])))
        S.dma(out=wn[:, 2, :], in_=prm['gqa_kn'].v(bc(prm['gqa_kn'].t[0:1, :])))
        dnw = S.sbuf([128, 128])
        S.dma(out=dnw[:], in_=prm['diff_nw'].v(bc(prm['diff_nw'].t[0:1, :])))
        lamc = S.sbuf([128, 2])
        S.dma(out=lamc[:], in_=prm['lamc'].v(bc(prm['lamc'].t[0:1, :])))
        S.dve("tensor_scalar", out=dnw[:], in0=dnw[:], scalar1=lamc[:, 0:1], scalar2=None, op0=ALU.mult)
        lamt = S.sbuf([128, 4, 64])
        S.dma(out=lamt.v(lamt.t[:, :, :].rearrange("p a b -> p (a b)")),
              in_=prm['diff_lambda'].v(bc(prm['diff_lambda'].t[0:1, :])))
        lj = S.sbuf([128, 2, 64])
        lam2 = S.sbuf([128, 2])
        S.dve("tensor_tensor", out=lj[:, 0, :], in0=lamt[:, 0, :], in1=lamt[:, 1, :], op=ALU.mult)
        S.dve("tensor_tensor", out=lj[:, 1, :], in0=lamt[:, 2, :], in1=lamt[:, 3, :], op=ALU.mult)
        S.dve("tensor_reduce", out=lam2[:], in_=lj[:], axis=AX.X, op=ALU.add)
        S.act("activation", out=lam2[:], in_=lam2[:], func=AF.Exp)
        nlam = S.sbuf([128, 1])
        S.dve("tensor_tensor", out=nlam[:], in0=lam2[:, 1:2], in1=lam2[:, 0:1], op=ALU.subtract)
        S.dve("tensor_scalar", out=nlam[:], in0=nlam[:], scalar1=lamc[:, 1:2], scalar2=None, op0=ALU.add)

        if DEBUG.get('setup_only'):
            S.dma(out=ocat.v(ocat.t[0:128, 0:384]), in_=wn.v(wn.t[:, :, :].rearrange("p a b -> p (a b)")), eng="pool")
            S.dma(out=ocat.v(ocat.t[0:128, 384:386]), in_=lam2[:, 0:2], eng="pool")
            S.dma(out=ocat.v(ocat.t[0:128, 512:640]), in_=dnw[:], eng="pool")
            return
        gqT = [S.sbuf([128, T], BF16) for _ in range(2)]
        gkT = S.sbuf([128, T], BF16)
        gv = S.sbuf([128, NT, 128], BF16)
        dqT = [S.sbuf([128, T], BF16) for _ in range(2)]
        dkT = [S.sbuf([128, T], BF16) for _ in range(2)]
        dv = S.sbuf([128, NT, 256], BF16)

        pst = [S.psum([128, 512]) for _ in range(2)]
        pS = [S.psum([128, 512]) for _ in range(2)]
        pO = [S.psum([128, 512]) for _ in range(2)]
        pR = [S.psum([128, 512]) for _ in range(2)]

        with S.scope():
            zin = [S.sbuf([128, 512 + 768]) for _ in range(2)]
            xo = [S.sbuf([128, 512 + 768]) for _ in range(2)]
            t1 = S.sbuf([128, 512 + 768])
            C128 = [S.sbuf([128, 128]) for _ in range(2)]
            S128 = [S.sbuf([128, 128]) for _ in range(2)]
            C64 = [S.sbuf([128, 64]) for _ in range(2)]
            S64 = [S.sbuf([128, 64]) for _ in range(2)]
            sq = S.sbuf([128, 384])
            ss = S.sbuf([128, 3])
            for t in range(DEBUG.get('ntl', NT)):
                rs_ = slice(t * 128, (t + 1) * 128)
                zi = zin[t % 2]
                x2 = xo[t % 2]
                S.dma(out=zi[:, 0:512], in_=z.v(z.t[rs_, gq0:gq0 + 512]))
                S.dma(out=zi[:, 512:1280], in_=z.v(z.t[rs_, df0:df0 + 768]))
                for nm, tl in (('ropeC128', C128), ('ropeS128', S128), ('ropeC64', C64), ('ropeS64', S64)):
                    S.dma(out=tl[t % 2][:], in_=consts[nm].v(consts[nm].t[rs_, :]))
                stop = DEBUG.get('stop', 99)
                if stop == 1:
                    S.dma(out=ocat.v(ocat.t[rs_, 0:1024]), in_=zi[:, 0:1024], eng="pool")
                    S.dma(out=ocat.v(ocat.t[rs_, 0:128]), in_=C128[t % 2][:], eng="pool")
                    S.dma(out=ocat.v(ocat.t[rs_, 128:256]), in_=S128[t % 2][:], eng="pool")
                    S.dma(out=ocat.v(ocat.t[rs_, 256:320]), in_=C64[t % 2][:], eng="pool")
                    S.dma(out=ocat.v(ocat.t[rs_, 320:384]), in_=S64[t % 2][:], eng="pool")
                    continue
                if DEBUG.get('skip_norm'):
                    pass
                else:
                  S.dve("tensor_tensor", out=sq[:], in0=zi[:, 0:384], in1=zi[:, 0:384], op=ALU.mult)
                  S.dve("tensor_reduce", out=ss[:], in_=sq.v(sq.t[:, :].rearrange("p (v d) -> p v d", d=128)),
                      axis=AX.X, op=ALU.add)
                  rsqrt_col(S, ss, ss, 1.0 / 128, 1e-6)
                  z3 = zi.v(zi.t[:, 0:384].rearrange("p (v d) -> p v d", d=128))
                  if not DEBUG.get('skip_bc'):
                    S.dve("tensor_tensor", out=z3, in0=z3,
                      in1=ss.v(ss.t[:, :].unsqueeze(2).to_broadcast([128, 3, 128])), op=ALU.mult)
                  S.dve("tensor_tensor", out=z3, in0=z3, in1=wn[:, :, :], op=ALU.mult)
                if DEBUG.get('skip_rope'):
                    S.dve("tensor_copy", out=x2[:, 0:1024], in_=zi[:, 0:1024])
                else:
                    rope_apply(S, zi, x2, t1, C128[t % 2], S128[t % 2], 3, 128, 0)
                    rope_apply(S, zi, x2, t1, C64[t % 2], S64[t % 2], 8, 64, 512)
                if stop == 3:
                    S.dma(out=ocat.v(ocat.t[rs_, 0:384]), in_=x2[:, 0:384], eng="pool")
                    S.dma(out=ocat.v(ocat.t[rs_, 512:1024]), in_=x2[:, 512:1024], eng="pool")
                    continue
                em = DEBUG.get('em', 0)
                p = pst[0]
                for j in range(3):
                    S.pe("transpose", out=p[:, j * 128:(j + 1) * 128], in_=x2[:, j * 128:(j + 1) * 128], identity=idt[:])
                if em != 1:
                    S.act("activation", out=gqT[0][:, rs_], in_=p[:, 0:128], func=AF.Copy)
                    S.act("activation", out=gqT[1][:, rs_], in_=p[:, 128:256], func=AF.Copy)
                if em != 2:
                    S.act("activation", out=gkT[:, rs_], in_=p[:, 256:384], func=AF.Copy)
                p = pst[1]
                if em != 3:
                  for j in range(4):
                    S.pe("transpose", out=p[:, j * 128:(j + 1) * 128], in_=x2[:, 512 + j * 128:512 + (j + 1) * 128], identity=idt[:])
                  if em != 1:
                    S.dve("tensor_copy", out=dqT[0][:, rs_], in_=p[:, 0:128])
                    S.dve("tensor_copy", out=dqT[1][:, rs_], in_=p[:, 128:256])
                  if em != 2:
                    S.dve("tensor_copy", out=dkT[0][:, rs_], in_=p[:, 256:384])
                    S.dve("tensor_copy", out=dkT[1][:, rs_], in_=p[:, 384:512])
                if stop == 4:
                    S.dma(out=ocat.v(ocat.t[rs_, 0:384]), in_=x2[:, 0:384], eng="pool")
                    S.dma(out=ocat.v(ocat.t[rs_, 512:1024]), in_=x2[:, 512:1024], eng="pool")
                    continue
                S.pool("tensor_copy", out=gv[:, t, :], in_=zi[:, 384:512])
                S.pool("tensor_copy", out=dv[:, t, :], in_=zi[:, 1024:1280])
                if DEBUG.get('pre_only'):
                    S.dma(out=ocat.v(ocat.t[rs_, 0:384]), in_=x2[:, 0:384], eng="pool")
                    S.dma(out=ocat.v(ocat.t[rs_, 512:1024]), in_=x2[:, 512:1024], eng="pool")
        if DEBUG.get('pre_only'):
            return

        pT = [S.sbuf([128, 512], BF16) for _ in range(3)]
        oTs = [S.sbuf([128, 512]) for _ in range(2)]
        oT2 = [S.sbuf([128, 512]) for _ in range(2)]
        rinv = [S.sbuf([128, 512]) for _ in range(2)]
        otok = [S.sbuf([128, 4, 128]) for _ in range(2)]
        sq2 = S.sbuf([128, 4, 128])
        ss2 = S.sbuf([128, 4])
        S.I("dve", "memset", ss2.t[:], 1.0, w=[ss2])
        cnt = [0]

        def one_map(qT, kT, prow, vv, vcol, q0, nq, nkt, scale):
            i = cnt[0]
            cnt[0] += 1
            po, pr = pO[i % 2], pR[i % 2]
            for kt in range(nkt):
                ps = pS[kt % 2]
                S.pe("matmul", out=ps[:, :nq], lhsT=kT[prow, kt * 128:(kt + 1) * 128], rhs=qT[prow, q0:q0 + nq],
                     start=True, stop=True)
                pt = pT[kt % 3]
                S.act("activation", out=pt[:, :nq], in_=ps[:, :nq], func=AF.Exp, scale=scale)
                S.pe("matmul", out=po[:, :nq], lhsT=vv[:, kt, vcol], rhs=pt[:, :nq], start=(kt == 0), stop=(kt == nkt - 1))
                S.pe("matmul", out=pr[:, :nq], lhsT=ones[:, :], rhs=pt[:, :nq], start=(kt == 0), stop=(kt == nkt - 1))
            return po, pr

        blocks = ([(0, 256, 2)] if need_ctx else []) + [(NCTX + i * 512, 512, NT) for i in range(8)]
        bi = 0
        for (q0, nq, nkt) in blocks:
            for hd in range(2):
                po, pr = one_map(gqT[hd], gkT, slice(0, 128), gv, slice(0, 128), q0, nq, nkt, 128 ** -0.5)
                ri = rinv[bi % 2]
                ot = oTs[bi % 2]
                S.dve("reciprocal", out=ri[:, :nq], in_=pr[:, :nq])
                S.dve("tensor_tensor", out=ot[:, :nq], in0=po[:, :nq], in1=ri[:, :nq], op=ALU.mult)
                ok = otok[bi % 2]
                p = pst[bi % 2]
                for j in range(nq // 128):
                    S.pe("transpose", out=p[:, j * 128:(j + 1) * 128], in_=ot[:, j * 128:(j + 1) * 128], identity=idt[:])
                S.act("activation", out=ok.v(ok.t[:, 0:nq // 128, :].rearrange("p a b -> p (a b)")), in_=p[:, :nq], func=AF.Copy)
                S.dma(out=ocat.v(ocat.t[q0:q0 + nq, 512 + hd * 128:512 + (hd + 1) * 128].rearrange("(a p) d -> p a d", p=128)),
                      in_=ok[:, 0:nq // 128, :], eng="pool")
                bi += 1
            for hd in range(2):
                po, pr = one_map(dqT[hd], dkT[hd], slice(0, 64), dv, slice(hd * 128, (hd + 1) * 128), q0, nq, nkt, 64 ** -0.5)
                ri = rinv[bi % 2]
                ot = oTs[bi % 2]
                S.dve("reciprocal", out=ri[:, :nq], in_=pr[:, :nq])
                S.dve("tensor_tensor", out=ot[:, :nq], in0=po[:, :nq], in1=ri[:, :nq], op=ALU.mult)
                po, pr = one_map(dqT[hd], dkT[hd], slice(64, 128), dv, slice(hd * 128, (hd + 1) * 128), q0, nq, nkt, 64 ** -0.5)
                o2 = oT2[bi % 2]
                S.dve("reciprocal", out=ri[:, :nq], in_=pr[:, :nq])
                S.dve("tensor_tensor", out=o2[:, :nq], in0=po[:, :nq], in1=ri[:, :nq], op=ALU.mult)
                S.dve("scalar_tensor_tensor", out=ot[:, :nq], in0=o2[:, :nq], scalar=nlam[:, 0:1], in1=ot[:, :nq],
                      op0=ALU.mult, op1=ALU.add)
                ok = otok[bi % 2]
                p = pst[bi % 2]
                na = nq // 128
                for j in range(na):
                    S.pe("transpose", out=p[:, j * 128:(j + 1) * 128], in_=ot[:, j * 128:(j + 1) * 128], identity=idt[:])
                okv = ok.v(ok.t[:, 0:na, :].rearrange("p a b -> p (a b)"))
                S.act("activation", out=okv, in_=p[:, :nq], func=AF.Copy)
                S.dve("tensor_tensor", out=sq2[:, 0:na, :], in0=ok[:, 0:na, :], in1=ok[:, 0:na, :], op=ALU.mult)
                S.dve("tensor_reduce", out=ss2[:, 0:na], in_=sq2[:, 0:na, :], axis=AX.X, op=ALU.add)
                rsqrt_col(S, ss2, ss2, 1.0 / 128, 1e-6)
                S.dve("tensor_tensor", out=ok[:, 0:na, :], in0=ok[:, 0:na, :],
                      in1=ss2.v(ss2.t[:, 0:na].unsqueeze(2).to_broadcast([128, na, 128])), op=ALU.mult)
                S.dve("tensor_tensor", out=ok[:, 0:na, :], in0=ok[:, 0:na, :],
                      in1=dnw.v(dnw.t[:, :].unsqueeze(1).to_broadcast([128, na, 128])), op=ALU.mult)
                S.dma(out=ocat.v(ocat.t[q0:q0 + nq, 768 + hd * 128:768 + (hd + 1) * 128].rearrange("(a p) d -> p a d", p=128)),
                      in_=ok[:, 0:na, :], eng="pool")
                bi += 1


def tri_consts():
    j = np.arange(128)[:, None]
    i = np.arange(128)[None, :]
    c = {}
    c['mincl_f'] = (j <= i).astype(np.float32)
    c['mincl_r'] = (j >= i).astype(np.float32)
    c['mstr_f'] = (j > i).astype(np.float32)
    c['mstr_r'] = (j < i).astype(np.float32)
    c['ones1'] = np.ones((1, 128), np.float32)
    hm = np.zeros((128, 2), np.float32); hm[:64, 0] = 1; hm[64:, 1] = 1
    c['hmask'] = hm
    c['ones128'] = np.ones((128, 128), np.float32)
    return c


def block_order(d):
    if d == 0:
        return list(range(NT))
    return [1, 0] + list(range(NT - 1, 1, -1))


def phase_gla(S, z, consts, prm, ocat):
    c0 = ZOFF['gla_q'][0]
    with S.scope():
        idt = S.sbuf([128, 128])
        S.dma(out=idt[:], in_=consts['ident'].v(consts['ident'].t))
        msk = {}
        for nm in ('mincl_f', 'mincl_r', 'mstr_f', 'mstr_r'):
            msk[nm] = S.sbuf([128, 128], name="gla_" + nm)
            S.dma(out=msk[nm][:], in_=consts[nm].v(consts[nm].t))
        mS = {}
        for nm in ('mincl_f', 'mincl_r', 'mstr_f', 'mstr_r'):
            mS[nm] = S.sbuf([128, 128], name="glaS_" + nm)
            S.dve("tensor_scalar", out=mS[nm][:], in0=msk[nm][:], scalar1=-1.0 / 16, scalar2=None, op0=ALU.mult)
        ones1 = S.sbuf([1, 128])
        S.dma(out=ones1[:], in_=consts['ones1'].v(consts['ones1'].t))
        w2p = [S.sbuf([32, 128]) for _ in range(2)]
        gb = [S.sbuf([1, 128]) for _ in range(2)]
        for d in range(2):
            S.dma(out=w2p[d][:], in_=prm[f'w2pad{d}'].v(prm[f'w2pad{d}'].t))
            S.dma(out=gb[d][:], in_=prm[f'gb{d}'].v(prm[f'gb{d}'].t))
        nwb = S.sbuf([128, 128])
        S.dma(out=nwb[:], in_=prm['gla_nw'].v(bc(prm['gla_nw'].t[0:1, :])))
        oacc = S.sbuf([128, NT, 256])
        Sst = S.sbuf([128, 128])
        R = 2
        zin = [S.sbuf([128, 800]) for _ in range(R)]
        lrT = [S.sbuf([32, 128]) for _ in range(R)]
        ee = [S.sbuf([128, 128]) for _ in range(R)]
        sp = [S.sbuf([128, 128]) for _ in range(R)]
        EbT = [S.sbuf([128, 128]) for _ in range(R)]
        EnbT = [S.sbuf([128, 128]) for _ in range(R)]
        Ebm = [S.sbuf([128, 128]) for _ in range(R)]
        qgT = [S.sbuf([128, 128]) for _ in range(R)]
        qgTh = [S.sbuf([128, 2, 128]) for _ in range(R)]
        kinvT = [S.sbuf([128, 2, 128]) for _ in range(R)]
        hm = S.sbuf([128, 2])
        S.dma(out=hm[:], in_=consts['hmask'].v(consts['hmask'].t))
        kd = [S.sbuf([128, 128]) for _ in range(R)]
        scm = [S.sbuf([128, 2, 128]) for _ in range(R)]
        osum = [S.sbuf([128, 2, 128]) for _ in range(R)]
        sg = [S.sbuf([128, 256]) for _ in range(R)]
        sq = S.sbuf([128, 2, 128])
        ss = S.sbuf([128, 2])
        pA, pB, pC, pD, pE, pF, pG, pH = [S.psum([128, 512]) for _ in range(8)]
        for d in range(2):
            sfx = '_f' if d == 0 else '_r'
            Mincl, Mstr, MinclS, MstrS = msk['mincl' + sfx], msk['mstr' + sfx], mS['mincl' + sfx], mS['mstr' + sfx]
            last = 127 if d == 0 else 0
            S.I("dve", "memset", Sst.t[:], 0.0, w=[Sst])
            for n, blk in enumerate(block_order(d)[:DEBUG.get('gnb', NT)]):
                i = n % R
                rs_ = slice(blk * 128, (blk + 1) * 128)
                zi = zin[i]
                S.dma(out=zi[:], in_=z.v(z.t[rs_, c0:c0 + 800]))
                q2, k2, v2, g2, lr = (zi[:, 0:128], zi[:, 128:256], zi[:, 256:512], zi[:, 512:768], zi[:, 768:800])
                S.pe("transpose", out=pA[0:32, 0:128], in_=lr, identity=idt[:])
                S.act("activation", out=lrT[i][:], in_=pA[0:32, 0:128], func=AF.Copy)
                if DEBUG.get('gstop') == 1:
                    continue
                S.pe("matmul", out=pB[:, 0:128], lhsT=lrT[i][:], rhs=w2p[d][:], start=True, stop=False)
                S.pe("matmul", out=pB[:, 0:128], lhsT=ones1[:], rhs=gb[d][:], start=False, stop=True)
                if DEBUG.get('gstop') == 2:
                    continue
                S.act("activation", out=ee[i][:], in_=pB[:, 0:128], func=AF.Exp, scale=-1.0)
                S.act("activation", out=sp[i][:], in_=ee[i][:], func=AF.Ln, bias=1.0)
                if DEBUG.get('gstop') == 3:
                    continue
                S.pe("matmul", out=pC[:, 0:128], lhsT=sp[i][:], rhs=MinclS[:], start=True, stop=True)
                S.pe("matmul", out=pD[:, 0:128], lhsT=MstrS[:], rhs=sp[i][:], start=True, stop=True)
                S.act("activation", out=EbT[i][:], in_=pC[:, 0:128], func=AF.Exp)
                S.act("activation", out=EnbT[i][:], in_=pC[:, 0:128], func=AF.Exp, scale=-1.0)
                S.act("activation", out=Ebm[i][:], in_=pD[:, 0:128], func=AF.Exp)
                if DEBUG.get('gstop') == 4:
                    continue
                S.pe("transpose", out=pE[:, 0:128], in_=q2, identity=idt[:])
                S.pe("transpose", out=pE[:, 128:256], in_=k2, identity=idt[:])
                S.dve("scalar_tensor_tensor", out=qgT[i][:], in0=pE[:, 0:128], scalar=0.125, in1=EbT[i][:],
                      op0=ALU.mult, op1=ALU.mult)
                for hh in range(2):
                    S.dve("scalar_tensor_tensor", out=kinvT[i][:, hh, :], in0=pE[:, 128:256], scalar=hm[:, hh:hh + 1],
                          in1=EnbT[i][:], op0=ALU.mult, op1=ALU.mult)
                    S.dve("tensor_scalar", out=qgTh[i][:, hh, :], in0=qgT[i][:], scalar1=hm[:, hh:hh + 1], scalar2=None,
                          op0=ALU.mult)
                S.dve("tensor_tensor", out=kd[i][:], in0=k2, in1=Ebm[i][:], op=ALU.mult)
                if DEBUG.get('gstop') == 6:
                    continue
                for hh in range(2):
                    r = slice(64 * hh, 64 * hh + 64)
                    S.pe("matmul", out=pF[:, hh * 128:(hh + 1) * 128], lhsT=kinvT[i][:, hh, :], rhs=qgT[i][:],
                         start=True, stop=True)
                S.dve("tensor_tensor", out=scm[i][:, :, :],
                      in0=pF.v(pF.t[:, 0:256].rearrange("p (a b) -> p a b", a=2)),
                      in1=Mincl.v(Mincl.t[:, :].unsqueeze(1).to_broadcast([128, 2, 128])), op=ALU.mult)
                if DEBUG.get('gstop') == 7:
                    continue
                for hh in range(2):
                    r = slice(64 * hh, 64 * hh + 64)
                    S.pe("matmul", out=pG[:, hh * 128:(hh + 1) * 128], lhsT=scm[i][:, hh, :],
                         rhs=zi[:, 256 + hh * 128:256 + (hh + 1) * 128], start=True, stop=False)
                    S.pe("matmul", out=pG[:, hh * 128:(hh + 1) * 128], lhsT=qgTh[i][:, hh, :], rhs=Sst[:],
                         start=False, stop=True)
                S.pe("matmul", out=pH[:, 0:256], lhsT=kd[i][:], rhs=v2, start=True, stop=True)
                if DEBUG.get('gstop') == 8:
                    continue
                for hh in range(2):
                    r = slice(64 * hh, 64 * hh + 64)
                    S.dve("scalar_tensor_tensor", out=Sst[r, :], in0=Sst[r, :], scalar=EbT[i][r, last:last + 1],
                          in1=pH[r, hh * 128:(hh + 1) * 128], op0=ALU.mult, op1=ALU.add)
                if d == 0:
                    S.act("activation", out=oacc[:, blk, :], in_=pG[:, 0:256], func=AF.Copy)
                else:
                    os_ = osum[i]
                    osv = os_.v(os_.t[:, :, :].rearrange("p a b -> p (a b)"))
                    S.dve("tensor_tensor", out=osv, in0=pG[:, 0:256], in1=oacc[:, blk, :], op=ALU.add)
                    S.act("activation", out=sg[i][:], in_=g2, func=AF.Silu)
                    S.dve("tensor_tensor", out=sq[:, :, :], in0=os_[:, :, :], in1=os_[:, :, :], op=ALU.mult)
                    S.dve("tensor_reduce", out=ss[:], in_=sq[:, :, :], axis=AX.X, op=ALU.add)
                    rsqrt_col(S, ss, ss, 1.0 / 128, 1e-6)
                    S.dve("tensor_tensor", out=os_[:, :, :], in0=os_[:, :, :],
                          in1=ss.v(ss.t[:, :].unsqueeze(2).to_broadcast([128, 2, 128])), op=ALU.mult)
                    S.dve("tensor_tensor", out=os_[:, :, :], in0=os_[:, :, :],
                          in1=nwb.v(nwb.t[:, :].unsqueeze(1).to_broadcast([128, 2, 128])), op=ALU.mult)
                    S.dve("tensor_tensor", out=osv, in0=osv, in1=sg[i][:], op=ALU.mult)
                    S.dma(out=ocat.v(ocat.t[rs_, 0:256]), in_=osv, eng="pool")
        if DEBUG.get('gstop'):
            S.dma(out=ocat.v(ocat.t[0:128, 0:128]), in_=nwb[:], eng="pool")


def dn_consts():
    p = np.arange(128)[:, None]
    f = np.arange(128)[None, :]
    same = (p // 64) == (f // 64)
    c = {}
    for d, le in (('f', lambda a, b: a <= b), ('r', lambda a, b: a >= b)):
        lt = (lambda a, b: a < b) if d == 'f' else (lambda a, b: a > b)
        c[f'dn_MTi_{d}'] = le(p, f).astype(np.float32)
        c[f'dn_MTSn_{d}'] = -(lt(p, f) & same).astype(np.float32)
        c[f'dn_MSn_{d}'] = -(lt(f, p) & same).astype(np.float32)
        c[f'dn_MSo_{d}'] = (lt(f, p) & ~same).astype(np.float32)
        c[f'dn_Mc_{d}'] = le(p, f).astype(np.float32)
    return c


def phase_dn_prep(S, z, prm, dnp):
    q0 = ZOFF['dn_q'][0]
    a0 = ZOFF['dn_a'][0]
    with S.scope():
        wk = S.sbuf([128, 5, 768])
        for k in range(5):
            S.dma(out=wk[:, k, :], in_=prm['dn_convw'].v(bc(prm['dn_convw'].t[k:k + 1, :])))
        nea = S.sbuf([128, 4])
        dtb = S.sbuf([128, 4])
        S.dma(out=nea[:], in_=prm['dn_alog'].v(bc(prm['dn_alog'].t[0:1, :])))
        S.dma(out=dtb[:], in_=prm['dn_dtb'].v(bc(prm['dn_dtb'].t[0:1, :])))
        S.act("activation", out=nea[:], in_=nea[:], func=AF.Exp)
        S.dve("tensor_scalar", out=nea[:], in0=nea[:], scalar1=-1.0, scalar2=None, op0=ALU.mult)
        xs = [[S.sbuf([128, 768]) for _ in range(5)] for _ in range(2)]
        ab = [S.sbuf([128, 8]) for _ in range(2)]
        tmp = [S.sbuf([128, 768]) for _ in range(2)]
        acc = S.sbuf([128, 768])
        yo = [S.sbuf([128, 776]) for _ in range(2)]
        sq = S.sbuf([128, 512])
        ss = S.sbuf([128, 4])
        e4 = S.sbuf([128, 4])
        e5 = S.sbuf([128, 4])
        for t in range(NT):
            t0 = t * 128
            lo, hi = (0, NCTX) if t < 2 else (NCTX, T)
            x5 = xs[t % 2]
            for k in range(5):
                s = k - 2
                a = max(t0 + s, lo)
                b = min(t0 + s + 128, hi)
                if a != t0 + s or b != t0 + s + 128:
                    S.I("dve", "memset", x5[k].t[:], 0.0, w=[x5[k]])
                S.dma(out=x5[k][a - (t0 + s):b - (t0 + s), :], in_=z.v(z.t[a:b, q0:q0 + 768]))
            S.dma(out=ab[t % 2][:], in_=z.v(z.t[t0:t0 + 128, a0:a0 + 8]))
            S.dve("tensor_tensor", out=acc[:], in0=x5[0][:], in1=wk[:, 0, :], op=ALU.mult)
            for k in range(1, 5):
                tm = tmp[k % 2]
                S.pool("tensor_tensor", out=tm[:], in0=x5[k][:], in1=wk[:, k, :], op=ALU.mult)
                S.dve("tensor_tensor", out=acc[:], in0=acc[:], in1=tm[:], op=ALU.add)
            y = yo[t % 2]
            S.act("activation", out=y[:, 0:768], in_=acc[:], func=AF.Silu)
            S.dve("tensor_tensor", out=sq[:], in0=y[:, 0:512], in1=y[:, 0:512], op=ALU.mult)
            S.dve("tensor_reduce", out=ss[:], in_=sq.v(sq.t[:, :].rearrange("p (v d) -> p v d", d=128)),
                  axis=AX.X, op=ALU.add)
            rsqrt_col(S, ss, ss, 1.0, 1e-6)
            S.dve("tensor_scalar", out=ss[:, 0:2], in0=ss[:, 0:2], scalar1=128 ** -0.5, scalar2=None, op0=ALU.mult)
            y3 = y.v(y.t[:, 0:512].rearrange("p (v d) -> p v d", d=128))
            S.dve("tensor_tensor", out=y3, in0=y3, in1=ss.v(ss.t[:, :].unsqueeze(2).to_broadcast([128, 4, 128])),
                  op=ALU.mult)
            S.dve("tensor_tensor", out=e4[:], in0=ab[t % 2][:, 0:4], in1=dtb[:], op=ALU.add)
            S.act("activation", out=e4[:], in_=e4[:], func=AF.Exp)
            S.act("activation", out=e4[:], in_=e4[:], func=AF.Ln, bias=1.0)
            S.dve("tensor_tensor", out=y[:, 768:772], in0=e4[:], in1=nea[:], op=ALU.mult)
            S.act("activation", out=e5[:], in_=ab[t % 2][:, 4:8], func=AF.Exp, scale=-1.0)
            S.dve("tensor_scalar", out=e5[:], in0=e5[:], scalar1=1.0, scalar2=None, op0=ALU.add)
            S.dve("reciprocal", out=y[:, 772:776], in_=e5[:])
            S.dma(out=dnp.v(dnp.t[t0:t0 + 128, :]), in_=y[:], eng="pool")


def phase_dn(S, z, dnp, consts, prm, ocat):
    g0 = ZOFF['dn_g'][0]
    with S.scope():
        idt = S.sbuf([128, 128])
        S.dma(out=idt[:], in_=consts['ident'].v(consts['ident'].t))
        ones = S.sbuf([128, 128])
        S.dma(out=ones[:], in_=consts['ones128'].v(consts['ones128'].t))
        M = {}
        for d in ('f', 'r'):
            for nm in ('MTi', 'MTSn', 'MSn', 'MSo', 'Mc'):
                key = f'dn_{nm}_{d}'
                M[key] = S.sbuf([128, 128], name='sb_' + key)
                S.dma(out=M[key][:], in_=consts[key].v(consts[key].t))
        nwb = S.sbuf([128, 128])
        S.dma(out=nwb[:], in_=prm['dn_nw'].v(bc(prm['dn_nw'].t[0:1, :])))
        oacc = S.sbuf([128, NT, 256])
        Sst = [S.sbuf([128, 128], name=f"dnS{h_}") for h_ in range(2)]
        R = 2
        zin = [S.sbuf([128, 776]) for _ in range(R)]
        gin = [S.sbuf([128, 256]) for _ in range(R)]
        sc = [S.sbuf([128, 4]) for _ in range(R)]
        egc = [S.sbuf([128, 2]) for _ in range(R)]
        edl = [S.sbuf([128, 2]) for _ in range(R)]
        dl = [S.sbuf([128, 2]) for _ in range(R)]
        bgc = [S.sbuf([128, 2]) for _ in range(R)]
        tdl = [S.sbuf([128, 2]) for _ in range(R)]

        def mk(n=128):
            return [[S.sbuf([128, n]) for _ in range(2)] for _ in range(R)]
        DD, kT, qT, qgT, E3, t1, E1, t2, E2 = mk(256), mk(), mk(), mk(), mk(), mk(), mk(), mk(), mk()
        DTm, attnT, bm1, bm2, XT, e2a, X, e2b, Lo = mk(), mk(), mk(), mk(), mk(), mk(), mk(), mk(), mk()
        P_ = [[[S.sbuf([128, 128]) for _ in range(2)] for _ in range(2)] for _ in range(R)]
        PT = [[[S.sbuf([128, 128]) for _ in range(2)] for _ in range(2)] for _ in range(R)]
        Rm = [[[S.sbuf([128, 128]) for _ in range(2)] for _ in range(2)] for _ in range(R)]
        RT = [[[S.sbuf([128, 128]) for _ in range(2)] for _ in range(2)] for _ in range(R)]
        A1, TmT, vb, kbg, kd, usb, wT, vnew = mk(), mk(), mk(), mk(), mk(), mk(), mk(), mk()
        osum = [S.sbuf([128, 2, 128]) for _ in range(R)]
        sg = [S.sbuf([128, 256]) for _ in range(R)]
        sq = S.sbuf([128, 2, 128])
        ss = S.sbuf([128, 2])
        pRB, pT, pK, pI1, pI2, pU, pO, pS = [S.psum([128, 512]) for _ in range(8)]
        for d in range(2):
            dn = 'f' if d == 0 else 'r'
            MTi, MTSn, MSn, MSo, Mc = (M[f'dn_{nm}_{dn}'] for nm in ('MTi', 'MTSn', 'MSn', 'MSo', 'Mc'))
            for hh in range(2):
                S.I("dve", "memset", Sst[hh].t[:], 0.0, w=[Sst[hh]])
            for n, blk in enumerate(block_order(d)[:DEBUG.get('dnb', NT)]):
                i = n % R
                rs_ = slice(blk * 128, (blk + 1) * 128)
                zi = zin[i]
                S.dma(out=zi[:], in_=dnp.v(dnp.t[rs_, :]))
                if d == 1:
                    S.dma(out=gin[i][:], in_=z.v(z.t[rs_, g0:g0 + 256]))
                g2 = zi[:, 768 + 2 * d:768 + 2 * d + 2]
                be = lambda hh: zi[:, 772 + 2 * d + hh:772 + 2 * d + hh + 1]
                S.pe("matmul", out=pS[:, 256:258], lhsT=Mc[:], rhs=g2, start=True, stop=True)
                S.pe("matmul", out=pS[:, 258:260], lhsT=ones[:], rhs=g2, start=True, stop=True)
                S.dve("tensor_copy", out=sc[i][:], in_=pS[:, 256:260])
                S.act("activation", out=egc[i][:], in_=sc[i][:, 0:2], func=AF.Exp)
                S.dve("tensor_tensor", out=tdl[i][:], in0=sc[i][:, 2:4], in1=sc[i][:, 0:2], op=ALU.subtract)
                S.act("activation", out=edl[i][:], in_=tdl[i][:], func=AF.Exp)
                S.act("activation", out=dl[i][:], in_=sc[i][:, 2:4], func=AF.Exp)
                S.dve("tensor_tensor", out=bgc[i][:], in0=egc[i][:], in1=zi[:, 772 + 2 * d:772 + 2 * d + 2], op=ALU.mult)
                for hh in range(2):
                    q_h = zi[:, hh * 128:(hh + 1) * 128]
                    k_h = zi[:, 256 + hh * 128:256 + (hh + 1) * 128]
                    v_h = zi[:, 512 + hh * 128:512 + (hh + 1) * 128]
                    gcc = sc[i][:, hh:hh + 1]
                    c2 = slice(hh * 256, hh * 256 + 256)
                    ca = slice(hh * 256, hh * 256 + 128)
                    cb = slice(hh * 256 + 128, hh * 256 + 256)
                    S.dve("tensor_scalar", out=DD[i][hh][:, 0:128], in0=idt[:], scalar1=gcc, scalar2=None, op0=ALU.mult)
                    S.dve("tensor_scalar", out=DD[i][hh][:, 128:256], in0=idt[:], scalar1=be(hh), scalar2=None, op0=ALU.mult)
                    S.pe("matmul", out=pRB[:, c2], lhsT=ones[:], rhs=DD[i][hh][:], start=True, stop=True)
                    S.pe("transpose", out=pT[:, ca], in_=q_h, identity=idt[:])
                    S.pe("transpose", out=pT[:, cb], in_=k_h, identity=idt[:])
                    S.dve("tensor_copy", out=qT[i][hh][:], in_=pT[:, ca])
                    S.dve("tensor_copy", out=kT[i][hh][:], in_=pT[:, cb])
                    S.act("activation", out=E3[i][hh][:], in_=pRB[:, ca], func=AF.Exp)
                    S.dve("tensor_tensor", out=qgT[i][hh][:], in0=qT[i][hh][:], in1=E3[i][hh][:], op=ALU.mult)
                    S.pe("matmul", out=pK[:, ca], lhsT=kT[i][hh][:], rhs=kT[i][hh][:], start=True, stop=True)
                    S.pe("matmul", out=pK[:, cb], lhsT=kT[i][hh][:], rhs=qT[i][hh][:], start=True, stop=True)
                    S.dve("tensor_scalar", out=t1[i][hh][:], in0=pRB[:, ca], scalar1=gcc, scalar2=0.0,
                          op0=ALU.subtract, op1=ALU.min)
                    S.dve("tensor_scalar", out=t2[i][hh][:], in0=pRB[:, ca], scalar1=gcc, scalar2=0.0,
                          op0=ALU.subtract, op1=ALU.max)
                    S.dve("tensor_tensor", out=bm1[i][hh][:], in0=pRB[:, cb], in1=MTSn[:], op=ALU.mult)
                    S.act("activation", out=E1[i][hh][:], in_=t1[i][hh][:], func=AF.Exp)
                    S.act("activation", out=E2[i][hh][:], in_=t2[i][hh][:], func=AF.Exp, scale=-1.0)
                    S.dve("tensor_tensor", out=DTm[i][hh][:], in0=E1[i][hh][:], in1=MTi[:], op=ALU.mult)
                    S.dve("tensor_tensor", out=attnT[i][hh][:], in0=pK[:, cb], in1=DTm[i][hh][:], op=ALU.mult)
                    S.dve("tensor_tensor", out=bm2[i][hh][:], in0=bm1[i][hh][:], in1=E1[i][hh][:], op=ALU.mult)
                    S.dve("tensor_tensor", out=XT[i][hh][:], in0=pK[:, ca], in1=bm2[i][hh][:], op=ALU.mult)
                    S.dve("scalar_tensor_tensor", out=e2a[i][hh][:], in0=E2[i][hh][:], scalar=be(hh), in1=MSn[:],
                          op0=ALU.mult, op1=ALU.mult)
                    S.dve("tensor_tensor", out=X[i][hh][:], in0=pK[:, ca], in1=e2a[i][hh][:], op=ALU.mult)
                    S.dve("scalar_tensor_tensor", out=e2b[i][hh][:], in0=E2[i][hh][:], scalar=be(hh), in1=MSo[:],
                          op0=ALU.mult, op1=ALU.mult)
                    S.dve("tensor_tensor", out=Lo[i][hh][:], in0=pK[:, ca], in1=e2b[i][hh][:], op=ALU.mult)
                    Pc, PTc, Rc, RTc = X[i][hh], XT[i][hh], Rm[i][hh][0], RT[i][hh][0]
                    S.dve("tensor_tensor", out=Rc[:], in0=X[i][hh][:], in1=idt[:], op=ALU.add)
                    S.dve("tensor_tensor", out=RTc[:], in0=XT[i][hh][:], in1=idt[:], op=ALU.add)
                    pI = pI1 if hh == 0 else pI2
                    for k in range(1, 6):
                        Pn, PTn = P_[i][hh][k % 2], PT[i][hh][k % 2]
                        Rn, RTn = Rm[i][hh][k % 2], RT[i][hh][k % 2]
                        S.pe("matmul", out=pI[:, 0:128], lhsT=PTc[:], rhs=Pc[:], start=True, stop=True)
                        S.pe("matmul", out=pI[:, 128:256], lhsT=Pc[:], rhs=PTc[:], start=True, stop=True)
                        S.act("activation", out=Pn[:], in_=pI[:, 0:128], func=AF.Copy)
                        S.act("activation", out=PTn[:], in_=pI[:, 128:256], func=AF.Copy)
                        S.pe("matmul", out=pI[:, 256:384], lhsT=PTn[:], rhs=Rc[:], start=True, stop=True)
                        S.pe("matmul", out=pI[:, 384:512], lhsT=Pn[:], rhs=RTc[:], start=True, stop=True)
                        S.act("activation", out=Rn[:], in_=pI[:, 256:384], func=AF.Copy) if False else None
                        S.dve("tensor_tensor", out=Rn[:], in0=pI[:, 256:384], in1=Rc[:], op=ALU.add)
                        S.dve("tensor_tensor", out=RTn[:], in0=pI[:, 384:512], in1=RTc[:], op=ALU.add)
                        Pc, PTc, Rc, RTc = Pn, PTn, Rn, RTn
                    Td, TdT = Rc, RTc
                    S.pe("matmul", out=pI[:, 0:128], lhsT=Lo[i][hh][:], rhs=TdT[:], start=True, stop=True)
                    S.act("activation", out=A1[i][hh][:], in_=pI[:, 0:128], func=AF.Copy)
                    S.pe("matmul", out=pI[:, 128:256], lhsT=Td[:], rhs=A1[i][hh][:], start=True, stop=True)
                    S.dve("tensor_tensor", out=TmT[i][hh][:], in0=TdT[:], in1=pI[:, 128:256], op=ALU.subtract)
                    S.pool("tensor_scalar", out=vb[i][hh][:], in0=v_h, scalar1=be(hh), scalar2=None, op0=ALU.mult)
                    S.pool("tensor_scalar", out=kbg[i][hh][:], in0=k_h, scalar1=bgc[i][:, hh:hh + 1], scalar2=None, op0=ALU.mult)
                    S.pool("tensor_scalar", out=kd[i][hh][:], in0=k_h, scalar1=edl[i][:, hh:hh + 1], scalar2=None, op0=ALU.mult)
                    cu = slice(hh * 256, hh * 256 + 128)
                    cw = slice(hh * 256 + 128, hh * 256 + 256)
                    S.pe("matmul", out=pU[:, cu], lhsT=TmT[i][hh][:], rhs=vb[i][hh][:], start=True, stop=True)
                    S.pe("matmul", out=pU[:, cw], lhsT=kbg[i][hh][:], rhs=TmT[i][hh][:], start=True, stop=True)
                    S.act("activation", out=usb[i][hh][:], in_=pU[:, cu], func=AF.Copy)
                    S.act("activation", out=wT[i][hh][:], in_=pU[:, cw], func=AF.Copy)
                    S.pe("matmul", out=pO[:, cu], lhsT=wT[i][hh][:], rhs=Sst[hh][:], start=True, stop=True)
                    S.dve("tensor_tensor", out=vnew[i][hh][:], in0=usb[i][hh][:], in1=pO[:, cu], op=ALU.subtract)
                    S.pe("matmul", out=pO[:, cw], lhsT=qgT[i][hh][:], rhs=Sst[hh][:], start=True, stop=False)
                    S.pe("matmul", out=pO[:, cw], lhsT=attnT[i][hh][:], rhs=vnew[i][hh][:], start=False, stop=True)
                    S.pe("matmul", out=pS[:, hh * 128:(hh + 1) * 128], lhsT=kd[i][hh][:], rhs=vnew[i][hh][:],
                         start=True, stop=True)
                    S.dve("scalar_tensor_tensor", out=Sst[hh][:], in0=Sst[hh][:], scalar=dl[i][:, hh:hh + 1],
                          in1=pS[:, hh * 128:(hh + 1) * 128], op0=ALU.mult, op1=ALU.add)
                    if d == 0:
                        S.dve("tensor_copy", out=oacc[:, blk, hh * 128:(hh + 1) * 128], in_=pO[:, cw])
                    else:
                        S.dve("tensor_tensor", out=osum[i][:, hh, :], in0=pO[:, cw],
                              in1=oacc[:, blk, hh * 128:(hh + 1) * 128], op=ALU.add)
                if d == 1:
                    os_ = osum[i]
                    osv = os_.v(os_.t[:, :, :].rearrange("p a b -> p (a b)"))
                    S.act("activation", out=sg[i][:], in_=gin[i][:], func=AF.Silu)
                    S.dve("tensor_tensor", out=sq[:, :, :], in0=os_[:, :, :], in1=os_[:, :, :], op=ALU.mult)
                    S.dve("tensor_reduce", out=ss[:], in_=sq[:, :, :], axis=AX.X, op=ALU.add)
                    rsqrt_col(S, ss, ss, 1.0 / 128, 1e-6)
                    S.dve("tensor_tensor", out=os_[:, :, :], in0=os_[:, :, :],
                          in1=ss.v(ss.t[:, :].unsqueeze(2).to_broadcast([128, 2, 128])), op=ALU.mult)
                    S.dve("tensor_tensor", out=os_[:, :, :], in0=os_[:, :, :],
                          in1=nwb.v(nwb.t[:, :].unsqueeze(1).to_broadcast([128, 2, 128])), op=ALU.mult)
                    S.dve("tensor_tensor", out=osv, in0=osv, in1=sg[i][:], op=ALU.mult)
                    S.dma(out=ocat.v(ocat.t[rs_, 256:512]), in_=osv, eng="pool")


CTX_TILES = (0, 17)
NOWN = 17


def phase_c1(S, ofull, xin, modv, w_out, xmid, ident, ctx_tiles=CTX_TILES, gathered=False, rpc=512):
    with S.scope():
        idt = S.sbuf([128, 128])
        S.dma(out=idt[:], in_=ident.v(ident.t))
        Wb = S.sbuf([128, 16, 2048], BF16)
        ot = [S.sbuf([128, 2048]) for _ in range(2)]
        xt = [S.sbuf([128, 2048]) for _ in range(2)]
        load_w_bf16(S, Wb, w_out, ot + xt, 16, 2048)
        G1 = S.sbuf([128, 2048])
        oT = [S.sbuf([128, 16, 128], BF16) for _ in range(2)]
        xm = [S.sbuf([128, 2048]) for _ in range(2)]
        pst = [S.psum([128, 512]) for _ in range(2)]
        py = [S.psum([128, 512]) for _ in range(4)]
        order = list(ctx_tiles) + [t for t in range(NT) if t not in ctx_tiles]
        for n, t in enumerate(order):
            if n == 0 or n == 2:
                r = 1 if n == 0 else 0
                S.dma(out=G1[:], in_=modv.v(bc(modv.t[r:r + 1, 4096:6144])))
            rs_ = slice(t * 128, (t + 1) * 128)
            o_, x_ = ot[n % 2], xt[n % 2]
            if gathered:
                for r in range(2):
                    g0 = gat_row(t, r, rpc)
                    S.dma(out=o_[:, r * 1024:(r + 1) * 1024], in_=ofull.v(ofull.t[g0:g0 + 128, :]))
            else:
                S.dma(out=o_[:], in_=ofull.v(ofull.t[rs_, :]))
            S.dma(out=x_[:], in_=xin.v(xin.t[rs_, :]))
            transpose_tile(S, o_, oT[n % 2], idt, pst)
            for ct in range(4):
                cs = slice(ct * 512, (ct + 1) * 512)
                for k in range(16):
                    S.pe("matmul", out=py[ct][:], lhsT=oT[n % 2][:, k, :], rhs=Wb[:, k, cs], start=(k == 0), stop=(k == 15))
                S.dve("tensor_tensor", out=xm[n % 2][:, cs], in0=py[ct][:], in1=G1[:, cs], op=ALU.mult)
                S.pool("tensor_tensor", out=xm[n % 2][:, cs], in0=xm[n % 2][:, cs], in1=x_[:, cs], op=ALU.add)
            S.dma(out=xmid.v(xmid.t[rs_, :]), in_=xm[n % 2][:], eng="pool")


def phase_c2(S, xmid, modv, nw2, router_w, h2T, wm, consts, ctx_tiles=CTX_TILES, nown=NOWN, natural=False):
    ident = consts['ident']
    with S.scope():
        idt = S.sbuf([128, 128])
        S.dma(out=idt[:], in_=ident.v(ident.t))
        nwb = S.sbuf([128, 2048])
        S.dma(out=nwb[:], in_=nw2.v(bc(nw2.t[0:1, :])))
        rw = S.sbuf([128, 16, 16])
        S.dma(out=rw[:], in_=router_w.v(router_w.t.rearrange("(k p) e -> p k e", p=128)))
        A = S.sbuf([128, 2048])
        Bv = S.sbuf([128, 2048])
        xt = [S.sbuf([128, 2048]) for _ in range(2)]
        hb = S.sbuf([128, 2048])
        junk = S.sbuf([128, 2048], BF16)
        hT32 = [S.sbuf([128, 16, 128]) for _ in range(2)]
        hTb = [S.sbuf([128, 16, 128], BF16) for _ in range(2)]
        ssq = S.sbuf([128, 1])
        rstd = S.sbuf([128, 1])
        mx = S.sbuf([128, 1])
        sm = S.sbuf([128, 1])
        ex = S.sbuf([128, 16])
        aff = S.sbuf([128, 16])
        affT = S.sbuf([16, T])
        pst = [S.psum([128, 512]) for _ in range(2)]
        pl = S.psum([128, 512])
        pa = S.psum([128, 512])
        order = list(ctx_tiles) + [t for t in range(NT) if t not in ctx_tiles]
        for n, t in enumerate(order):
            if n == 0 or n == 2:
                r = 1 if n == 0 else 0
                S.dma(out=A[:], in_=modv.v(bc(modv.t[r:r + 1, 4 * 2048:5 * 2048])))
                S.dma(out=Bv[:], in_=modv.v(bc(modv.t[r:r + 1, 3 * 2048:4 * 2048])))
                S.dve("scalar_tensor_tensor", out=A[:], in0=A[:], scalar=1.0, in1=nwb[:], op0=ALU.add, op1=ALU.mult)
            rs_ = slice(t * 128, (t + 1) * 128)
            x_ = xt[n % 2]
            S.dma(out=x_[:], in_=xmid.v(xmid.t[rs_, :]))
            rms_mod(S, x_, A, Bv, hb, junk, ssq, rstd)
            transpose_tile(S, hb, hT32[n % 2], idt, pst)
            if t < nown:
                S.pool("tensor_copy", out=hTb[n % 2][:, :, :], in_=hT32[n % 2][:, :, :])
                S.dma(out=h2T.v(h2T.t[:, :, t * 128:(t + 1) * 128]), in_=hTb[n % 2][:, :, :], eng="pool")
            for k in range(16):
                S.pe("matmul", out=pl[:, 0:16], lhsT=hT32[n % 2][:, k, :], rhs=rw[:, k, :], start=(k == 0), stop=(k == 15))
            S.dve("tensor_reduce", out=mx[:], in_=pl[:, 0:16], axis=AX.X, op=ALU.max)
            S.dve("tensor_scalar", out=mx[:], in0=mx[:], scalar1=-1.0, scalar2=None, op0=ALU.mult)
            S.act("activation", out=ex[:], in_=pl[:, 0:16], func=AF.Exp, bias=mx[:], accum_out=sm[:])
            S.dve("reciprocal", out=sm[:], in_=sm[:])
            S.dve("tensor_scalar", out=aff[:], in0=ex[:], scalar1=sm[:], scalar2=None, op0=ALU.mult)
            S.pe("transpose", out=pa[0:16, 0:128], in_=aff[:], identity=idt[:])
            S.act("activation", out=affT[:, rs_], in_=pa[0:16, 0:128], func=AF.Copy)
        work = S.sbuf([16, T])
        m8 = S.sbuf([16, 8])
        S.dve("tensor_copy", out=work[:], in_=affT[:])
        if natural:
            lat = work[:, NCTX:T]
            ctxv = work[:, 0:NCTX]
        else:
            lat = work.v(bass.AP(work.t, 128, [[T, 16], [2176, 2], [1, 2048]]))
            ctxv = work.v(bass.AP(work.t, 0, [[T, 16], [2176, 2], [1, 128]]))
        for (view, kk) in ((lat, 512), (ctxv, 32)):
            for _ in range(kk // 8):
                S.dve("max", out=m8[:], in_=view)
                S.dve("match_replace", out=view, in_to_replace=m8[:], in_values=view, imm_value=0.0)
        S.dve("tensor_tensor", out=work[:], in0=affT[:], in1=work[:], op=ALU.subtract)
        wmt = [S.sbuf([128, 16]) for _ in range(2)]
        for t in range(NT):
            S.pe("transpose", out=pa[:, 0:16], in_=work[:, t * 128:(t + 1) * 128], identity=idt[0:16, 0:16])
            S.act("activation", out=wmt[t % 2][:], in_=pa[:, 0:16], func=AF.Copy)
            S.dma(out=wm.v(wm.t[t * 128:(t + 1) * 128, :]), in_=wmt[t % 2][:], eng="pool")


def phase_moe(S, h2T, wm, xmid, modv, wg, wu, wd, xout, final_nw=None):
    groups = [(0, 6), (6, 6), (12, 5)]
    with S.scope():
        G2c = S.sbuf([128, 2048])
        G2l = None
        hT = S.sbuf([128, 16, 768], BF16)
        yacc = S.sbuf([128, 6, 2048])
        hid = S.sbuf([128, 8, 768], BF16)
        wmt = S.sbuf([128, 6, 16])
        sgu = [[S.sbuf([128, 16, 128]) for _ in range(2)] for _ in range(2)]
        bgu = [[S.sbuf([128, 16, 128], BF16) for _ in range(2)] for _ in range(2)]
        sd = [S.sbuf([128, 8, 256]) for _ in range(2)]
        bd = [S.sbuf([128, 8, 256], BF16) for _ in range(2)]
        sgt = [S.sbuf([128, 512], BF16) for _ in range(2)]
        xm = S.sbuf([128, 2048])
        nwf = None
        if final_nw is not None:
            nwf = S.sbuf([128, 2048])
            S.dma(out=nwf[:], in_=final_nw.v(bc(final_nw.t[0:1, :])))
            junk = S.sbuf([128, 2048], BF16)
            ssq = S.sbuf([128, 1])
            rstd = S.sbuf([128, 1])
        pg = [S.psum([128, 512]) for _ in range(2)]
        pu = [S.psum([128, 512]) for _ in range(2)]
        pd = [S.psum([128, 512]) for _ in range(4)]
        cnt = 0
        dcnt = 0
        for (t0, ntl) in groups:
            ntok = ntl * 128
            S.dma(out=hT[:, :, 0:ntok], in_=h2T.v(h2T.t[:, :, t0 * 128:t0 * 128 + ntok]))
            S.dma(out=wmt[:, 0:ntl, :], in_=wm.v(wm.t[t0 * 128:t0 * 128 + ntok, :].rearrange("(a p) e -> p a e", p=128)))
            S.I("pool", "memset", yacc.t[:], 0.0, w=[yacc])
            subs = [(s0, min(512, ntok - s0)) for s0 in range(0, ntok, 512)]
            for e in range(16):
                for fc in range(8):
                    rg = cnt % 2
                    cnt += 1
                    fs = slice(fc * 128, (fc + 1) * 128)
                    for j, wsrc in enumerate((wg, wu)):
                        S.dma(out=sgu[rg][j][:, :, :], in_=wsrc.v(wsrc.t[e].rearrange("(k p) f -> p k f", p=128)[:, :, fs]))
                        S.I("pool" if j == 0 else "dve", "tensor_copy", out=bgu[rg][j][:, :, :], in_=sgu[rg][j][:, :, :])
                    for si, (s0, sn) in enumerate(subs):
                        for k in range(16):
                            S.pe("matmul", out=pg[si % 2][:, :sn], lhsT=bgu[rg][0][:, k, :], rhs=hT[:, k, s0:s0 + sn],
                                 start=(k == 0), stop=(k == 15))
                        for k in range(16):
                            S.pe("matmul", out=pu[si % 2][:, :sn], lhsT=bgu[rg][1][:, k, :], rhs=hT[:, k, s0:s0 + sn],
                                 start=(k == 0), stop=(k == 15))
                        S.act("activation", out=sgt[si % 2][:, :sn], in_=pg[si % 2][:, :sn], func=AF.Silu)
                        S.dve("tensor_tensor", out=hid[:, fc, s0:s0 + sn], in0=pu[si % 2][:, :sn], in1=sgt[si % 2][:, :sn], op=ALU.mult)
                for dc in range(8):
                    rg = dcnt % 2
                    dcnt += 1
                    ds_ = slice(dc * 256, (dc + 1) * 256)
                    S.dma(out=sd[rg][:, :, :], in_=wd.v(wd.t[e].rearrange("(k p) d -> p k d", p=128)[:, :, ds_]))
                    S.act("activation", out=bd[rg][:, :, :], in_=sd[rg][:, :, :], func=AF.Copy)
                    for tl in range(ntl):
                        pp = pd[(dc * ntl + tl) % 4]
                        for k in range(8):
                            S.pe("matmul", out=pp[:, 0:256], lhsT=hid[:, k, tl * 128:(tl + 1) * 128], rhs=bd[rg][:, k, :],
                                 start=(k == 0), stop=(k == 7))
                        S.dve("scalar_tensor_tensor", out=yacc[:, tl, ds_], in0=pp[:, 0:256], scalar=wmt[:, tl, e:e + 1],
                              in1=yacc[:, tl, ds_], op0=ALU.mult, op1=ALU.add)
            for tl in range(ntl):
                t = t0 + tl
                if t == 0:
                    S.dma(out=G2c[:], in_=modv.v(bc(modv.t[1:2, 5 * 2048:6 * 2048])))
                if t == 1:
                    S.dma(out=G2c[:], in_=modv.v(bc(modv.t[0:1, 5 * 2048:6 * 2048])))
                S.dma(out=xm[:], in_=xmid.v(xmid.t[t * 128:(t + 1) * 128, :]))
                S.dve("tensor_tensor", out=yacc[:, tl, :], in0=yacc[:, tl, :], in1=G2c[:], op=ALU.mult)
                S.pool("tensor_tensor", out=yacc[:, tl, :], in0=yacc[:, tl, :], in1=xm[:], op=ALU.add)
                if nwf is not None:
                    S.act("activation", out=junk[:], in_=yacc[:, tl, :], func=AF.Square, accum_out=ssq[:])
                    rsqrt_col(S, rstd, ssq, 1.0 / 2048, 1e-6)
                    S.dve("scalar_tensor_tensor", out=yacc[:, tl, :], in0=yacc[:, tl, :], scalar=rstd[:], in1=nwf[:],
                          op0=ALU.mult, op1=ALU.mult)
                S.dma(out=xout.v(xout.t[t * 128:(t + 1) * 128, :]), in_=yacc[:, tl, :], eng="pool")


def phase_moe_part(S, h2T, wm, wg, wu, wd, ypart, nexp=8):
    groups = [(t0, min(6, NT - t0)) for t0 in range(0, NT, 6)]
    with S.scope():
        hT = S.sbuf([128, 16, 768], BF16)
        yacc = S.sbuf([128, 6, 2048])
        hid = S.sbuf([128, 8, 768], BF16)
        wmt = S.sbuf([128, 6, 16])
        sgu = [[S.sbuf([128, 16, 128]) for _ in range(2)] for _ in range(2)]
        bgu = [[S.sbuf([128, 16, 128], BF16) for _ in range(2)] for _ in range(2)]
        sd = [S.sbuf([128, 8, 256]) for _ in range(2)]
        bd = [S.sbuf([128, 8, 256], BF16) for _ in range(2)]
        sgt = [S.sbuf([128, 512], BF16) for _ in range(2)]
        pg = [S.psum([128, 512]) for _ in range(2)]
        pu = [S.psum([128, 512]) for _ in range(2)]
        pd = [S.psum([128, 512]) for _ in range(4)]
        cnt = 0
        dcnt = 0
        for (t0, ntl) in groups:
            ntok = ntl * 128
            S.dma(out=hT[:, :, 0:ntok], in_=h2T.v(h2T.t[:, :, t0 * 128:t0 * 128 + ntok]))
            S.dma(out=wmt[:, 0:ntl, :], in_=wm.v(wm.t[t0 * 128:t0 * 128 + ntok, :].rearrange("(a p) e -> p a e", p=128)))
            S.I("pool", "memset", yacc.t[:], 0.0, w=[yacc])
            subs = [(s0, min(512, ntok - s0)) for s0 in range(0, ntok, 512)]
            for e in range(nexp):
                for fc in range(8):
                    rg = cnt % 2
                    cnt += 1
                    fs = slice(fc * 128, (fc + 1) * 128)
                    for j, wsrc in enumerate((wg, wu)):
                        S.dma(out=sgu[rg][j][:, :, :], in_=wsrc.v(wsrc.t[e].rearrange("(k p) f -> p k f", p=128)[:, :, fs]))
                        S.I("pool" if j == 0 else "dve", "tensor_copy", out=bgu[rg][j][:, :, :], in_=sgu[rg][j][:, :, :])
                    for si, (s0, sn) in enumerate(subs):
                        for k in range(16):
                            S.pe("matmul", out=pg[si % 2][:, :sn], lhsT=bgu[rg][0][:, k, :], rhs=hT[:, k, s0:s0 + sn],
                                 start=(k == 0), stop=(k == 15))
                        for k in range(16):
                            S.pe("matmul", out=pu[si % 2][:, :sn], lhsT=bgu[rg][1][:, k, :], rhs=hT[:, k, s0:s0 + sn],
                                 start=(k == 0), stop=(k == 15))
                        S.act("activation", out=sgt[si % 2][:, :sn], in_=pg[si % 2][:, :sn], func=AF.Silu)
                        S.dve("tensor_tensor", out=hid[:, fc, s0:s0 + sn], in0=pu[si % 2][:, :sn], in1=sgt[si % 2][:, :sn], op=ALU.mult)
                for dc in range(8):
                    rg = dcnt % 2
                    dcnt += 1
                    ds_ = slice(dc * 256, (dc + 1) * 256)
                    S.dma(out=sd[rg][:, :, :], in_=wd.v(wd.t[e].rearrange("(k p) d -> p k d", p=128)[:, :, ds_]))
                    S.act("activation", out=bd[rg][:, :, :], in_=sd[rg][:, :, :], func=AF.Copy)
                    for tl in range(ntl):
                        pp = pd[(dc * ntl + tl) % 4]
                        for k in range(8):
                            S.pe("matmul", out=pp[:, 0:256], lhsT=hid[:, k, tl * 128:(tl + 1) * 128], rhs=bd[rg][:, k, :],
                                 start=(k == 0), stop=(k == 7))
                        S.dve("scalar_tensor_tensor", out=yacc[:, tl, ds_], in0=pp[:, 0:256], scalar=wmt[:, tl, e:e + 1],
                              in1=yacc[:, tl, ds_], op0=ALU.mult, op1=ALU.add)
            S.dma(out=ypart.v(ypart.t[t0 * 128:t0 * 128 + ntok, :].rearrange("(a p) d -> p a d", p=128)),
                  in_=yacc[:, 0:ntl, :], eng="pool")


def phase_fin(S, ygat, xmid, modv, xnext, final_nw=None, out_lat=None, rpc=256):
    with S.scope():
        G2 = S.sbuf([128, 2048])
        y0 = [S.sbuf([128, 2048]) for _ in range(2)]
        y1 = [S.sbuf([128, 2048]) for _ in range(2)]
        xm = [S.sbuf([128, 2048]) for _ in range(2)]
        if final_nw is not None:
            nwf = S.sbuf([128, 2048])
            S.dma(out=nwf[:], in_=final_nw.v(bc(final_nw.t[0:1, :])))
            junk = S.sbuf([128, 2048], BF16)
            ssq = S.sbuf([128, 1])
            rstd = S.sbuf([128, 1])
        for t in range(NT):
            if final_nw is not None and t < 2:
                continue
            if t == 0 or t == 2 or (final_nw is not None and t == 2):
                r = 1 if t == 0 else 0
                S.dma(out=G2[:], in_=modv.v(bc(modv.t[r:r + 1, 5 * 2048:6 * 2048])))
            rs_ = slice(t * 128, (t + 1) * 128)
            a, b, x_ = y0[t % 2], y1[t % 2], xm[t % 2]
            ga, gb_ = gat_row(t, 0, rpc), gat_row(t, 1, rpc)
            S.dma(out=a[:], in_=ygat.v(ygat.t[ga:ga + 128, :]))
            S.dma(out=b[:], in_=ygat.v(ygat.t[gb_:gb_ + 128, :]))
            S.dma(out=x_[:], in_=xmid.v(xmid.t[rs_, :]))
            S.dve("tensor_tensor", out=a[:], in0=a[:], in1=b[:], op=ALU.add)
            S.pool("tensor_tensor", out=a[:], in0=a[:], in1=G2[:], op=ALU.mult)
            S.dve("tensor_tensor", out=a[:], in0=a[:], in1=x_[:], op=ALU.add)
            if final_nw is not None:
                S.act("activation", out=junk[:], in_=a[:], func=AF.Square, accum_out=ssq[:])
                rsqrt_col(S, rstd, ssq, 1.0 / 2048, 1e-6)
                S.dve("scalar_tensor_tensor", out=a[:], in0=a[:], scalar=rstd[:], in1=nwf[:], op0=ALU.mult, op1=ALU.mult)
                S.dma(out=out_lat.v(out_lat.t[(t - 2) * 128:(t - 1) * 128, :]), in_=a[:], eng="pool")
            else:
                S.dma(out=xnext.v(xnext.t[rs_, :]), in_=a[:], eng="pool")


def pair_gather(S, src, dst, rpc):
    for r0 in range(0, T, rpc):
        r1 = min(T, r0 + rpc)
        S.I("pool", "collective_compute", "AllGather", ALU.bypass, replica_groups=[[0, 1], [2, 3], [4, 5], [6, 7]],
            ins=[src.t[r0:r1, :]], outs=[dst.t[2 * r0:2 * r1, :]], r=[src], w=[dst])


def gat_row(t, r, rpc):
    r0 = (t * 128 // rpc) * rpc
    rows_c = min(rpc, T - r0)
    return 2 * r0 + r * rows_c + (t * 128 - r0)


import math

DEPTH = 2
_CONSTS = None
_PROGS = {}


def host_consts():
    global _CONSTS
    if _CONSTS is None:
        _CONSTS = {'ident': np.eye(128, dtype=np.float32), **rope_tables(), **tri_consts(), **dn_consts()}
    return _CONSTS


A_PRM_SHAPES = {'w2pad0': [32, 128], 'w2pad1': [32, 128], 'gb0': [1, 128], 'gb1': [1, 128], 'gla_nw': [1, 128],
                'dn_convw': [5, 768], 'dn_alog': [1, 4], 'dn_dtb': [1, 4], 'dn_nw': [1, 128],
                'gqa_qn': [1, 128], 'gqa_kn': [1, 128], 'diff_lambda': [1, 256], 'diff_nw': [1, 128], 'lamc': [1, 2]}


def layer_params(inp, l, h):
    p = {}
    w2 = inp['gla_gate_w2'][l]
    gbias = inp['gla_gate_b'][l]
    for d in range(2):
        wp = np.zeros((32, 128), np.float32)
        wp[d * 16:(d + 1) * 16] = w2[d][:, 128 * h:128 * h + 128]
        p[f'w2pad{d}'] = wp
        p[f'gb{d}'] = np.ascontiguousarray(gbias[d][None, 128 * h:128 * h + 128])
    p['gla_nw'] = np.ascontiguousarray(inp['gla_norm_w'][l][None])
    cw = inp['dn_conv_w'][l]
    p['dn_convw'] = np.ascontiguousarray(np.concatenate(
        [cw[:, 256 * h:256 * h + 256], cw[:, 512 + 256 * h:512 + 256 * h + 256], cw[:, 1024 + 256 * h:1024 + 256 * h + 256]], 1))
    p['dn_alog'] = np.ascontiguousarray(inp['dn_a_log'][l][:, 2 * h:2 * h + 2].reshape(1, 4))
    p['dn_dtb'] = np.ascontiguousarray(inp['dn_dt_bias'][l][:, 2 * h:2 * h + 2].reshape(1, 4))
    p['dn_nw'] = np.ascontiguousarray(inp['dn_norm_w'][l][None])
    p['gqa_qn'] = np.ascontiguousarray(inp['gqa_q_norm'][l][None])
    p['gqa_kn'] = np.ascontiguousarray(inp['gqa_k_norm'][l][None])
    p['diff_lambda'] = np.ascontiguousarray(inp['diff_lambda'][l].reshape(1, 256))
    p['diff_nw'] = np.ascontiguousarray(inp['diff_norm_w'][l][None])
    lam_init = 0.8 - 0.6 * math.exp(-0.3 * l)
    p['lamc'] = np.array([[1.0 - lam_init, -lam_init]], np.float32)
    return p


def prog_F():
    if 'F' in _PROGS:
        return _PROGS['F']
    nc = bass.Bass("TRN2", target_bir_lowering=False)
    C = host_consts()
    with ExitStack() as st:
        S = Sched(nc, st)
        xin0 = S.dram("xin", [T, 2048], kind="ExternalInput")
        c2 = S.dram("c2", [2, 2048], kind="ExternalInput")
        consts = {k: S.dram(k, list(v.shape), kind="ExternalInput") for k, v in C.items()}
        fnw = S.dram("fnw", [1, 2048], kind="ExternalInput")
        yout = S.dram("yout", [4096, 2048], kind="ExternalOutput")
        z = S.dram("z", [T, ZC])
        dnp = S.dram("dnp", [T, 776])
        xcur = xin0
        for l in range(DEPTH):
            last = l == DEPTH - 1
            E = lambda nm, shp, dt=F32: S.dram(f"{nm}_{l}", shp, dt, kind="ExternalInput")
            mod_w = E("mod_w", [2048, 12288])
            mod_b = E("mod_b", [1, 12288])
            nw = E("nw", [1, 2048])
            w_in = E("w_in", [2048, ZC])
            prm = {k: E(k, shp) for k, shp in A_PRM_SHAPES.items()}
            w_out = E("w_out", [2048, 2048])
            nw2 = E("nw2", [1, 2048])
            router_w = E("router_w", [2048, 16])
            wg = E("wg", [8, 2048, 1024])
            wu = E("wu", [8, 2048, 1024])
            wd = E("wd", [8, 1024, 2048])
            I_ = lambda nm, shp, dt=F32: S.dram(f"{nm}_{l}", shp, dt)
            modv = I_("modv", [2, 12288])
            ocat = I_("ocat", [T, 1024])
            ogat = I_("ogat", [2 * T, 1024])
            xmid = I_("xmid", [T, 2048])
            wm = I_("wm", [T, 16])
            h2T = I_("h2T", [128, 16, T], BF16)
            ypart = I_("ypart", [T, 2048])
            ygat = I_("ygat", [2 * T, 2048])
            xnext = None if last else I_("xnext", [T, 2048])
            phase_mod(S, c2, mod_w, mod_b, modv)
            phase_in(S, xcur, modv, nw, w_in, z, consts['ident'])
            phase_gla(S, z, consts, prm, ocat)
            phase_dn_prep(S, z, prm, dnp)
            phase_dn(S, z, dnp, consts, prm, ocat)
            phase_attn(S, z, consts, prm, ocat)
            pair_gather(S, ocat, ogat, 512)
            phase_c1(S, ogat, xcur, modv, w_out, xmid, consts['ident'], ctx_tiles=(0, 1), gathered=True)
            phase_c2(S, xmid, modv, nw2, router_w, h2T, wm, consts, ctx_tiles=(0, 1), nown=NT, natural=True)
            phase_moe_part(S, h2T, wm, wg, wu, wd, ypart)
            pair_gather(S, ypart, ygat, 256)
            phase_fin(S, ygat, xmid, modv, xnext, final_nw=fnw if last else None, out_lat=yout if last else None)
            xcur = xnext
        outs = [o for o in S.all_ops if o.is_dma and o.eng == "pool" and o.fn[0] == "dma_start"]
        S.emit(final_waits=outs[-40:])
        _PROGS['F_info'] = (S.nsem, {e: len(v) for e, v in S.ops.items()})
    _PROGS['F'] = nc
    return nc


def kernel(**inp):
    inp = {k: np.asarray(v) for k, v in inp.items()}
    C = host_consts()
    B = 4
    cores = [(b, h) for b in range(B) for h in range(2)]
    nc = prog_F()
    perm_o = np.array([g * 512 + h * 256 + j for h in range(2) for g in range(4) for j in range(256)])
    shared = {}
    per_h = [dict(), dict()]
    for l in range(DEPTH):
        shared[f"mod_w_{l}"] = np.ascontiguousarray(inp['mod_w'][l])
        shared[f"mod_b_{l}"] = np.ascontiguousarray(inp['mod_b'][l][None])
        shared[f"nw_{l}"] = np.ascontiguousarray(inp['norm1_w'][l][None])
        shared[f"w_out_{l}"] = np.ascontiguousarray(inp['w_out'][l][perm_o])
        shared[f"nw2_{l}"] = np.ascontiguousarray(inp['norm2_w'][l][None])
        for h in range(2):
            d = per_h[h]
            d[f"w_in_{l}"] = np.ascontiguousarray(inp['w_in'][l][:, wcols(h)])
            for k, v in layer_params(inp, l, h).items():
                d[f"{k}_{l}"] = v
            ecols = np.concatenate([np.arange(8 * h, 8 * h + 8), np.arange(8 * (1 - h), 8 * (1 - h) + 8)])
            d[f"router_w_{l}"] = np.ascontiguousarray(inp['router_w'][l][:, ecols])
            d[f"wg_{l}"] = np.ascontiguousarray(inp['exp_w_gate'][l][8 * h:8 * h + 8])
            d[f"wu_{l}"] = np.ascontiguousarray(inp['exp_w_up'][l][8 * h:8 * h + 8])
            d[f"wd_{l}"] = np.ascontiguousarray(inp['exp_w_down'][l][8 * h:8 * h + 8])
    fnw = np.ascontiguousarray(inp['final_norm_w'][None])
    in_maps = []
    for (b, h) in cores:
        m = {"xin": np.concatenate([inp['ctx'][b], inp['x'][b]], 0),
             "c2": np.ascontiguousarray(np.stack([inp['c'][b], inp['c_ctx']])), "fnw": fnw, **C, **shared, **per_h[h]}
        in_maps.append(m)
    res = run_bass_kernel_spmd(nc, in_maps, core_ids=list(range(8)))
    return np.stack([np.asarray(res.results[2 * b]["yout"]) for b in range(B)], 0).astype(np.float32)
```

```python
import numpy as np
import concourse.bass as bass
import concourse.mybir as mybir
from concourse.bass_utils import run_bass_kernel_spmd

F32 = mybir.dt.float32
BF16 = mybir.dt.bfloat16
ALU = mybir.AluOpType
AF = mybir.ActivationFunctionType
AX = mybir.AxisListType

SEM_LIMIT = 24000


class Buf:
    def __init__(self, t, name):
        self.t = t
        self.name = name
        self.last_w = None
        self.readers = []
        self.dma_sem = None
        self.is_dram = False
        self.is_psum = False
        self.slot = None

    def __getitem__(self, idx):
        return View(self, self.t[idx])

    def sub(self, key):
        return Buf(self.t, f"{self.name}.{key}")

    def v(self, ap):
        return View(self, ap)


class View:
    def __init__(self, buf, ap):
        self.buf = buf
        self.ap = ap


class Op:
    __slots__ = ("eng", "fn", "waits", "inc", "idx", "is_dma", "dma_buf", "dma_val")

    def __init__(self, eng, fn):
        self.eng = eng
        self.fn = fn
        self.waits = []
        self.inc = False
        self.idx = None
        self.is_dma = False
        self.dma_buf = None
        self.dma_val = None


ENGS = ("pe", "act", "dve", "pool", "sp")


class _Scope:
    def __init__(self, S):
        self.S = S

    def __enter__(self):
        from contextlib import ExitStack
        self.old = self.S.stack
        self.S.scope_bufs.append([])
        self.st = ExitStack()
        self.st.__enter__()
        self.S.stack = self.st
        return self

    def __exit__(self, *a):
        self.S.barrier()
        for b in self.S.scope_bufs.pop():
            if b.slot is not None:
                for e_, sl in b.slot.items():
                    self.S.free_slots_e.setdefault(e_, []).append(sl)
                b.slot = None
        self.S.stack = self.old
        return self.st.__exit__(*a)


class Sched:
    def __init__(self, nc, stack):
        self.nc = nc
        self.stack = stack
        self.ops = {e: [] for e in ENGS}
        self.all_ops = []
        self.nbuf = 0
        self.dma_counts = {}
        self._bar_pos = 0
        self._bar = {}
        self.free_slots = []
        self.free_slots_e = {}
        self.nslots = 0
        self.scope_bufs = [[]]

    def sbuf(self, shape, dt=F32, name=None):
        self.nbuf += 1
        name = f"{name}_{self.nbuf}" if name else f"sb{self.nbuf}"
        t = self.stack.enter_context(self.nc.sbuf_tensor(name, list(shape), dt))
        b = Buf(t, name)
        self.scope_bufs[-1].append(b)
        return b

    def psum(self, shape, dt=F32, name=None):
        self.nbuf += 1
        name = name or f"ps{self.nbuf}"
        t = self.stack.enter_context(self.nc.psum_tensor(name, list(shape), dt))
        b = Buf(t, name)
        b.is_psum = True
        return b

    def scope(self):
        return _Scope(self)

    def dram(self, name, shape, dt=F32, kind="Internal"):
        t = self.nc.dram_tensor(name, list(shape), dt, kind=kind)
        b = Buf(t.ap(), name)
        b.is_dram = True
        return b

    def barrier(self):
        lasts = []
        for e in ENGS:
            if e in ("sp", "pool"):
                continue
            if self.ops[e]:
                lasts.append(self.ops[e][-1])
        dmas = [o for o in self.all_ops[self._bar_pos:] if o.is_dma]
        self._bar_pos = len(self.all_ops)
        lastd = {}
        for o in dmas:
            lastd[o.dma_val] = o
        for e in ("sp", "pool"):
            nd = [o for o in self.ops[e] if not o.is_dma]
            if nd:
                lasts.append(nd[-1])
        self._bar = {e: lasts + list(lastd.values()) for e in ENGS}

    def _dep(self, op, reads, writes):
        for b in reads:
            w = b.last_w
            if w is not None:
                if not (w.eng == "pe" and op.eng == "pe"):
                    op.waits.append(w)
            if b.is_psum:
                for r in b.readers:
                    if r.eng != op.eng:
                        op.waits.append(r)
            b.readers.append(op)
        for b in writes:
            w = b.last_w
            if w is not None and not (w.is_dma and op.is_dma and w.eng == op.eng) \
                    and not (w.eng == "pe" and op.eng == "pe"):
                op.waits.append(w)
            for r in b.readers:
                if r is op:
                    continue
                if not (r.eng == "pe" and op.eng == "pe"):
                    op.waits.append(r)
            b.last_w = op
            b.readers = []

    def I(self, eng, meth, *args, r=(), w=(), **kw):
        reads = list(r)
        writes = list(w)
        kw2 = {}
        for k, val in kw.items():
            if isinstance(val, View):
                if k in ("out", "accum_out"):
                    writes.append(val.buf)
                else:
                    reads.append(val.buf)
                kw2[k] = val.ap
            else:
                kw2[k] = val
        is_dma = meth in ("dma_start", "collective_compute")
        op = Op(eng, None)
        op.is_dma = is_dma
        if is_dma:
            sb = None
            if meth == "collective_compute":
                sb = writes[0]
            for k in ("in_", "out"):
                if isinstance(kw.get(k), View):
                    if sb is None or not kw[k].buf.is_dram:
                        sb = kw[k].buf
            if sb.slot is None:
                sb.slot = {}
            if eng not in sb.slot:
                fl = self.free_slots_e.setdefault(eng, [])
                if fl:
                    sb.slot[eng] = fl.pop()
                else:
                    sb.slot[eng] = self.nslots
                    self.nslots += 1
            op.dma_buf = sb
            op.dma_val = sb.slot[eng]
        self._dep(op, reads, writes)
        if self._bar.get(eng):
            op.waits.extend(o for o in self._bar.pop(eng) if o is not op)
        op.fn = (meth, args, kw2)
        self.ops[eng].append(op)
        self.all_ops.append(op)
        return op

    def pe(self, meth, **kw):
        return self.I("pe", meth, **kw)

    def act(self, meth, **kw):
        return self.I("act", meth, **kw)

    def dve(self, meth, **kw):
        return self.I("dve", meth, **kw)

    def pool(self, meth, **kw):
        return self.I("pool", meth, **kw)

    def dma(self, out, in_, eng="sp", **kw):
        return self.I(eng, "dma_start", out=out, in_=in_, **kw)

    def emit(self, final_waits=()):
        nc = self.nc
        for op in self.all_ops:
            for wop in op.waits:
                wop.inc = True
        for op in final_waits:
            op.inc = True
        for op in self.all_ops:
            if op.is_dma:
                op.inc = True
        eng_cnt = {e: 0 for e in ENGS}
        slot_state = {}
        for op in self.all_ops:
            if not op.inc:
                continue
            if op.is_dma:
                b = op.dma_val
                stt = slot_state.setdefault(b, [0, 0])
                iv = 1 if op.fn[0] == "collective_compute" else 16
                if stt[1] + iv > SEM_LIMIT:
                    stt[0] += 1
                    stt[1] = 0
                stt[1] += iv
                op.idx = (stt[0], stt[1], iv)
            else:
                eng_cnt[op.eng] += 1
                op.idx = eng_cnt[op.eng]
        self.eng_sems = {}
        for e in ENGS:
            n = (eng_cnt[e] + SEM_LIMIT - 1) // SEM_LIMIT
            self.eng_sems[e] = [
                self.stack.enter_context(nc.semaphore(f"s_{e}_{i}")) for i in range(n)
            ]
        self.dma_sems = {}
        for b, stt in slot_state.items():
            self.dma_sems[b] = [
                self.stack.enter_context(nc.semaphore(f"d_slot{b}_{i}"))
                for i in range(stt[0] + 1)
            ]
        nsem = sum(len(v) for v in self.eng_sems.values()) + sum(
            len(v) for v in self.dma_sems.values())
        self.nsem = nsem

        def sem_of(op):
            if op.is_dma:
                k, val, iv = op.idx
                return self.dma_sems[op.dma_val][k], val
            k = (op.idx - 1) // SEM_LIMIT
            return self.eng_sems[op.eng][k], op.idx - k * SEM_LIMIT

        block = self.stack.enter_context(nc.Block())
        engmap = {"pe": block.tensor, "act": block.scalar, "dve": block.vector,
                  "pool": block.gpsimd, "sp": block.sync}

        def make(ename):
            ops = self.ops[ename]
            fw = [o for o in final_waits]

            def body(eng):
                known = {}
                for op in ops:
                    need = {}
                    for wop in op.waits:
                        sem, val = sem_of(wop)
                        key = id(sem)
                        if key not in need or need[key][1] < val:
                            need[key] = (sem, val)
                    for key, (sem, val) in need.items():
                        if known.get(key, 0) >= val:
                            continue
                        known[key] = val
                        eng.wait_ge(sem, val)
                    meth, args, kw = op.fn
                    ins = getattr(eng, meth)(*args, **kw)
                    if op.inc:
                        sem, val = sem_of(op)
                        ins.then_inc(sem, op.idx[2] if op.is_dma else 1)
                if ename == "sp":
                    need = {}
                    for wop in fw:
                        sem, val = sem_of(wop)
                        key = id(sem)
                        if key not in need or need[key][1] < val:
                            need[key] = (sem, val)
                    for key, (sem, val) in need.items():
                        eng.wait_ge(sem, val)
            return body

        for e in ENGS:
            if self.ops[e] or (e == "sp" and final_waits):
                engmap[e](make(e))


from contextlib import ExitStack

T = 4352
NT = 34
D = 2048
KC = 16
NCTX = 256
FAM = [('gla_q', 128), ('gla_k', 128), ('gla_v', 256), ('gla_g', 256), ('gla_lr', 32),
       ('dn_q', 256), ('dn_k', 256), ('dn_v', 256), ('dn_g', 256), ('dn_a', 4), ('dn_b', 4),
       ('gqa_q', 256), ('gqa_k', 128), ('gqa_v', 128),
       ('diff_q', 256), ('diff_k', 256), ('diff_v', 256)]
ZOFF = {}
_o = 0
for _n, _w in FAM:
    ZOFF[_n] = (_o, _w)
    _o += _w
ZC = _o


def wcols(h):
    r = np.arange
    c = []
    c += list(r(0 + 128 * h, 0 + 128 * h + 128))
    c += list(r(256 + 128 * h, 256 + 128 * h + 128))
    c += list(r(512 + 256 * h, 512 + 256 * h + 256))
    c += list(r(1024 + 256 * h, 1024 + 256 * h + 256))
    c += list(r(1536, 1568))
    c += list(r(1568 + 256 * h, 1568 + 256 * h + 256))
    c += list(r(1568 + 512 + 256 * h, 1568 + 512 + 256 * h + 256))
    c += list(r(1568 + 1024 + 256 * h, 1568 + 1024 + 256 * h + 256))
    c += list(r(3104 + 256 * h, 3104 + 256 * h + 256))
    c += [3616 + d * 4 + 2 * h + j for d in range(2) for j in range(2)]
    c += [3624 + d * 4 + 2 * h + j for d in range(2) for j in range(2)]
    c += list(r(3632 + 256 * h, 3632 + 256 * h + 256))
    c += list(r(4144 + 128 * h, 4144 + 128 * h + 128))
    c += list(r(4144 + 256 + 128 * h, 4144 + 256 + 128 * h + 128))
    c += list(r(4656 + 256 * h, 4656 + 256 * h + 256))
    c += list(r(5168 + 256 * h, 5168 + 256 * h + 256))
    c += list(r(5680 + 256 * h, 5680 + 256 * h + 256))
    assert len(c) == ZC
    return np.array(c)


def bc(ap, n=128):
    return ap.partition_broadcast(n)


def phase_mod(S, c2, mod_w, mod_b, modv):
    with S.scope():
        cT = S.sbuf([128, 16, 2])
        for r in range(2):
            S.dma(out=cT[:, :, r:r + 1], in_=c2.v(c2.t[r:r + 1, :].rearrange("r (k p) -> p k r", p=128)),
                  allow_slow_non_contiguous=True)
        sT = S.sbuf([128, 16, 2])
        S.act("activation", out=sT[:], in_=cT[:], func=AF.Silu)
        mb = S.sbuf([2, 12288])
        S.dma(out=mb[:], in_=mod_b.v(bc(mod_b.t[0:1, :], 2)))
        mv = S.sbuf([2, 12288])
        wr = [S.sbuf([128, 16, 512]) for _ in range(2)]
        ps = [S.psum([2, 512]) for _ in range(2)]
        mwv = mod_w.t.rearrange("(k p) n -> p k n", p=128)
        for ct in range(24):
            wb = wr[ct % 2]
            cs = slice(ct * 512, (ct + 1) * 512)
            S.dma(out=wb[:, 0:8, :], in_=mod_w.v(mwv[:, 0:8, cs]))
            S.dma(out=wb[:, 8:16, :], in_=mod_w.v(mwv[:, 8:16, cs]))
            for k in range(16):
                S.pe("matmul", out=ps[ct % 2][:], lhsT=sT[:, k, :], rhs=wb[:, k, :],
                     start=(k == 0), stop=(k == 15))
            S.dve("tensor_tensor", out=mv[:, cs], in0=ps[ct % 2][:], in1=mb[:, cs], op=ALU.add)
        S.dma(out=modv.v(modv.t), in_=mv[:], eng="pool")


def load_w_bf16(S, Wb, w_dram, stages, nk, ncols):
    for k in range(nk):
        st = stages[k % len(stages)]
        S.dma(out=st[:, :ncols], in_=w_dram.v(w_dram.t[k * 128:(k + 1) * 128, :]))
        S.I("dve" if k % 2 == 0 else "pool", "tensor_copy", out=Wb[:, k, :], in_=st[:, :ncols])


def rsqrt_col(S, out, in_, scale, eps):
    S.dve("tensor_scalar", out=out[:], in0=in_[:], scalar1=scale, scalar2=eps,
          op0=ALU.mult, op1=ALU.add)
    S.act("activation", out=out[:], in_=out[:], func=AF.Sqrt)
    S.dve("reciprocal", out=out[:], in_=out[:])


def rms_mod(S, x, A, Bv, hb, junk, ssq, rstd, width=2048):
    S.act("activation", out=junk[:], in_=x[:], func=AF.Square, accum_out=ssq[:])
    rsqrt_col(S, rstd, ssq, 1.0 / width, 1e-6)
    S.dve("scalar_tensor_tensor", out=hb[:], in0=x[:], scalar=rstd[:], in1=A[:],
          op0=ALU.mult, op1=ALU.mult)
    S.pool("tensor_tensor", out=hb[:], in0=hb[:], in1=Bv[:], op=ALU.add)


def transpose_tile(S, src, dstT, idt, pst, nk=16, base=0):
    ng = (nk + 3) // 4
    for g in range(ng):
        p = pst[(base + g) % len(pst)]
        n = min(4, nk - g * 4)
        for j in range(n):
            k = g * 4 + j
            S.pe("transpose", out=p[:, j * 128:(j + 1) * 128], in_=src[:, k * 128:(k + 1) * 128],
                 identity=idt[:])
        eng = "act" if g % 2 else "dve"
        dv = dstT.v(dstT.t[:, g * 4:g * 4 + n, :].rearrange("p a b -> p (a b)"))
        if eng == "act":
            S.act("activation", out=dv, in_=p[:, :n * 128], func=AF.Copy)
        else:
            S.dve("tensor_copy", out=dv, in_=p[:, :n * 128])


def phase_in(S, xin, modv, nw, w_in, z, ident):
    with S.scope():
        idt = S.sbuf([128, 128])
        S.dma(out=idt[:], in_=ident.v(ident.t))
        Wb = S.sbuf([128, 16, ZC], BF16)
        zst = [S.sbuf([128, ZC]) for _ in range(2)]
        load_w_bf16(S, Wb, w_in, zst, 16, ZC)
        nwb = S.sbuf([128, 2048])
        S.dma(out=nwb[:], in_=nw.v(bc(nw.t[0:1, :])))
        A = S.sbuf([128, 2048])
        Bv = S.sbuf([128, 2048])
        xr = [S.sbuf([128, 2048]) for _ in range(2)]
        hb = S.sbuf([128, 2048])
        junk = S.sbuf([128, 2048], BF16)
        hT = [S.sbuf([128, 16, 128], BF16) for _ in range(2)]
        ssq = S.sbuf([128, 1])
        rstd = S.sbuf([128, 1])
        pst = [S.psum([128, 512]) for _ in range(2)]
        psz = [S.psum([128, 512]) for _ in range(3)]
        for t in range(NT):
            if t == 0 or t == 2:
                r = 1 if t == 0 else 0
                S.dma(out=A[:], in_=modv.v(bc(modv.t[r:r + 1, 2048:4096])))
                S.dma(out=Bv[:], in_=modv.v(bc(modv.t[r:r + 1, 0:2048])))
                S.dve("scalar_tensor_tensor", out=A[:], in0=A[:], scalar=1.0, in1=nwb[:],
                      op0=ALU.add, op1=ALU.mult)
            x = xr[t % 2]
            S.dma(out=x[:], in_=xin.v(xin.t[t * 128:(t + 1) * 128, :]))
            rms_mod(S, x, A, Bv, hb, junk, ssq, rstd)
            transpose_tile(S, hb, hT[t % 2], idt, pst)
            zs = zst[t % 2]
            for ct in range((ZC + 511) // 512):
                w = min(512, ZC - ct * 512)
                p = psz[ct % 3]
                for k in range(16):
                    S.pe("matmul", out=p[:, :w], lhsT=hT[t % 2][:, k, :],
                         rhs=Wb[:, k, ct * 512:ct * 512 + w], start=(k == 0), stop=(k == 15))
                if ct % 2:
                    S.act("activation", out=zs[:, ct * 512:ct * 512 + w], in_=p[:, :w], func=AF.Copy)
                else:
                    S.dve("tensor_copy", out=zs[:, ct * 512:ct * 512 + w], in_=p[:, :w])
            S.dma(out=z.v(z.t[t * 128:(t + 1) * 128, :]), in_=zs[:], eng="pool")


def rope_tables():
    out = {}
    t = np.arange(4096)
    pos = {0: (t // 64).astype(np.float64), 1: (t % 64).astype(np.float64)}
    for d in (128, 64):
        half = d // 2
        nf = half // 2
        inv = 10000.0 ** (-np.arange(0, half, 2, dtype=np.float64) / half)
        C = np.ones((T, d), np.float32)
        Sg = np.zeros((T, d), np.float32)
        for ax in range(2):
            ang = (pos[ax][:, None].astype(np.float32) * inv[None, :].astype(np.float32)).astype(np.float32)
            cs, sn = np.cos(ang), np.sin(ang)
            o = ax * half
            C[NCTX:, o:o + nf] = cs
            C[NCTX:, o + nf:o + 2 * nf] = cs
            Sg[NCTX:, o:o + nf] = -sn
            Sg[NCTX:, o + nf:o + 2 * nf] = sn
        out[f'ropeC{d}'] = C
        out[f'ropeS{d}'] = Sg
    return out


def rope_apply(S, x, xo, t1, Ct, St, nvec, d, c0):
    nf = d // 4
    cs = slice(c0, c0 + nvec * d)
    v3 = lambda b: b.v(b.t[:, cs].rearrange("p (v d) -> p v d", d=d))
    cb = Ct.v(Ct.t[:, :].unsqueeze(1).to_broadcast([128, nvec, d]))
    S.dve("tensor_tensor", out=v3(t1), in0=v3(x), in1=cb, op=ALU.mult)
    v4 = lambda b: b.t[:, cs].rearrange("p (v a h f) -> p v a h f", a=2, h=2, f=nf)
    s4 = St.t[:, :].rearrange("p (a h f) -> p a h f", a=2, h=2)
    for hf in range(2):
        sb = St.v(s4[:, :, hf, :].unsqueeze(1).to_broadcast([128, nvec, 2, nf]))
        S.dve("tensor_tensor", out=xo.v(v4(xo)[:, :, :, hf, :]), in0=x.v(v4(x)[:, :, :, 1 - hf, :]),
               in1=sb, op=ALU.mult)
    S.dve("tensor_tensor", out=xo[:, cs], in0=xo[:, cs], in1=t1[:, cs], op=ALU.add)


DEBUG = {}


def phase_attn(S, z, consts, prm, ocat, need_ctx=True):
    gq0 = ZOFF['gqa_q'][0]
    df0 = ZOFF['diff_q'][0]
    with S.scope():
        idt = S.sbuf([128, 128])
        S.dma(out=idt[:], in_=consts['ident'].v(consts['ident'].t))
        ones = S.sbuf([128, 128], BF16)
        S.dve("memset", out=ones[:], constant=1.0) if False else S.I("dve", "memset", ones.t[:], 1.0, w=[ones])
        wn = S.sbuf([128, 3, 128])
        S.dma(out=wn[:, 0, :], in_=prm['gqa_qn'].v(bc(prm['gqa_qn'].t[0:1, :])))
        S.dma(out=wn[:, 1, :], in_=prm['gqa_qn'].v(bc(prm['gqa_qn'].t[0:1, :])))
        S.dma(out=wn[:, 2, :], in_=prm['gqa_kn'].v(bc(prm['gqa_kn'].t[0:1, :])))
        dnw = S.sbuf([128, 128])
        S.dma(out=dnw[:], in_=prm['diff_nw'].v(bc(prm['diff_nw'].t[0:1, :])))
        lamc = S.sbuf([128, 2])
        S.dma(out=lamc[:], in_=prm['lamc'].v(bc(prm['lamc'].t[0:1, :])))
        S.dve("tensor_scalar", out=dnw[:], in0=dnw[:], scalar1=lamc[:, 0:1], scalar2=None, op0=ALU.mult)
        lamt = S.sbuf([128, 4, 64])
        S.dma(out=lamt.v(lamt.t[:, :, :].rearrange("p a b -> p (a b)")),
              in_=prm['diff_lambda'].v(bc(prm['diff_lambda'].t[0:1, :])))
        lj = S.sbuf([128, 2, 64])
        lam2 = S.sbuf([128, 2])
        S.dve("tensor_tensor", out=lj[:, 0, :], in0=lamt[:, 0, :], in1=lamt[:, 1, :], op=ALU.mult)
        S.dve("tensor_tensor", out=lj[:, 1, :], in0=lamt[:, 2, :], in1=lamt[:, 3, :], op=ALU.mult)
        S.dve("tensor_reduce", out=lam2[:], in_=lj[:], axis=AX.X, op=ALU.add)
        S.act("activation", out=lam2[:], in_=lam2[:], func=AF.Exp)
        nlam = S.sbuf([128, 1])
        S.dve("tensor_tensor", out=nlam[:], in0=lam2[:, 1:2], in1=lam2[:, 0:1], op=ALU.subtract)
        S.dve("tensor_scalar", out=nlam[:], in0=nlam[:], scalar1=lamc[:, 1:2], scalar2=None, op0=ALU.add)

        if DEBUG.get('setup_only'):
            S.dma(out=ocat.v(ocat.t[0:128, 0:384]), in_=wn.v(wn.t[:, :, :].rearrange("p a b -> p (a b)")), eng="pool")
            S.dma(out=ocat.v(ocat.t[0:128, 384:386]), in_=lam2[:, 0:2], eng="pool")
            S.dma(out=ocat.v(ocat.t[0:128, 512:640]), in_=dnw[:], eng="pool")
            return
        gqT = [S.sbuf([128, T], BF16) for _ in range(2)]
        gkT = S.sbuf([128, T], BF16)
        gv = S.sbuf([128, NT, 128], BF16)
        dqT = [S.sbuf([128, T], BF16) for _ in range(2)]
        dkT = [S.sbuf([128, T], BF16) for _ in range(2)]
        dv = S.sbuf([128, NT, 256], BF16)

        pst = [S.psum([128, 512]) for _ in range(2)]
        pS = [S.psum([128, 512]) for _ in range(2)]
        pO = [S.psum([128, 512]) for _ in range(2)]
        pR = [S.psum([128, 512]) for _ in range(2)]

        with S.scope():
            zin = [S.sbuf([128, 512 + 768]) for _ in range(2)]
            xo = [S.sbuf([128, 512 + 768]) for _ in range(2)]
            t1 = S.sbuf([128, 512 + 768])
            C128 = [S.sbuf([128, 128]) for _ in range(2)]
            S128 = [S.sbuf([128, 128]) for _ in range(2)]
            C64 = [S.sbuf([128, 64]) for _ in range(2)]
            S64 = [S.sbuf([128, 64]) for _ in range(2)]
            sq = S.sbuf([128, 384])
            ss = S.sbuf([128, 3])
            for t in range(DEBUG.get('ntl', NT)):
                rs_ = slice(t * 128, (t + 1) * 128)
                zi = zin[t % 2]
                x2 = xo[t % 2]
                S.dma(out=zi[:, 0:512], in_=z.v(z.t[rs_, gq0:gq0 + 512]))
                S.dma(out=zi[:, 512:1280], in_=z.v(z.t[rs_, df0:df0 + 768]))
                for nm, tl in (('ropeC128', C128), ('ropeS128', S128), ('ropeC64', C64), ('ropeS64', S64)):
                    S.dma(out=tl[t % 2][:], in_=consts[nm].v(consts[nm].t[rs_, :]))
                stop = DEBUG.get('stop', 99)
                if stop == 1:
                    S.dma(out=ocat.v(ocat.t[rs_, 0:1024]), in_=zi[:, 0:1024], eng="pool")
                    S.dma(out=ocat.v(ocat.t[rs_, 0:128]), in_=C128[t % 2][:], eng="pool")
                    S.dma(out=ocat.v(ocat.t[rs_, 128:256]), in_=S128[t % 2][:], eng="pool")
                    S.dma(out=ocat.v(ocat.t[rs_, 256:320]), in_=C64[t % 2][:], eng="pool")
                    S.dma(out=ocat.v(ocat.t[rs_, 320:384]), in_=S64[t % 2][:], eng="pool")
                    continue
                if DEBUG.get('skip_norm'):
                    pass
                else:
                  S.dve("tensor_tensor", out=sq[:], in0=zi[:, 0:384], in1=zi[:, 0:384], op=ALU.mult)
                  S.dve("tensor_reduce", out=ss[:], in_=sq.v(sq.t[:, :].rearrange("p (v d) -> p v d", d=128)),
                      axis=AX.X, op=ALU.add)
                  rsqrt_col(S, ss, ss, 1.0 / 128, 1e-6)
                  z3 = zi.v(zi.t[:, 0:384].rearrange("p (v d) -> p v d", d=128))
                  if not DEBUG.get('skip_bc'):
                    S.dve("tensor_tensor", out=z3, in0=z3,
                      in1=ss.v(ss.t[:, :].unsqueeze(2).to_broadcast([128, 3, 128])), op=ALU.mult)
                  S.dve("tensor_tensor", out=z3, in0=z3, in1=wn[:, :, :], op=ALU.mult)
                if DEBUG.get('skip_rope'):
                    S.dve("tensor_copy", out=x2[:, 0:1024], in_=zi[:, 0:1024])
                else:
                    rope_apply(S, zi, x2, t1, C128[t % 2], S128[t % 2], 3, 128, 0)
                    rope_apply(S, zi, x2, t1, C64[t % 2], S64[t % 2], 8, 64, 512)
                if stop == 3:
                    S.dma(out=ocat.v(ocat.t[rs_, 0:384]), in_=x2[:, 0:384], eng="pool")
                    S.dma(out=ocat.v(ocat.t[rs_, 512:1024]), in_=x2[:, 512:1024], eng="pool")
                    continue
                em = DEBUG.get('em', 0)
                p = pst[0]
                for j in range(3):
                    S.pe("transpose", out=p[:, j * 128:(j + 1) * 128], in_=x2[:, j * 128:(j + 1) * 128], identity=idt[:])
                if em != 1:
                    S.act("activation", out=gqT[0][:, rs_], in_=p[:, 0:128], func=AF.Copy)
                    S.act("activation", out=gqT[1][:, rs_], in_=p[:, 128:256], func=AF.Copy)
                if em != 2:
                    S.act("activation", out=gkT[:, rs_], in_=p[:, 256:384], func=AF.Copy)
                p = pst[1]
                if em != 3:
                  for j in range(4):
                    S.pe("transpose", out=p[:, j * 128:(j + 1) * 128], in_=x2[:, 512 + j * 128:512 + (j + 1) * 128], identity=idt[:])
                  if em != 1:
                    S.dve("tensor_copy", out=dqT[0][:, rs_], in_=p[:, 0:128])
                    S.dve("tensor_copy", out=dqT[1][:, rs_], in_=p[:, 128:256])
                  if em != 2:
                    S.dve("tensor_copy", out=dkT[0][:, rs_], in_=p[:, 256:384])
                    S.dve("tensor_copy", out=dkT[1][:, rs_], in_=p[:, 384:512])
                if stop == 4:
                    S.dma(out=ocat.v(ocat.t[rs_, 0:384]), in_=x2[:, 0:384], eng="pool")
                    S.dma(out=ocat.v(ocat.t[rs_, 512:1024]), in_=x2[:, 512:1024], eng="pool")
                    continue
                S.pool("tensor_copy", out=gv[:, t, :], in_=zi[:, 384:512])
                S.pool("tensor_copy", out=dv[:, t, :], in_=zi[:, 1024:1280])
                if DEBUG.get('pre_only'):
                    S.dma(out=ocat.v(ocat.t[rs_, 0:384]), in_=x2[:, 0:384], eng="pool")
                    S.dma(out=ocat.v(ocat.t[rs_, 512:1024]), in_=x2[:, 512:1024], eng="pool")
        if DEBUG.get('pre_only'):
            return

        pT = [S.sbuf([128, 512], BF16) for _ in range(3)]
        oTs = [S.sbuf([128, 512]) for _ in range(2)]
        oT2 = [S.sbuf([128, 512]) for _ in range(2)]
        rinv = [S.sbuf([128, 512]) for _ in range(2)]
        otok = [S.sbuf([128, 4, 128]) for _ in range(2)]
        sq2 = S.sbuf([128, 4, 128])
        ss2 = S.sbuf([128, 4])
        S.I("dve", "memset", ss2.t[:], 1.0, w=[ss2])
        cnt = [0]

        def one_map(qT, kT, prow, vv, vcol, q0, nq, nkt, scale):
            i = cnt[0]
            cnt[0] += 1
            po, pr = pO[i % 2], pR[i % 2]

            def s_step(kt):
                ps = pS[kt % 2]
                S.pe("matmul", out=ps[:, :nq], lhsT=kT[prow, kt * 128:(kt + 1) * 128], rhs=qT[prow, q0:q0 + nq],
                     start=True, stop=True)
                pt = pT[kt % 3]
                S.act("activation", out=pt[:, :nq], in_=ps[:, :nq], func=AF.Exp, scale=scale)

            def pv_step(kt):
                pt = pT[kt % 3]
                S.pe("matmul", out=po[:, :nq], lhsT=vv[:, kt, vcol], rhs=pt[:, :nq], start=(kt == 0), stop=(kt == nkt - 1))
                S.pe("matmul", out=pr[:, :nq], lhsT=ones[:, :], rhs=pt[:, :nq], start=(kt == 0), stop=(kt == nkt - 1))

            s_step(0)
            for kt in range(nkt):
                if kt + 1 < nkt:
                    s_step(kt + 1)
                pv_step(kt)
            return po, pr

        blocks = ([(0, 256, 2)] if need_ctx else []) + [(NCTX + i * 512, 512, NT) for i in range(8)]
        bi = 0
        for (q0, nq, nkt) in blocks:
            for hd in range(2):
                po, pr = one_map(gqT[hd], gkT, slice(0, 128), gv, slice(0, 128), q0, nq, nkt, 128 ** -0.5)
                ri = rinv[bi % 2]
                ot = oTs[bi % 2]
                S.dve("reciprocal", out=ri[:, :nq], in_=pr[:, :nq])
                S.dve("tensor_tensor", out=ot[:, :nq], in0=po[:, :nq], in1=ri[:, :nq], op=ALU.mult)
                ok = otok[bi % 2]
                p = pst[bi % 2]
                for j in range(nq // 128):
                    S.pe("transpose", out=p[:, j * 128:(j + 1) * 128], in_=ot[:, j * 128:(j + 1) * 128], identity=idt[:])
                S.act("activation", out=ok.v(ok.t[:, 0:nq // 128, :].rearrange("p a b -> p (a b)")), in_=p[:, :nq], func=AF.Copy)
                S.dma(out=ocat.v(ocat.t[q0:q0 + nq, 512 + hd * 128:512 + (hd + 1) * 128].rearrange("(a p) d -> p a d", p=128)),
                      in_=ok[:, 0:nq // 128, :], eng="pool")
                bi += 1
            for hd in range(2):
                po, pr = one_map(dqT[hd], dkT[hd], slice(0, 64), dv, slice(hd * 128, (hd + 1) * 128), q0, nq, nkt, 64 ** -0.5)
                ri = rinv[bi % 2]
                ot = oTs[bi % 2]
                S.dve("reciprocal", out=ri[:, :nq], in_=pr[:, :nq])
                S.dve("tensor_tensor", out=ot[:, :nq], in0=po[:, :nq], in1=ri[:, :nq], op=ALU.mult)
                po, pr = one_map(dqT[hd], dkT[hd], slice(64, 128), dv, slice(hd * 128, (hd + 1) * 128), q0, nq, nkt, 64 ** -0.5)
                o2 = oT2[bi % 2]
                S.dve("reciprocal", out=ri[:, :nq], in_=pr[:, :nq])
                S.dve("tensor_tensor", out=o2[:, :nq], in0=po[:, :nq], in1=ri[:, :nq], op=ALU.mult)
                S.dve("scalar_tensor_tensor", out=ot[:, :nq], in0=o2[:, :nq], scalar=nlam[:, 0:1], in1=ot[:, :nq],
                      op0=ALU.mult, op1=ALU.add)
                ok = otok[bi % 2]
                p = pst[bi % 2]
                na = nq // 128
                for j in range(na):
                    S.pe("transpose", out=p[:, j * 128:(j + 1) * 128], in_=ot[:, j * 128:(j + 1) * 128], identity=idt[:])
                okv = ok.v(ok.t[:, 0:na, :].rearrange("p a b -> p (a b)"))
                S.act("activation", out=okv, in_=p[:, :nq], func=AF.Copy)
                S.dve("tensor_tensor", out=sq2[:, 0:na, :], in0=ok[:, 0:na, :], in1=ok[:, 0:na, :], op=ALU.mult)
                S.dve("tensor_reduce", out=ss2[:, 0:na], in_=sq2[:, 0:na, :], axis=AX.X, op=ALU.add)
                rsqrt_col(S, ss2, ss2, 1.0 / 128, 1e-6)
                S.dve("tensor_tensor", out=ok[:, 0:na, :], in0=ok[:, 0:na, :],
                      in1=ss2.v(ss2.t[:, 0:na].unsqueeze(2).to_broadcast([128, na, 128])), op=ALU.mult)
                S.dve("tensor_tensor", out=ok[:, 0:na, :], in0=ok[:, 0:na, :],
                      in1=dnw.v(dnw.t[:, :].unsqueeze(1).to_broadcast([128, na, 128])), op=ALU.mult)
                S.dma(out=ocat.v(ocat.t[q0:q0 + nq, 768 + hd * 128:768 + (hd + 1) * 128].rearrange("(a p) d -> p a d", p=128)),
                      in_=ok[:, 0:na, :], eng="pool")
                bi += 1


def tri_consts():
    j = np.arange(128)[:, None]
    i = np.arange(128)[None, :]
    c = {}
    c['mincl_f'] = (j <= i).astype(np.float32)
    c['mincl_r'] = (j >= i).astype(np.float32)
    c['mstr_f'] = (j > i).astype(np.float32)
    c['mstr_r'] = (j < i).astype(np.float32)
    c['ones1'] = np.ones((1, 128), np.float32)
    hm = np.zeros((128, 2), np.float32); hm[:64, 0] = 1; hm[64:, 1] = 1
    c['hmask'] = hm
    c['ones128'] = np.ones((128, 128), np.float32)
    return c


def block_order(d):
    if d == 0:
        return list(range(NT))
    return [1, 0] + list(range(NT - 1, 1, -1))


def phase_gla(S, z, consts, prm, ocat):
    c0 = ZOFF['gla_q'][0]
    with S.scope():
        idt = S.sbuf([128, 128])
        S.dma(out=idt[:], in_=consts['ident'].v(consts['ident'].t))
        msk = {}
        for nm in ('mincl_f', 'mincl_r', 'mstr_f', 'mstr_r'):
            msk[nm] = S.sbuf([128, 128], name="gla_" + nm)
            S.dma(out=msk[nm][:], in_=consts[nm].v(consts[nm].t))
        mS = {}
        for nm in ('mincl_f', 'mincl_r', 'mstr_f', 'mstr_r'):
            mS[nm] = S.sbuf([128, 128], name="glaS_" + nm)
            S.dve("tensor_scalar", out=mS[nm][:], in0=msk[nm][:], scalar1=-1.0 / 16, scalar2=None, op0=ALU.mult)
        ones1 = S.sbuf([1, 128])
        S.dma(out=ones1[:], in_=consts['ones1'].v(consts['ones1'].t))
        w2p = [S.sbuf([32, 128]) for _ in range(2)]
        gb = [S.sbuf([1, 128]) for _ in range(2)]
        for d in range(2):
            S.dma(out=w2p[d][:], in_=prm[f'w2pad{d}'].v(prm[f'w2pad{d}'].t))
            S.dma(out=gb[d][:], in_=prm[f'gb{d}'].v(prm[f'gb{d}'].t))
        nwb = S.sbuf([128, 128])
        S.dma(out=nwb[:], in_=prm['gla_nw'].v(bc(prm['gla_nw'].t[0:1, :])))
        oacc = S.sbuf([128, NT, 256])
        Sst = S.sbuf([128, 128])
        R = 2
        zin = [S.sbuf([128, 800]) for _ in range(R)]
        lrT = [S.sbuf([32, 128]) for _ in range(R)]
        ee = [S.sbuf([128, 128]) for _ in range(R)]
        sp = [S.sbuf([128, 128]) for _ in range(R)]
        EbT = [S.sbuf([128, 128]) for _ in range(R)]
        EnbT = [S.sbuf([128, 128]) for _ in range(R)]
        Ebm = [S.sbuf([128, 128]) for _ in range(R)]
        qgT = [S.sbuf([128, 128]) for _ in range(R)]
        qgTh = [S.sbuf([128, 2, 128]) for _ in range(R)]
        kinvT = [S.sbuf([128, 2, 128]) for _ in range(R)]
        hm = S.sbuf([128, 2])
        S.dma(out=hm[:], in_=consts['hmask'].v(consts['hmask'].t))
        kd = [S.sbuf([128, 128]) for _ in range(R)]
        scm = [S.sbuf([128, 2, 128]) for _ in range(R)]
        osum = [S.sbuf([128, 2, 128]) for _ in range(R)]
        sg = [S.sbuf([128, 256]) for _ in range(R)]
        sq = S.sbuf([128, 2, 128])
        ss = S.sbuf([128, 2])
        pA, pB, pC, pD, pE, pF, pG, pH = [S.psum([128, 512]) for _ in range(8)]
        for d in range(2):
            sfx = '_f' if d == 0 else '_r'
            Mincl, Mstr, MinclS, MstrS = msk['mincl' + sfx], msk['mstr' + sfx], mS['mincl' + sfx], mS['mstr' + sfx]
            last = 127 if d == 0 else 0
            S.I("dve", "memset", Sst.t[:], 0.0, w=[Sst])
            for n, blk in enumerate(block_order(d)[:DEBUG.get('gnb', NT)]):
                i = n % R
                rs_ = slice(blk * 128, (blk + 1) * 128)
                zi = zin[i]
                S.dma(out=zi[:], in_=z.v(z.t[rs_, c0:c0 + 800]))
                q2, k2, v2, g2, lr = (zi[:, 0:128], zi[:, 128:256], zi[:, 256:512], zi[:, 512:768], zi[:, 768:800])
                S.pe("transpose", out=pA[0:32, 0:128], in_=lr, identity=idt[:])
                S.act("activation", out=lrT[i][:], in_=pA[0:32, 0:128], func=AF.Copy)
                if DEBUG.get('gstop') == 1:
                    continue
                S.pe("matmul", out=pB[:, 0:128], lhsT=lrT[i][:], rhs=w2p[d][:], start=True, stop=False)
                S.pe("matmul", out=pB[:, 0:128], lhsT=ones1[:], rhs=gb[d][:], start=False, stop=True)
                if DEBUG.get('gstop') == 2:
                    continue
                S.act("activation", out=ee[i][:], in_=pB[:, 0:128], func=AF.Exp, scale=-1.0)
                S.act("activation", out=sp[i][:], in_=ee[i][:], func=AF.Ln, bias=1.0)
                if DEBUG.get('gstop') == 3:
                    continue
                S.pe("matmul", out=pC[:, 0:128], lhsT=sp[i][:], rhs=MinclS[:], start=True, stop=True)
                S.pe("matmul", out=pD[:, 0:128], lhsT=MstrS[:], rhs=sp[i][:], start=True, stop=True)
                S.act("activation", out=EbT[i][:], in_=pC[:, 0:128], func=AF.Exp)
                S.act("activation", out=EnbT[i][:], in_=pC[:, 0:128], func=AF.Exp, scale=-1.0)
                S.act("activation", out=Ebm[i][:], in_=pD[:, 0:128], func=AF.Exp)
                if DEBUG.get('gstop') == 4:
                    continue
                S.pe("transpose", out=pE[:, 0:128], in_=q2, identity=idt[:])
                S.pe("transpose", out=pE[:, 128:256], in_=k2, identity=idt[:])
                S.dve("scalar_tensor_tensor", out=qgT[i][:], in0=pE[:, 0:128], scalar=0.125, in1=EbT[i][:],
                      op0=ALU.mult, op1=ALU.mult)
                for hh in range(2):
                    S.dve("scalar_tensor_tensor", out=kinvT[i][:, hh, :], in0=pE[:, 128:256], scalar=hm[:, hh:hh + 1],
                          in1=EnbT[i][:], op0=ALU.mult, op1=ALU.mult)
                    S.dve("tensor_scalar", out=qgTh[i][:, hh, :], in0=qgT[i][:], scalar1=hm[:, hh:hh + 1], scalar2=None,
                          op0=ALU.mult)
                S.dve("tensor_tensor", out=kd[i][:], in0=k2, in1=Ebm[i][:], op=ALU.mult)
                if DEBUG.get('gstop') == 6:
                    continue
                for hh in range(2):
                    r = slice(64 * hh, 64 * hh + 64)
                    S.pe("matmul", out=pF[:, hh * 128:(hh + 1) * 128], lhsT=kinvT[i][:, hh, :], rhs=qgT[i][:],
                         start=True, stop=True)
                S.dve("tensor_tensor", out=scm[i][:, :, :],
                      in0=pF.v(pF.t[:, 0:256].rearrange("p (a b) -> p a b", a=2)),
                      in1=Mincl.v(Mincl.t[:, :].unsqueeze(1).to_broadcast([128, 2, 128])), op=ALU.mult)
                if DEBUG.get('gstop') == 7:
                    continue
                for hh in range(2):
                    r = slice(64 * hh, 64 * hh + 64)
                    S.pe("matmul", out=pG[:, hh * 128:(hh + 1) * 128], lhsT=scm[i][:, hh, :],
                         rhs=zi[:, 256 + hh * 128:256 + (hh + 1) * 128], start=True, stop=False)
                    S.pe("matmul", out=pG[:, hh * 128:(hh + 1) * 128], lhsT=qgTh[i][:, hh, :], rhs=Sst[:],
                         start=False, stop=True)
                S.pe("matmul", out=pH[:, 0:256], lhsT=kd[i][:], rhs=v2, start=True, stop=True)
                if DEBUG.get('gstop') == 8:
                    continue
                for hh in range(2):
                    r = slice(64 * hh, 64 * hh + 64)
                    S.dve("scalar_tensor_tensor", out=Sst[r, :], in0=Sst[r, :], scalar=EbT[i][r, last:last + 1],
                          in1=pH[r, hh * 128:(hh + 1) * 128], op0=ALU.mult, op1=ALU.add)
                if d == 0:
                    S.act("activation", out=oacc[:, blk, :], in_=pG[:, 0:256], func=AF.Copy)
                else:
                    os_ = osum[i]
                    osv = os_.v(os_.t[:, :, :].rearrange("p a b -> p (a b)"))
                    S.dve("tensor_tensor", out=osv, in0=pG[:, 0:256], in1=oacc[:, blk, :], op=ALU.add)
                    S.act("activation", out=sg[i][:], in_=g2, func=AF.Silu)
                    S.dve("tensor_tensor", out=sq[:, :, :], in0=os_[:, :, :], in1=os_[:, :, :], op=ALU.mult)
                    S.dve("tensor_reduce", out=ss[:], in_=sq[:, :, :], axis=AX.X, op=ALU.add)
                    rsqrt_col(S, ss, ss, 1.0 / 128, 1e-6)
                    S.dve("tensor_tensor", out=os_[:, :, :], in0=os_[:, :, :],
                          in1=ss.v(ss.t[:, :].unsqueeze(2).to_broadcast([128, 2, 128])), op=ALU.mult)
                    S.dve("tensor_tensor", out=os_[:, :, :], in0=os_[:, :, :],
                          in1=nwb.v(nwb.t[:, :].unsqueeze(1).to_broadcast([128, 2, 128])), op=ALU.mult)
                    S.dve("tensor_tensor", out=osv, in0=osv, in1=sg[i][:], op=ALU.mult)
                    S.dma(out=ocat.v(ocat.t[rs_, 0:256]), in_=osv, eng="pool")
        if DEBUG.get('gstop'):
            S.dma(out=ocat.v(ocat.t[0:128, 0:128]), in_=nwb[:], eng="pool")


def dn_consts():
    p = np.arange(128)[:, None]
    f = np.arange(128)[None, :]
    same = (p // 64) == (f // 64)
    c = {}
    for d, le in (('f', lambda a, b: a <= b), ('r', lambda a, b: a >= b)):
        lt = (lambda a, b: a < b) if d == 'f' else (lambda a, b: a > b)
        c[f'dn_MTi_{d}'] = le(p, f).astype(np.float32)
        c[f'dn_MTSn_{d}'] = -(lt(p, f) & same).astype(np.float32)
        c[f'dn_MSn_{d}'] = -(lt(f, p) & same).astype(np.float32)
        c[f'dn_MSo_{d}'] = (lt(f, p) & ~same).astype(np.float32)
        c[f'dn_Mc_{d}'] = le(p, f).astype(np.float32)
    return c


def phase_dn_prep(S, z, prm, dnp):
    q0 = ZOFF['dn_q'][0]
    a0 = ZOFF['dn_a'][0]
    with S.scope():
        wk = S.sbuf([128, 5, 768])
        for k in range(5):
            S.dma(out=wk[:, k, :], in_=prm['dn_convw'].v(bc(prm['dn_convw'].t[k:k + 1, :])))
        nea = S.sbuf([128, 4])
        dtb = S.sbuf([128, 4])
        S.dma(out=nea[:], in_=prm['dn_alog'].v(bc(prm['dn_alog'].t[0:1, :])))
        S.dma(out=dtb[:], in_=prm['dn_dtb'].v(bc(prm['dn_dtb'].t[0:1, :])))
        S.act("activation", out=nea[:], in_=nea[:], func=AF.Exp)
        S.dve("tensor_scalar", out=nea[:], in0=nea[:], scalar1=-1.0, scalar2=None, op0=ALU.mult)
        xs = [[S.sbuf([128, 768]) for _ in range(5)] for _ in range(2)]
        ab = [S.sbuf([128, 8]) for _ in range(2)]
        tmp = [S.sbuf([128, 768]) for _ in range(2)]
        acc = S.sbuf([128, 768])
        yo = [S.sbuf([128, 776]) for _ in range(2)]
        sq = S.sbuf([128, 512])
        ss = S.sbuf([128, 4])
        e4 = S.sbuf([128, 4])
        e5 = S.sbuf([128, 4])
        for t in range(NT):
            t0 = t * 128
            lo, hi = (0, NCTX) if t < 2 else (NCTX, T)
            x5 = xs[t % 2]
            for k in range(5):
                s = k - 2
                a = max(t0 + s, lo)
                b = min(t0 + s + 128, hi)
                if a != t0 + s or b != t0 + s + 128:
                    S.I("dve", "memset", x5[k].t[:], 0.0, w=[x5[k]])
                S.dma(out=x5[k][a - (t0 + s):b - (t0 + s), :], in_=z.v(z.t[a:b, q0:q0 + 768]))
            S.dma(out=ab[t % 2][:], in_=z.v(z.t[t0:t0 + 128, a0:a0 + 8]))
            S.dve("tensor_tensor", out=acc[:], in0=x5[0][:], in1=wk[:, 0, :], op=ALU.mult)
            for k in range(1, 5):
                tm = tmp[k % 2]
                S.pool("tensor_tensor", out=tm[:], in0=x5[k][:], in1=wk[:, k, :], op=ALU.mult)
                S.dve("tensor_tensor", out=acc[:], in0=acc[:], in1=tm[:], op=ALU.add)
            y = yo[t % 2]
            S.act("activation", out=y[:, 0:768], in_=acc[:], func=AF.Silu)
            S.dve("tensor_tensor", out=sq[:], in0=y[:, 0:512], in1=y[:, 0:512], op=ALU.mult)
            S.dve("tensor_reduce", out=ss[:], in_=sq.v(sq.t[:, :].rearrange("p (v d) -> p v d", d=128)),
                  axis=AX.X, op=ALU.add)
            rsqrt_col(S, ss, ss, 1.0, 1e-6)
            S.dve("tensor_scalar", out=ss[:, 0:2], in0=ss[:, 0:2], scalar1=128 ** -0.5, scalar2=None, op0=ALU.mult)
            y3 = y.v(y.t[:, 0:512].rearrange("p (v d) -> p v d", d=128))
            S.dve("tensor_tensor", out=y3, in0=y3, in1=ss.v(ss.t[:, :].unsqueeze(2).to_broadcast([128, 4, 128])),
                  op=ALU.mult)
            S.dve("tensor_tensor", out=e4[:], in0=ab[t % 2][:, 0:4], in1=dtb[:], op=ALU.add)
            S.act("activation", out=e4[:], in_=e4[:], func=AF.Exp)
            S.act("activation", out=e4[:], in_=e4[:], func=AF.Ln, bias=1.0)
            S.dve("tensor_tensor", out=y[:, 768:772], in0=e4[:], in1=nea[:], op=ALU.mult)
            S.act("activation", out=e5[:], in_=ab[t % 2][:, 4:8], func=AF.Exp, scale=-1.0)
            S.dve("tensor_scalar", out=e5[:], in0=e5[:], scalar1=1.0, scalar2=None, op0=ALU.add)
            S.dve("reciprocal", out=y[:, 772:776], in_=e5[:])
            S.dma(out=dnp.v(dnp.t[t0:t0 + 128, :]), in_=y[:], eng="pool")


def phase_dn(S, z, dnp, consts, prm, ocat):
    g0 = ZOFF['dn_g'][0]
    with S.scope():
        idt = S.sbuf([128, 128])
        S.dma(out=idt[:], in_=consts['ident'].v(consts['ident'].t))
        ones = S.sbuf([128, 128])
        S.dma(out=ones[:], in_=consts['ones128'].v(consts['ones128'].t))
        M = {}
        for d in ('f', 'r'):
            for nm in ('MTi', 'MTSn', 'MSn', 'MSo', 'Mc'):
                key = f'dn_{nm}_{d}'
                M[key] = S.sbuf([128, 128], name='sb_' + key)
                S.dma(out=M[key][:], in_=consts[key].v(consts[key].t))
        nwb = S.sbuf([128, 128])
        S.dma(out=nwb[:], in_=prm['dn_nw'].v(bc(prm['dn_nw'].t[0:1, :])))
        oacc = S.sbuf([128, NT, 256])
        Sst = [S.sbuf([128, 128], name=f"dnS{h_}") for h_ in range(2)]
        R = 2
        zin = [S.sbuf([128, 776]) for _ in range(R)]
        gin = [S.sbuf([128, 256]) for _ in range(R)]
        sc = [S.sbuf([128, 4]) for _ in range(R)]
        egc = [S.sbuf([128, 2]) for _ in range(R)]
        edl = [S.sbuf([128, 2]) for _ in range(R)]
        dl = [S.sbuf([128, 2]) for _ in range(R)]
        bgc = [S.sbuf([128, 2]) for _ in range(R)]
        tdl = [S.sbuf([128, 2]) for _ in range(R)]

        def mk(n=128):
            return [[S.sbuf([128, n]) for _ in range(2)] for _ in range(R)]
        DD, kT, qT, qgT, E3, t1, E1, t2, E2 = mk(256), mk(), mk(), mk(), mk(), mk(), mk(), mk(), mk()
        DTm, attnT, bm1, bm2, XT, e2a, X, e2b, Lo = mk(), mk(), mk(), mk(), mk(), mk(), mk(), mk(), mk()
        P_ = [[[S.sbuf([128, 128]) for _ in range(2)] for _ in range(2)] for _ in range(R)]
        PT = [[[S.sbuf([128, 128]) for _ in range(2)] for _ in range(2)] for _ in range(R)]
        Rm = [[[S.sbuf([128, 128]) for _ in range(2)] for _ in range(2)] for _ in range(R)]
        RT = [[[S.sbuf([128, 128]) for _ in range(2)] for _ in range(2)] for _ in range(R)]
        A1, TmT, vb, kbg, kd, usb, wT, vnew = mk(), mk(), mk(), mk(), mk(), mk(), mk(), mk()
        osum = [S.sbuf([128, 2, 128]) for _ in range(R)]
        sg = [S.sbuf([128, 256]) for _ in range(R)]
        sq = S.sbuf([128, 2, 128])
        ss = S.sbuf([128, 2])
        bA = [S.psum([128, 512]) for _ in range(2)]
        bB = [S.psum([128, 512]) for _ in range(2)]
        bC = [S.psum([128, 512]) for _ in range(2)]
        bD = [S.psum([128, 512]) for _ in range(2)]
        for d in range(2):
            dn = 'f' if d == 0 else 'r'
            MTi, MTSn, MSn, MSo, Mc = (M[f'dn_{nm}_{dn}'] for nm in ('MTi', 'MTSn', 'MSn', 'MSo', 'Mc'))
            for hh in range(2):
                S.I("dve", "memset", Sst[hh].t[:], 0.0, w=[Sst[hh]])
            for n, blk in enumerate(block_order(d)[:DEBUG.get('dnb', NT)]):
                i = n % R
                rs_ = slice(blk * 128, (blk + 1) * 128)
                zi = zin[i]
                S.dma(out=zi[:], in_=dnp.v(dnp.t[rs_, :]))
                if d == 1:
                    S.dma(out=gin[i][:], in_=z.v(z.t[rs_, g0:g0 + 256]))
                g2 = zi[:, 768 + 2 * d:768 + 2 * d + 2]
                be = lambda hh: zi[:, 772 + 2 * d + hh:772 + 2 * d + hh + 1]
                S.pe("matmul", out=bD[0][:, 384:386], lhsT=Mc[:], rhs=g2, start=True, stop=True)
                S.pe("matmul", out=bD[0][:, 386:388], lhsT=ones[:], rhs=g2, start=True, stop=True)
                S.dve("tensor_copy", out=sc[i][:], in_=bD[0][:, 384:388])
                S.act("activation", out=egc[i][:], in_=sc[i][:, 0:2], func=AF.Exp)
                S.dve("tensor_tensor", out=tdl[i][:], in0=sc[i][:, 2:4], in1=sc[i][:, 0:2], op=ALU.subtract)
                S.act("activation", out=edl[i][:], in_=tdl[i][:], func=AF.Exp)
                S.act("activation", out=dl[i][:], in_=sc[i][:, 2:4], func=AF.Exp)
                S.dve("tensor_tensor", out=bgc[i][:], in0=egc[i][:], in1=zi[:, 772 + 2 * d:772 + 2 * d + 2], op=ALU.mult)
                def head_steps(hh, i=i, zi=zi, be=be, blk=blk, d=d):
                    pRB = pT = bA[hh]
                    pK = pU = bB[hh]
                    pI = bC[hh]
                    pO = pS = bD[hh]
                    q_h = zi[:, hh * 128:(hh + 1) * 128]
                    k_h = zi[:, 256 + hh * 128:256 + (hh + 1) * 128]
                    v_h = zi[:, 512 + hh * 128:512 + (hh + 1) * 128]
                    gcc = sc[i][:, hh:hh + 1]
                    c2 = slice(0, 256)
                    ca = slice(0, 128)
                    cb = slice(128, 256)
                    ta = slice(256, 384)
                    tb = slice(384, 512)
                    S.dve("tensor_scalar", out=DD[i][hh][:, 0:128], in0=idt[:], scalar1=gcc, scalar2=None, op0=ALU.mult)
                    S.dve("tensor_scalar", out=DD[i][hh][:, 128:256], in0=idt[:], scalar1=be(hh), scalar2=None, op0=ALU.mult)
                    S.pe("matmul", out=pRB[:, c2], lhsT=ones[:], rhs=DD[i][hh][:], start=True, stop=True)
                    S.pe("transpose", out=pT[:, ta], in_=q_h, identity=idt[:])
                    S.pe("transpose", out=pT[:, tb], in_=k_h, identity=idt[:])
                    yield
                    S.dve("tensor_copy", out=qT[i][hh][:], in_=pT[:, ta])
                    S.dve("tensor_copy", out=kT[i][hh][:], in_=pT[:, tb])
                    S.act("activation", out=E3[i][hh][:], in_=pRB[:, ca], func=AF.Exp)
                    S.dve("tensor_tensor", out=qgT[i][hh][:], in0=qT[i][hh][:], in1=E3[i][hh][:], op=ALU.mult)
                    S.pe("matmul", out=pK[:, ca], lhsT=kT[i][hh][:], rhs=kT[i][hh][:], start=True, stop=True)
                    S.pe("matmul", out=pK[:, cb], lhsT=kT[i][hh][:], rhs=qT[i][hh][:], start=True, stop=True)
                    yield
                    S.dve("tensor_scalar", out=t1[i][hh][:], in0=pRB[:, ca], scalar1=gcc, scalar2=0.0,
                          op0=ALU.subtract, op1=ALU.min)
                    S.dve("tensor_scalar", out=t2[i][hh][:], in0=pRB[:, ca], scalar1=gcc, scalar2=0.0,
                          op0=ALU.subtract, op1=ALU.max)
                    S.dve("tensor_tensor", out=bm1[i][hh][:], in0=pRB[:, cb], in1=MTSn[:], op=ALU.mult)
                    S.act("activation", out=E1[i][hh][:], in_=t1[i][hh][:], func=AF.Exp)
                    S.act("activation", out=E2[i][hh][:], in_=t2[i][hh][:], func=AF.Exp, scale=-1.0)
                    yield
                    S.dve("tensor_tensor", out=DTm[i][hh][:], in0=E1[i][hh][:], in1=MTi[:], op=ALU.mult)
                    S.dve("tensor_tensor", out=attnT[i][hh][:], in0=pK[:, cb], in1=DTm[i][hh][:], op=ALU.mult)
                    S.dve("tensor_tensor", out=bm2[i][hh][:], in0=bm1[i][hh][:], in1=E1[i][hh][:], op=ALU.mult)
                    S.dve("tensor_tensor", out=XT[i][hh][:], in0=pK[:, ca], in1=bm2[i][hh][:], op=ALU.mult)
                    S.dve("scalar_tensor_tensor", out=e2a[i][hh][:], in0=E2[i][hh][:], scalar=be(hh), in1=MSn[:],
                          op0=ALU.mult, op1=ALU.mult)
                    S.dve("tensor_tensor", out=X[i][hh][:], in0=pK[:, ca], in1=e2a[i][hh][:], op=ALU.mult)
                    S.dve("scalar_tensor_tensor", out=e2b[i][hh][:], in0=E2[i][hh][:], scalar=be(hh), in1=MSo[:],
                          op0=ALU.mult, op1=ALU.mult)
                    S.dve("tensor_tensor", out=Lo[i][hh][:], in0=pK[:, ca], in1=e2b[i][hh][:], op=ALU.mult)
                    Pc, PTc, Rc, RTc = X[i][hh], XT[i][hh], Rm[i][hh][0], RT[i][hh][0]
                    S.dve("tensor_tensor", out=Rc[:], in0=X[i][hh][:], in1=idt[:], op=ALU.add)
                    S.dve("tensor_tensor", out=RTc[:], in0=XT[i][hh][:], in1=idt[:], op=ALU.add)
                    yield
                    for k in range(1, 6):
                        Pn, PTn = P_[i][hh][k % 2], PT[i][hh][k % 2]
                        Rn, RTn = Rm[i][hh][k % 2], RT[i][hh][k % 2]
                        S.pe("matmul", out=pI[:, 0:128], lhsT=PTc[:], rhs=Pc[:], start=True, stop=True)
                        S.pe("matmul", out=pI[:, 128:256], lhsT=Pc[:], rhs=PTc[:], start=True, stop=True)
                        S.act("activation", out=Pn[:], in_=pI[:, 0:128], func=AF.Copy)
                        S.act("activation", out=PTn[:], in_=pI[:, 128:256], func=AF.Copy)
                        yield
                        S.pe("matmul", out=pI[:, 256:384], lhsT=PTn[:], rhs=Rc[:], start=True, stop=True)
                        S.pe("matmul", out=pI[:, 384:512], lhsT=Pn[:], rhs=RTc[:], start=True, stop=True)
                        S.act("activation", out=Rn[:], in_=pI[:, 256:384], func=AF.Copy) if False else None
                        S.dve("tensor_tensor", out=Rn[:], in0=pI[:, 256:384], in1=Rc[:], op=ALU.add)
                        S.dve("tensor_tensor", out=RTn[:], in0=pI[:, 384:512], in1=RTc[:], op=ALU.add)
                        Pc, PTc, Rc, RTc = Pn, PTn, Rn, RTn
                        yield
                    Td, TdT = Rc, RTc
                    S.pe("matmul", out=pI[:, 0:128], lhsT=Lo[i][hh][:], rhs=TdT[:], start=True, stop=True)
                    yield
                    S.act("activation", out=A1[i][hh][:], in_=pI[:, 0:128], func=AF.Copy)
                    S.pe("matmul", out=pI[:, 128:256], lhsT=Td[:], rhs=A1[i][hh][:], start=True, stop=True)
                    S.dve("tensor_tensor", out=TmT[i][hh][:], in0=TdT[:], in1=pI[:, 128:256], op=ALU.subtract)
                    S.pool("tensor_scalar", out=vb[i][hh][:], in0=v_h, scalar1=be(hh), scalar2=None, op0=ALU.mult)
                    S.pool("tensor_scalar", out=kbg[i][hh][:], in0=k_h, scalar1=bgc[i][:, hh:hh + 1], scalar2=None, op0=ALU.mult)
                    S.pool("tensor_scalar", out=kd[i][hh][:], in0=k_h, scalar1=edl[i][:, hh:hh + 1], scalar2=None, op0=ALU.mult)
                    cu = slice(256, 384)
                    cw = slice(384, 512)
                    yield
                    S.pe("matmul", out=pU[:, cu], lhsT=TmT[i][hh][:], rhs=vb[i][hh][:], start=True, stop=True)
                    S.pe("matmul", out=pU[:, cw], lhsT=kbg[i][hh][:], rhs=TmT[i][hh][:], start=True, stop=True)
                    S.act("activation", out=usb[i][hh][:], in_=pU[:, cu], func=AF.Copy)
                    S.act("activation", out=wT[i][hh][:], in_=pU[:, cw], func=AF.Copy)
                    yield
                    S.pe("matmul", out=pO[:, 0:128], lhsT=wT[i][hh][:], rhs=Sst[hh][:], start=True, stop=True)
                    yield
                    S.dve("tensor_tensor", out=vnew[i][hh][:], in0=usb[i][hh][:], in1=pO[:, 0:128], op=ALU.subtract)
                    S.pe("matmul", out=pO[:, 128:256], lhsT=qgT[i][hh][:], rhs=Sst[hh][:], start=True, stop=False)
                    S.pe("matmul", out=pO[:, 128:256], lhsT=attnT[i][hh][:], rhs=vnew[i][hh][:], start=False, stop=True)
                    S.pe("matmul", out=pS[:, 256:384], lhsT=kd[i][hh][:], rhs=vnew[i][hh][:],
                         start=True, stop=True)
                    yield
                    S.dve("scalar_tensor_tensor", out=Sst[hh][:], in0=Sst[hh][:], scalar=dl[i][:, hh:hh + 1],
                          in1=pS[:, 256:384], op0=ALU.mult, op1=ALU.add)
                    if d == 0:
                        S.dve("tensor_copy", out=oacc[:, blk, hh * 128:(hh + 1) * 128], in_=pO[:, 128:256])
                    else:
                        S.dve("tensor_tensor", out=osum[i][:, hh, :], in0=pO[:, 128:256],
                              in1=oacc[:, blk, hh * 128:(hh + 1) * 128], op=ALU.add)

                gens = [head_steps(0), head_steps(1)]
                while gens:
                    for gn in list(gens):
                        try:
                            next(gn)
                        except StopIteration:
                            gens.remove(gn)
                if d == 1:
                    os_ = osum[i]
                    osv = os_.v(os_.t[:, :, :].rearrange("p a b -> p (a b)"))
                    S.act("activation", out=sg[i][:], in_=gin[i][:], func=AF.Silu)
                    S.dve("tensor_tensor", out=sq[:, :, :], in0=os_[:, :, :], in1=os_[:, :, :], op=ALU.mult)
                    S.dve("tensor_reduce", out=ss[:], in_=sq[:, :, :], axis=AX.X, op=ALU.add)
                    rsqrt_col(S, ss, ss, 1.0 / 128, 1e-6)
                    S.dve("tensor_tensor", out=os_[:, :, :], in0=os_[:, :, :],
                          in1=ss.v(ss.t[:, :].unsqueeze(2).to_broadcast([128, 2, 128])), op=ALU.mult)
                    S.dve("tensor_tensor", out=os_[:, :, :], in0=os_[:, :, :],
                          in1=nwb.v(nwb.t[:, :].unsqueeze(1).to_broadcast([128, 2, 128])), op=ALU.mult)
                    S.dve("tensor_tensor", out=osv, in0=osv, in1=sg[i][:], op=ALU.mult)
                    S.dma(out=ocat.v(ocat.t[rs_, 256:512]), in_=osv, eng="pool")


CTX_TILES = (0, 17)
NOWN = 17


def phase_c1(S, ofull, xin, modv, w_out, xmid, ident, ctx_tiles=CTX_TILES, gathered=False, rpc=512):
    with S.scope():
        idt = S.sbuf([128, 128])
        S.dma(out=idt[:], in_=ident.v(ident.t))
        Wb = S.sbuf([128, 16, 2048], BF16)
        ot = [S.sbuf([128, 2048]) for _ in range(2)]
        xt = [S.sbuf([128, 2048]) for _ in range(2)]
        load_w_bf16(S, Wb, w_out, ot + xt, 16, 2048)
        G1 = S.sbuf([128, 2048])
        oT = [S.sbuf([128, 16, 128], BF16) for _ in range(2)]
        xm = [S.sbuf([128, 2048]) for _ in range(2)]
        pst = [S.psum([128, 512]) for _ in range(2)]
        py = [S.psum([128, 512]) for _ in range(4)]
        order = list(ctx_tiles) + [t for t in range(NT) if t not in ctx_tiles]
        for n, t in enumerate(order):
            if n == 0 or n == 2:
                r = 1 if n == 0 else 0
                S.dma(out=G1[:], in_=modv.v(bc(modv.t[r:r + 1, 4096:6144])))
            rs_ = slice(t * 128, (t + 1) * 128)
            o_, x_ = ot[n % 2], xt[n % 2]
            if gathered:
                for r in range(2):
                    g0 = gat_row(t, r, rpc)
                    S.dma(out=o_[:, r * 1024:(r + 1) * 1024], in_=ofull.v(ofull.t[g0:g0 + 128, :]))
            else:
                S.dma(out=o_[:], in_=ofull.v(ofull.t[rs_, :]))
            S.dma(out=x_[:], in_=xin.v(xin.t[rs_, :]))
            transpose_tile(S, o_, oT[n % 2], idt, pst)
            for ct in range(4):
                cs = slice(ct * 512, (ct + 1) * 512)
                for k in range(16):
                    S.pe("matmul", out=py[ct][:], lhsT=oT[n % 2][:, k, :], rhs=Wb[:, k, cs], start=(k == 0), stop=(k == 15))
                S.dve("tensor_tensor", out=xm[n % 2][:, cs], in0=py[ct][:], in1=G1[:, cs], op=ALU.mult)
                S.pool("tensor_tensor", out=xm[n % 2][:, cs], in0=xm[n % 2][:, cs], in1=x_[:, cs], op=ALU.add)
            S.dma(out=xmid.v(xmid.t[rs_, :]), in_=xm[n % 2][:], eng="pool")


def phase_c2(S, xmid, modv, nw2, router_w, h2T, wm, consts, ctx_tiles=CTX_TILES, nown=NOWN, natural=False):
    ident = consts['ident']
    with S.scope():
        idt = S.sbuf([128, 128])
        S.dma(out=idt[:], in_=ident.v(ident.t))
        nwb = S.sbuf([128, 2048])
        S.dma(out=nwb[:], in_=nw2.v(bc(nw2.t[0:1, :])))
        rw = S.sbuf([128, 16, 16])
        S.dma(out=rw[:], in_=router_w.v(router_w.t.rearrange("(k p) e -> p k e", p=128)))
        A = S.sbuf([128, 2048])
        Bv = S.sbuf([128, 2048])
        xt = [S.sbuf([128, 2048]) for _ in range(2)]
        hb = S.sbuf([128, 2048])
        junk = S.sbuf([128, 2048], BF16)
        hT32 = [S.sbuf([128, 16, 128]) for _ in range(2)]
        hTb = [S.sbuf([128, 16, 128], BF16) for _ in range(2)]
        ssq = S.sbuf([128, 1])
        rstd = S.sbuf([128, 1])
        mx = S.sbuf([128, 1])
        sm = S.sbuf([128, 1])
        ex = S.sbuf([128, 16])
        aff = S.sbuf([128, 16])
        affT = S.sbuf([16, T])
        pst = [S.psum([128, 512]) for _ in range(2)]
        pl = S.psum([128, 512])
        pa = S.psum([128, 512])
        order = list(ctx_tiles) + [t for t in range(NT) if t not in ctx_tiles]
        for n, t in enumerate(order):
            if n == 0 or n == 2:
                r = 1 if n == 0 else 0
                S.dma(out=A[:], in_=modv.v(bc(modv.t[r:r + 1, 4 * 2048:5 * 2048])))
                S.dma(out=Bv[:], in_=modv.v(bc(modv.t[r:r + 1, 3 * 2048:4 * 2048])))
                S.dve("scalar_tensor_tensor", out=A[:], in0=A[:], scalar=1.0, in1=nwb[:], op0=ALU.add, op1=ALU.mult)
            rs_ = slice(t * 128, (t + 1) * 128)
            x_ = xt[n % 2]
            S.dma(out=x_[:], in_=xmid.v(xmid.t[rs_, :]))
            rms_mod(S, x_, A, Bv, hb, junk, ssq, rstd)
            transpose_tile(S, hb, hT32[n % 2], idt, pst)
            if t < nown:
                S.pool("tensor_copy", out=hTb[n % 2][:, :, :], in_=hT32[n % 2][:, :, :])
                S.dma(out=h2T.v(h2T.t[:, :, t * 128:(t + 1) * 128]), in_=hTb[n % 2][:, :, :], eng="pool")
            for k in range(16):
                S.pe("matmul", out=pl[:, 0:16], lhsT=hT32[n % 2][:, k, :], rhs=rw[:, k, :], start=(k == 0), stop=(k == 15))
            S.dve("tensor_reduce", out=mx[:], in_=pl[:, 0:16], axis=AX.X, op=ALU.max)
            S.dve("tensor_scalar", out=mx[:], in0=mx[:], scalar1=-1.0, scalar2=None, op0=ALU.mult)
            S.act("activation", out=ex[:], in_=pl[:, 0:16], func=AF.Exp, bias=mx[:], accum_out=sm[:])
            S.dve("reciprocal", out=sm[:], in_=sm[:])
            S.dve("tensor_scalar", out=aff[:], in0=ex[:], scalar1=sm[:], scalar2=None, op0=ALU.mult)
            S.pe("transpose", out=pa[0:16, 0:128], in_=aff[:], identity=idt[:])
            S.act("activation", out=affT[:, rs_], in_=pa[0:16, 0:128], func=AF.Copy)
        work = S.sbuf([16, T])
        m8 = S.sbuf([16, 8])
        S.dve("tensor_copy", out=work[:], in_=affT[:])
        if natural:
            lat = work[:, NCTX:T]
            ctxv = work[:, 0:NCTX]
        else:
            lat = work.v(bass.AP(work.t, 128, [[T, 16], [2176, 2], [1, 2048]]))
            ctxv = work.v(bass.AP(work.t, 0, [[T, 16], [2176, 2], [1, 128]]))
        for (view, kk) in ((lat, 512), (ctxv, 32)):
            for _ in range(kk // 8):
                S.dve("max", out=m8[:], in_=view)
                S.dve("match_replace", out=view, in_to_replace=m8[:], in_values=view, imm_value=0.0)
        S.dve("tensor_tensor", out=work[:], in0=affT[:], in1=work[:], op=ALU.subtract)
        wmt = [S.sbuf([128, 16]) for _ in range(2)]
        for t in range(NT):
            S.pe("transpose", out=pa[:, 0:16], in_=work[:, t * 128:(t + 1) * 128], identity=idt[0:16, 0:16])
            S.act("activation", out=wmt[t % 2][:], in_=pa[:, 0:16], func=AF.Copy)
            S.dma(out=wm.v(wm.t[t * 128:(t + 1) * 128, :]), in_=wmt[t % 2][:], eng="pool")


def phase_moe(S, h2T, wm, xmid, modv, wg, wu, wd, xout, final_nw=None):
    groups = [(0, 6), (6, 6), (12, 5)]
    with S.scope():
        G2c = S.sbuf([128, 2048])
        G2l = None
        hT = S.sbuf([128, 16, 768], BF16)
        yacc = S.sbuf([128, 6, 2048])
        hid = S.sbuf([128, 8, 768], BF16)
        wmt = S.sbuf([128, 6, 16])
        sgu = [[S.sbuf([128, 16, 128]) for _ in range(2)] for _ in range(2)]
        bgu = [[S.sbuf([128, 16, 128], BF16) for _ in range(2)] for _ in range(2)]
        sd = [S.sbuf([128, 8, 256]) for _ in range(2)]
        bd = [S.sbuf([128, 8, 256], BF16) for _ in range(2)]
        sgt = [S.sbuf([128, 512], BF16) for _ in range(2)]
        xm = S.sbuf([128, 2048])
        nwf = None
        if final_nw is not None:
            nwf = S.sbuf([128, 2048])
            S.dma(out=nwf[:], in_=final_nw.v(bc(final_nw.t[0:1, :])))
            junk = S.sbuf([128, 2048], BF16)
            ssq = S.sbuf([128, 1])
            rstd = S.sbuf([128, 1])
        pg = [S.psum([128, 512]) for _ in range(2)]
        pu = [S.psum([128, 512]) for _ in range(2)]
        pd = [S.psum([128, 512]) for _ in range(4)]
        cnt = 0
        dcnt = 0
        for (t0, ntl) in groups:
            ntok = ntl * 128
            S.dma(out=hT[:, :, 0:ntok], in_=h2T.v(h2T.t[:, :, t0 * 128:t0 * 128 + ntok]))
            S.dma(out=wmt[:, 0:ntl, :], in_=wm.v(wm.t[t0 * 128:t0 * 128 + ntok, :].rearrange("(a p) e -> p a e", p=128)))
            S.I("pool", "memset", yacc.t[:], 0.0, w=[yacc])
            subs = [(s0, min(512, ntok - s0)) for s0 in range(0, ntok, 512)]
            for e in range(16):
                for fc in range(8):
                    rg = cnt % 2
                    cnt += 1
                    fs = slice(fc * 128, (fc + 1) * 128)
                    for j, wsrc in enumerate((wg, wu)):
                        S.dma(out=sgu[rg][j][:, :, :], in_=wsrc.v(wsrc.t[e].rearrange("(k p) f -> p k f", p=128)[:, :, fs]))
                        S.I("pool" if j == 0 else "dve", "tensor_copy", out=bgu[rg][j][:, :, :], in_=sgu[rg][j][:, :, :])
                    for si, (s0, sn) in enumerate(subs):
                        for k in range(16):
                            S.pe("matmul", out=pg[si % 2][:, :sn], lhsT=bgu[rg][0][:, k, :], rhs=hT[:, k, s0:s0 + sn],
                                 start=(k == 0), stop=(k == 15))
                        for k in range(16):
                            S.pe("matmul", out=pu[si % 2][:, :sn], lhsT=bgu[rg][1][:, k, :], rhs=hT[:, k, s0:s0 + sn],
                                 start=(k == 0), stop=(k == 15))
                        S.act("activation", out=sgt[si % 2][:, :sn], in_=pg[si % 2][:, :sn], func=AF.Silu)
                        S.dve("tensor_tensor", out=hid[:, fc, s0:s0 + sn], in0=pu[si % 2][:, :sn], in1=sgt[si % 2][:, :sn], op=ALU.mult)
                for dc in range(8):
                    rg = dcnt % 2
                    dcnt += 1
                    ds_ = slice(dc * 256, (dc + 1) * 256)
                    S.dma(out=sd[rg][:, :, :], in_=wd.v(wd.t[e].rearrange("(k p) d -> p k d", p=128)[:, :, ds_]))
                    S.act("activation", out=bd[rg][:, :, :], in_=sd[rg][:, :, :], func=AF.Copy)
                    for tl in range(ntl):
                        pp = pd[(dc * ntl + tl) % 4]
                        for k in range(8):
                            S.pe("matmul", out=pp[:, 0:256], lhsT=hid[:, k, tl * 128:(tl + 1) * 128], rhs=bd[rg][:, k, :],
                                 start=(k == 0), stop=(k == 7))
                        S.dve("scalar_tensor_tensor", out=yacc[:, tl, ds_], in0=pp[:, 0:256], scalar=wmt[:, tl, e:e + 1],
                              in1=yacc[:, tl, ds_], op0=ALU.mult, op1=ALU.add)
            for tl in range(ntl):
                t = t0 + tl
                if t == 0:
                    S.dma(out=G2c[:], in_=modv.v(bc(modv.t[1:2, 5 * 2048:6 * 2048])))
                if t == 1:
                    S.dma(out=G2c[:], in_=modv.v(bc(modv.t[0:1, 5 * 2048:6 * 2048])))
                S.dma(out=xm[:], in_=xmid.v(xmid.t[t * 128:(t + 1) * 128, :]))
                S.dve("tensor_tensor", out=yacc[:, tl, :], in0=yacc[:, tl, :], in1=G2c[:], op=ALU.mult)
                S.pool("tensor_tensor", out=yacc[:, tl, :], in0=yacc[:, tl, :], in1=xm[:], op=ALU.add)
                if nwf is not None:
                    S.act("activation", out=junk[:], in_=yacc[:, tl, :], func=AF.Square, accum_out=ssq[:])
                    rsqrt_col(S, rstd, ssq, 1.0 / 2048, 1e-6)
                    S.dve("scalar_tensor_tensor", out=yacc[:, tl, :], in0=yacc[:, tl, :], scalar=rstd[:], in1=nwf[:],
                          op0=ALU.mult, op1=ALU.mult)
                S.dma(out=xout.v(xout.t[t * 128:(t + 1) * 128, :]), in_=yacc[:, tl, :], eng="pool")


def phase_moe_part(S, h2T, wm, wg, wu, wd, ypart, nexp=8):
    groups = [(t0, min(6, NT - t0)) for t0 in range(0, NT, 6)]
    with S.scope():
        hT = S.sbuf([128, 16, 768], BF16)
        yacc = S.sbuf([128, 6, 2048])
        hid = S.sbuf([128, 8, 768], BF16)
        wmt = S.sbuf([128, 6, 16])
        sgu = [[S.sbuf([128, 16, 128]) for _ in range(2)] for _ in range(2)]
        bgu = [[S.sbuf([128, 16, 128], BF16) for _ in range(2)] for _ in range(2)]
        sd = [S.sbuf([128, 8, 256]) for _ in range(2)]
        bd = [S.sbuf([128, 8, 256], BF16) for _ in range(2)]
        sgt = [S.sbuf([128, 512], BF16) for _ in range(2)]
        pg = [S.psum([128, 512]) for _ in range(2)]
        pu = [S.psum([128, 512]) for _ in range(2)]
        pd = [S.psum([128, 512]) for _ in range(4)]
        cnt = 0
        dcnt = 0
        for (t0, ntl) in groups:
            ntok = ntl * 128
            S.dma(out=hT[:, :, 0:ntok], in_=h2T.v(h2T.t[:, :, t0 * 128:t0 * 128 + ntok]))
            S.dma(out=wmt[:, 0:ntl, :], in_=wm.v(wm.t[t0 * 128:t0 * 128 + ntok, :].rearrange("(a p) e -> p a e", p=128)))
            S.I("pool", "memset", yacc.t[:], 0.0, w=[yacc])
            subs = [(s0, min(512, ntok - s0)) for s0 in range(0, ntok, 512)]
            for e in range(nexp):
                for fc in range(8):
                    rg = cnt % 2
                    cnt += 1
                    fs = slice(fc * 128, (fc + 1) * 128)
                    for j, wsrc in enumerate((wg, wu)):
                        S.dma(out=sgu[rg][j][:, :, :], in_=wsrc.v(wsrc.t[e].rearrange("(k p) f -> p k f", p=128)[:, :, fs]))
                        S.I("pool" if j == 0 else "dve", "tensor_copy", out=bgu[rg][j][:, :, :], in_=sgu[rg][j][:, :, :])
                    for si, (s0, sn) in enumerate(subs):
                        for k in range(16):
                            S.pe("matmul", out=pg[si % 2][:, :sn], lhsT=bgu[rg][0][:, k, :], rhs=hT[:, k, s0:s0 + sn],
                                 start=(k == 0), stop=(k == 15))
                        for k in range(16):
                            S.pe("matmul", out=pu[si % 2][:, :sn], lhsT=bgu[rg][1][:, k, :], rhs=hT[:, k, s0:s0 + sn],
                                 start=(k == 0), stop=(k == 15))
                        S.act("activation", out=sgt[si % 2][:, :sn], in_=pg[si % 2][:, :sn], func=AF.Silu)
                        S.dve("tensor_tensor", out=hid[:, fc, s0:s0 + sn], in0=pu[si % 2][:, :sn], in1=sgt[si % 2][:, :sn], op=ALU.mult)
                for dc in range(8):
                    rg = dcnt % 2
                    dcnt += 1
                    ds_ = slice(dc * 256, (dc + 1) * 256)
                    S.dma(out=sd[rg][:, :, :], in_=wd.v(wd.t[e].rearrange("(k p) d -> p k d", p=128)[:, :, ds_]))
                    S.act("activation", out=bd[rg][:, :, :], in_=sd[rg][:, :, :], func=AF.Copy)
                    for tl in range(ntl):
                        pp = pd[(dc * ntl + tl) % 4]
                        for k in range(8):
                            S.pe("matmul", out=pp[:, 0:256], lhsT=hid[:, k, tl * 128:(tl + 1) * 128], rhs=bd[rg][:, k, :],
                                 start=(k == 0), stop=(k == 7))
                        S.dve("scalar_tensor_tensor", out=yacc[:, tl, ds_], in0=pp[:, 0:256], scalar=wmt[:, tl, e:e + 1],
                              in1=yacc[:, tl, ds_], op0=ALU.mult, op1=ALU.add)
            S.dma(out=ypart.v(ypart.t[t0 * 128:t0 * 128 + ntok, :].rearrange("(a p) d -> p a d", p=128)),
                  in_=yacc[:, 0:ntl, :], eng="pool")


def phase_fin(S, ygat, xmid, modv, xnext, final_nw=None, out_lat=None, rpc=256):
    with S.scope():
        G2 = S.sbuf([128, 2048])
        y0 = [S.sbuf([128, 2048]) for _ in range(2)]
        y1 = [S.sbuf([128, 2048]) for _ in range(2)]
        xm = [S.sbuf([128, 2048]) for _ in range(2)]
        if final_nw is not None:
            nwf = S.sbuf([128, 2048])
            S.dma(out=nwf[:], in_=final_nw.v(bc(final_nw.t[0:1, :])))
            junk = S.sbuf([128, 2048], BF16)
            ssq = S.sbuf([128, 1])
            rstd = S.sbuf([128, 1])
        for t in range(NT):
            if final_nw is not None and t < 2:
                continue
            if t == 0 or t == 2 or (final_nw is not None and t == 2):
                r = 1 if t == 0 else 0
                S.dma(out=G2[:], in_=modv.v(bc(modv.t[r:r + 1, 5 * 2048:6 * 2048])))
            rs_ = slice(t * 128, (t + 1) * 128)
            a, b, x_ = y0[t % 2], y1[t % 2], xm[t % 2]
            ga, gb_ = gat_row(t, 0, rpc), gat_row(t, 1, rpc)
            S.dma(out=a[:], in_=ygat.v(ygat.t[ga:ga + 128, :]))
            S.dma(out=b[:], in_=ygat.v(ygat.t[gb_:gb_ + 128, :]))
            S.dma(out=x_[:], in_=xmid.v(xmid.t[rs_, :]))
            S.dve("tensor_tensor", out=a[:], in0=a[:], in1=b[:], op=ALU.add)
            S.pool("tensor_tensor", out=a[:], in0=a[:], in1=G2[:], op=ALU.mult)
            S.dve("tensor_tensor", out=a[:], in0=a[:], in1=x_[:], op=ALU.add)
            if final_nw is not None:
                S.act("activation", out=junk[:], in_=a[:], func=AF.Square, accum_out=ssq[:])
                rsqrt_col(S, rstd, ssq, 1.0 / 2048, 1e-6)
                S.dve("scalar_tensor_tensor", out=a[:], in0=a[:], scalar=rstd[:], in1=nwf[:], op0=ALU.mult, op1=ALU.mult)
                S.dma(out=out_lat.v(out_lat.t[(t - 2) * 128:(t - 1) * 128, :]), in_=a[:], eng="pool")
            else:
                S.dma(out=xnext.v(xnext.t[rs_, :]), in_=a[:], eng="pool")


def pair_gather(S, src, dst, rpc):
    for r0 in range(0, T, rpc):
        r1 = min(T, r0 + rpc)
        S.I("pool", "collective_compute", "AllGather", ALU.bypass, replica_groups=[[0, 1], [2, 3], [4, 5], [6, 7]],
            ins=[src.t[r0:r1, :]], outs=[dst.t[2 * r0:2 * r1, :]], r=[src], w=[dst])


def gat_row(t, r, rpc):
    r0 = (t * 128 // rpc) * rpc
    rows_c = min(rpc, T - r0)
    return 2 * r0 + r * rows_c + (t * 128 - r0)


import math

DEPTH = 2
_CONSTS = None
_PROGS = {}


def host_consts():
    global _CONSTS
    if _CONSTS is None:
        _CONSTS = {'ident': np.eye(128, dtype=np.float32), **rope_tables(), **tri_consts(), **dn_consts()}
    return _CONSTS


A_PRM_SHAPES = {'w2pad0': [32, 128], 'w2pad1': [32, 128], 'gb0': [1, 128], 'gb1': [1, 128], 'gla_nw': [1, 128],
                'dn_convw': [5, 768], 'dn_alog': [1, 4], 'dn_dtb': [1, 4], 'dn_nw': [1, 128],
                'gqa_qn': [1, 128], 'gqa_kn': [1, 128], 'diff_lambda': [1, 256], 'diff_nw': [1, 128], 'lamc': [1, 2]}


def layer_params(inp, l, h):
    p = {}
    w2 = inp['gla_gate_w2'][l]
    gbias = inp['gla_gate_b'][l]
    for d in range(2):
        wp = np.zeros((32, 128), np.float32)
        wp[d * 16:(d + 1) * 16] = w2[d][:, 128 * h:128 * h + 128]
        p[f'w2pad{d}'] = wp
        p[f'gb{d}'] = np.ascontiguousarray(gbias[d][None, 128 * h:128 * h + 128])
    p['gla_nw'] = np.ascontiguousarray(inp['gla_norm_w'][l][None])
    cw = inp['dn_conv_w'][l]
    p['dn_convw'] = np.ascontiguousarray(np.concatenate(
        [cw[:, 256 * h:256 * h + 256], cw[:, 512 + 256 * h:512 + 256 * h + 256], cw[:, 1024 + 256 * h:1024 + 256 * h + 256]], 1))
    p['dn_alog'] = np.ascontiguousarray(inp['dn_a_log'][l][:, 2 * h:2 * h + 2].reshape(1, 4))
    p['dn_dtb'] = np.ascontiguousarray(inp['dn_dt_bias'][l][:, 2 * h:2 * h + 2].reshape(1, 4))
    p['dn_nw'] = np.ascontiguousarray(inp['dn_norm_w'][l][None])
    p['gqa_qn'] = np.ascontiguousarray(inp['gqa_q_norm'][l][None])
    p['gqa_kn'] = np.ascontiguousarray(inp['gqa_k_norm'][l][None])
    p['diff_lambda'] = np.ascontiguousarray(inp['diff_lambda'][l].reshape(1, 256))
    p['diff_nw'] = np.ascontiguousarray(inp['diff_norm_w'][l][None])
    lam_init = 0.8 - 0.6 * math.exp(-0.3 * l)
    p['lamc'] = np.array([[1.0 - lam_init, -lam_init]], np.float32)
    return p


def prog_F():
    if 'F' in _PROGS:
        return _PROGS['F']
    nc = bass.Bass("TRN2", target_bir_lowering=False)
    C = host_consts()
    with ExitStack() as st:
        S = Sched(nc, st)
        xin0 = S.dram("xin", [T, 2048], kind="ExternalInput")
        c2 = S.dram("c2", [2, 2048], kind="ExternalInput")
        consts = {k: S.dram(k, list(v.shape), kind="ExternalInput") for k, v in C.items()}
        fnw = S.dram("fnw", [1, 2048], kind="ExternalInput")
        yout = S.dram("yout", [4096, 2048], kind="ExternalOutput")
        z = S.dram("z", [T, ZC])
        dnp = S.dram("dnp", [T, 776])
        xcur = xin0
        for l in range(DEPTH):
            last = l == DEPTH - 1
            E = lambda nm, shp, dt=F32: S.dram(f"{nm}_{l}", shp, dt, kind="ExternalInput")
            mod_w = E("mod_w", [2048, 12288])
            mod_b = E("mod_b", [1, 12288])
            nw = E("nw", [1, 2048])
            w_in = E("w_in", [2048, ZC])
            prm = {k: E(k, shp) for k, shp in A_PRM_SHAPES.items()}
            w_out = E("w_out", [2048, 2048])
            nw2 = E("nw2", [1, 2048])
            router_w = E("router_w", [2048, 16])
            wg = E("wg", [8, 2048, 1024])
            wu = E("wu", [8, 2048, 1024])
            wd = E("wd", [8, 1024, 2048])
            I_ = lambda nm, shp, dt=F32: S.dram(f"{nm}_{l}", shp, dt)
            modv = I_("modv", [2, 12288])
            ocat = I_("ocat", [T, 1024])
            ogat = I_("ogat", [2 * T, 1024])
            xmid = I_("xmid", [T, 2048])
            wm = I_("wm", [T, 16])
            h2T = I_("h2T", [128, 16, T], BF16)
            ypart = I_("ypart", [T, 2048])
            ygat = I_("ygat", [2 * T, 2048])
            xnext = None if last else I_("xnext", [T, 2048])
            phase_mod(S, c2, mod_w, mod_b, modv)
            phase_in(S, xcur, modv, nw, w_in, z, consts['ident'])
            phase_gla(S, z, consts, prm, ocat)
            phase_dn_prep(S, z, prm, dnp)
            phase_dn(S, z, dnp, consts, prm, ocat)
            phase_attn(S, z, consts, prm, ocat)
            pair_gather(S, ocat, ogat, 512)
            phase_c1(S, ogat, xcur, modv, w_out, xmid, consts['ident'], ctx_tiles=(0, 1), gathered=True)
            phase_c2(S, xmid, modv, nw2, router_w, h2T, wm, consts, ctx_tiles=(0, 1), nown=NT, natural=True)
            phase_moe_part(S, h2T, wm, wg, wu, wd, ypart)
            pair_gather(S, ypart, ygat, 256)
            phase_fin(S, ygat, xmid, modv, xnext, final_nw=fnw if last else None, out_lat=yout if last else None)
            xcur = xnext
        outs = [o for o in S.all_ops if o.is_dma and o.eng == "pool" and o.fn[0] == "dma_start"]
        S.emit(final_waits=outs[-40:])
        _PROGS['F_info'] = (S.nsem, {e: len(v) for e, v in S.ops.items()})
    _PROGS['F'] = nc
    return nc


def kernel(**inp):
    inp = {k: np.asarray(v) for k, v in inp.items()}
    C = host_consts()
    B = 4
    cores = [(b, h) for b in range(B) for h in range(2)]
    nc = prog_F()
    perm_o = np.array([g * 512 + h * 256 + j for h in range(2) for g in range(4) for j in range(256)])
    shared = {}
    per_h = [dict(), dict()]
    for l in range(DEPTH):
        shared[f"mod_w_{l}"] = np.ascontiguousarray(inp['mod_w'][l])
        shared[f"mod_b_{l}"] = np.ascontiguousarray(inp['mod_b'][l][None])
        shared[f"nw_{l}"] = np.ascontiguousarray(inp['norm1_w'][l][None])
        shared[f"w_out_{l}"] = np.ascontiguousarray(inp['w_out'][l][perm_o])
        shared[f"nw2_{l}"] = np.ascontiguousarray(inp['norm2_w'][l][None])
        for h in range(2):
            d = per_h[h]
            d[f"w_in_{l}"] = np.ascontiguousarray(inp['w_in'][l][:, wcols(h)])
            for k, v in layer_params(inp, l, h).items():
                d[f"{k}_{l}"] = v
            ecols = np.concatenate([np.arange(8 * h, 8 * h + 8), np.arange(8 * (1 - h), 8 * (1 - h) + 8)])
            d[f"router_w_{l}"] = np.ascontiguousarray(inp['router_w'][l][:, ecols])
            d[f"wg_{l}"] = np.ascontiguousarray(inp['exp_w_gate'][l][8 * h:8 * h + 8])
            d[f"wu_{l}"] = np.ascontiguousarray(inp['exp_w_up'][l][8 * h:8 * h + 8])
            d[f"wd_{l}"] = np.ascontiguousarray(inp['exp_w_down'][l][8 * h:8 * h + 8])
    fnw = np.ascontiguousarray(inp['final_norm_w'][None])
    in_maps = []
    for (b, h) in cores:
        m = {"xin": np.concatenate([inp['ctx'][b], inp['x'][b]], 0),
             "c2": np.ascontiguousarray(np.stack([inp['c'][b], inp['c_ctx']])), "fnw": fnw, **C, **shared, **per_h[h]}
        in_maps.append(m)
    res = run_bass_kernel_spmd(nc, in_maps, core_ids=list(range(8)))
    return np.stack([np.asarray(res.results[2 * b]["yout"]) for b in range(B)], 0).astype(np.float32)
```

```python
import numpy as np
import concourse.bass as bass
import concourse.mybir as mybir
from concourse.bass_utils import run_bass_kernel_spmd

F32 = mybir.dt.float32
BF16 = mybir.dt.bfloat16
ALU = mybir.AluOpType
AF = mybir.ActivationFunctionType
AX = mybir.AxisListType

SEM_LIMIT = 24000


class Buf:
    def __init__(self, t, name):
        self.t = t
        self.name = name
        self.last_w = None
        self.readers = []
        self.dma_sem = None
        self.is_dram = False
        self.is_psum = False
        self.slot = None

    def __getitem__(self, idx):
        return View(self, self.t[idx])

    def sub(self, key):
        return Buf(self.t, f"{self.name}.{key}")

    def v(self, ap):
        return View(self, ap)


class View:
    def __init__(self, buf, ap):
        self.buf = buf
        self.ap = ap


class Op:
    __slots__ = ("eng", "fn", "waits", "inc", "idx", "is_dma", "dma_buf", "dma_val", "strict")

    def __init__(self, eng, fn):
        self.eng = eng
        self.fn = fn
        self.waits = []
        self.inc = False
        self.idx = None
        self.is_dma = False
        self.dma_buf = None
        self.dma_val = None
        self.strict = False


ENGS = ("pe", "act", "dve", "pool", "sp")


class _Scope:
    def __init__(self, S):
        self.S = S

    def __enter__(self):
        from contextlib import ExitStack
        self.old = self.S.stack
        self.S.scope_bufs.append([])
        self.st = ExitStack()
        self.st.__enter__()
        self.S.stack = self.st
        return self

    def __exit__(self, *a):
        self.S.barrier()
        for b in self.S.scope_bufs.pop():
            if b.slot is not None:
                for e_, sl in b.slot.items():
                    self.S.free_slots_e.setdefault(e_, []).append(sl)
                b.slot = None
        self.S.stack = self.old
        return self.st.__exit__(*a)


class Sched:
    def __init__(self, nc, stack):
        self.nc = nc
        self.stack = stack
        self.ops = {e: [] for e in ENGS}
        self.all_ops = []
        self.nbuf = 0
        self.dma_counts = {}
        self._bar_pos = 0
        self._bar = {}
        self.free_slots = []
        self.free_slots_e = {}
        self.nslots = 0
        self.scope_bufs = [[]]

    def sbuf(self, shape, dt=F32, name=None):
        self.nbuf += 1
        name = f"{name}_{self.nbuf}" if name else f"sb{self.nbuf}"
        t = self.stack.enter_context(self.nc.sbuf_tensor(name, list(shape), dt))
        b = Buf(t, name)
        self.scope_bufs[-1].append(b)
        return b

    def psum(self, shape, dt=F32, name=None):
        self.nbuf += 1
        name = name or f"ps{self.nbuf}"
        t = self.stack.enter_context(self.nc.psum_tensor(name, list(shape), dt))
        b = Buf(t, name)
        b.is_psum = True
        return b

    def scope(self):
        return _Scope(self)

    def dram(self, name, shape, dt=F32, kind="Internal"):
        t = self.nc.dram_tensor(name, list(shape), dt, kind=kind)
        b = Buf(t.ap(), name)
        b.is_dram = True
        return b

    def barrier(self):
        lasts = []
        for e in ENGS:
            if e in ("sp", "pool"):
                continue
            if self.ops[e]:
                lasts.append(self.ops[e][-1])
        dmas = [o for o in self.all_ops[self._bar_pos:] if o.is_dma]
        self._bar_pos = len(self.all_ops)
        lastd = {}
        for o in dmas:
            lastd[o.dma_val] = o
        for e in ("sp", "pool"):
            nd = [o for o in self.ops[e] if not o.is_dma]
            if nd:
                lasts.append(nd[-1])
        self._bar = {e: lasts + list(lastd.values()) for e in ENGS}

    def _dep(self, op, reads, writes):
        for b in reads:
            w = b.last_w
            if w is not None:
                if not (w.eng == "pe" and op.eng == "pe"):
                    op.waits.append(w)
            if b.is_psum:
                for r in b.readers:
                    if r.eng != op.eng:
                        op.waits.append(r)
            b.readers.append(op)
        for b in writes:
            w = b.last_w
            if w is not None and not (w.is_dma and op.is_dma and w.eng == op.eng and not op.strict) \
                    and not (w.eng == "pe" and op.eng == "pe"):
                op.waits.append(w)
            for r in b.readers:
                if r is op:
                    continue
                if not (r.eng == "pe" and op.eng == "pe"):
                    op.waits.append(r)
            b.last_w = op
            b.readers = []

    def I(self, eng, meth, *args, r=(), w=(), **kw):
        reads = list(r)
        writes = list(w)
        kw2 = {}
        for k, val in kw.items():
            if isinstance(val, View):
                if k in ("out", "accum_out"):
                    writes.append(val.buf)
                else:
                    reads.append(val.buf)
                kw2[k] = val.ap
            else:
                kw2[k] = val
        is_dma = meth in ("dma_start", "collective_compute", "indirect_dma_start")
        op = Op(eng, None)
        op.is_dma = is_dma
        op.strict = meth == "indirect_dma_start"
        if is_dma:
            sb = None
            if meth == "collective_compute":
                sb = writes[0]
            if meth == "indirect_dma_start":
                sb = writes[0] if not writes[0].is_dram else reads[0]
            for k in ("in_", "out"):
                if isinstance(kw.get(k), View):
                    if sb is None or not kw[k].buf.is_dram:
                        sb = kw[k].buf
            if sb.slot is None:
                sb.slot = {}
            if eng not in sb.slot:
                fl = self.free_slots_e.setdefault(eng, [])
                if fl:
                    sb.slot[eng] = fl.pop()
                else:
                    sb.slot[eng] = self.nslots
                    self.nslots += 1
            op.dma_buf = sb
            op.dma_val = sb.slot[eng]
        self._dep(op, reads, writes)
        if self._bar.get(eng):
            op.waits.extend(o for o in self._bar.pop(eng) if o is not op)
        op.fn = (meth, args, kw2)
        self.ops[eng].append(op)
        self.all_ops.append(op)
        return op

    def pe(self, meth, **kw):
        return self.I("pe", meth, **kw)

    def act(self, meth, **kw):
        return self.I("act", meth, **kw)

    def dve(self, meth, **kw):
        return self.I("dve", meth, **kw)

    def pool(self, meth, **kw):
        return self.I("pool", meth, **kw)

    def dma(self, out, in_, eng="sp", **kw):
        return self.I(eng, "dma_start", out=out, in_=in_, **kw)

    def emit(self, final_waits=()):
        nc = self.nc
        for op in self.all_ops:
            for wop in op.waits:
                wop.inc = True
        for op in final_waits:
            op.inc = True
        for op in self.all_ops:
            if op.is_dma:
                op.inc = True
        eng_cnt = {e: 0 for e in ENGS}
        slot_state = {}
        for op in self.all_ops:
            if not op.inc:
                continue
            if op.is_dma:
                b = op.dma_val
                stt = slot_state.setdefault(b, [0, 0])
                iv = 1 if op.fn[0] == "collective_compute" else 16
                if stt[1] + iv > SEM_LIMIT:
                    stt[0] += 1
                    stt[1] = 0
                stt[1] += iv
                op.idx = (stt[0], stt[1], iv)
            else:
                eng_cnt[op.eng] += 1
                op.idx = eng_cnt[op.eng]
        self.eng_sems = {}
        for e in ENGS:
            n = (eng_cnt[e] + SEM_LIMIT - 1) // SEM_LIMIT
            self.eng_sems[e] = [
                self.stack.enter_context(nc.semaphore(f"s_{e}_{i}")) for i in range(n)
            ]
        self.dma_sems = {}
        for b, stt in slot_state.items():
            self.dma_sems[b] = [
                self.stack.enter_context(nc.semaphore(f"d_slot{b}_{i}"))
                for i in range(stt[0] + 1)
            ]
        nsem = sum(len(v) for v in self.eng_sems.values()) + sum(
            len(v) for v in self.dma_sems.values())
        self.nsem = nsem

        def sem_of(op):
            if op.is_dma:
                k, val, iv = op.idx
                return self.dma_sems[op.dma_val][k], val
            k = (op.idx - 1) // SEM_LIMIT
            return self.eng_sems[op.eng][k], op.idx - k * SEM_LIMIT

        block = self.stack.enter_context(nc.Block())
        engmap = {"pe": block.tensor, "act": block.scalar, "dve": block.vector,
                  "pool": block.gpsimd, "sp": block.sync}

        def make(ename):
            ops = self.ops[ename]
            fw = [o for o in final_waits]

            def body(eng):
                known = {}
                for op in ops:
                    need = {}
                    for wop in op.waits:
                        sem, val = sem_of(wop)
                        key = id(sem)
                        if key not in need or need[key][1] < val:
                            need[key] = (sem, val)
                    for key, (sem, val) in need.items():
                        if known.get(key, 0) >= val:
                            continue
                        known[key] = val
                        eng.wait_ge(sem, val)
                    meth, args, kw = op.fn
                    ins = getattr(eng, meth)(*args, **kw)
                    if op.inc:
                        sem, val = sem_of(op)
                        ins.then_inc(sem, op.idx[2] if op.is_dma else 1)
                if ename == "sp":
                    need = {}
                    for wop in fw:
                        sem, val = sem_of(wop)
                        key = id(sem)
                        if key not in need or need[key][1] < val:
                            need[key] = (sem, val)
                    for key, (sem, val) in need.items():
                        eng.wait_ge(sem, val)
            return body

        for e in ENGS:
            if self.ops[e] or (e == "sp" and final_waits):
                engmap[e](make(e))


from contextlib import ExitStack

T = 4352
NT = 34
D = 2048
KC = 16
NCTX = 256
FAM = [('gla_q', 128), ('gla_k', 128), ('gla_v', 256), ('gla_g', 256), ('gla_lr', 32),
       ('dn_q', 256), ('dn_k', 256), ('dn_v', 256), ('dn_g', 256), ('dn_a', 4), ('dn_b', 4),
       ('gqa_q', 256), ('gqa_k', 128), ('gqa_v', 128),
       ('diff_q', 256), ('diff_k', 256), ('diff_v', 256)]
ZOFF = {}
_o = 0
for _n, _w in FAM:
    ZOFF[_n] = (_o, _w)
    _o += _w
ZC = _o


def wcols(h):
    r = np.arange
    c = []
    c += list(r(0 + 128 * h, 0 + 128 * h + 128))
    c += list(r(256 + 128 * h, 256 + 128 * h + 128))
    c += list(r(512 + 256 * h, 512 + 256 * h + 256))
    c += list(r(1024 + 256 * h, 1024 + 256 * h + 256))
    c += list(r(1536, 1568))
    c += list(r(1568 + 256 * h, 1568 + 256 * h + 256))
    c += list(r(1568 + 512 + 256 * h, 1568 + 512 + 256 * h + 256))
    c += list(r(1568 + 1024 + 256 * h, 1568 + 1024 + 256 * h + 256))
    c += list(r(3104 + 256 * h, 3104 + 256 * h + 256))
    c += [3616 + d * 4 + 2 * h + j for d in range(2) for j in range(2)]
    c += [3624 + d * 4 + 2 * h + j for d in range(2) for j in range(2)]
    c += list(r(3632 + 256 * h, 3632 + 256 * h + 256))
    c += list(r(4144 + 128 * h, 4144 + 128 * h + 128))
    c += list(r(4144 + 256 + 128 * h, 4144 + 256 + 128 * h + 128))
    c += list(r(4656 + 256 * h, 4656 + 256 * h + 256))
    c += list(r(5168 + 256 * h, 5168 + 256 * h + 256))
    c += list(r(5680 + 256 * h, 5680 + 256 * h + 256))
    assert len(c) == ZC
    return np.array(c)


def bc(ap, n=128):
    return ap.partition_broadcast(n)


def phase_mod(S, c2, mod_w, mod_b, modv):
    with S.scope():
        cT = S.sbuf([128, 16, 2])
        for r in range(2):
            S.dma(out=cT[:, :, r:r + 1], in_=c2.v(c2.t[r:r + 1, :].rearrange("r (k p) -> p k r", p=128)),
                  allow_slow_non_contiguous=True)
        sT = S.sbuf([128, 16, 2])
        S.act("activation", out=sT[:], in_=cT[:], func=AF.Silu)
        mb = S.sbuf([2, 12288])
        S.dma(out=mb[:], in_=mod_b.v(bc(mod_b.t[0:1, :], 2)))
        mv = S.sbuf([2, 12288])
        wr = [S.sbuf([128, 16, 512]) for _ in range(2)]
        ps = [S.psum([2, 512]) for _ in range(2)]
        mwv = mod_w.t.rearrange("(k p) n -> p k n", p=128)
        for ct in range(24):
            wb = wr[ct % 2]
            cs = slice(ct * 512, (ct + 1) * 512)
            S.dma(out=wb[:, 0:8, :], in_=mod_w.v(mwv[:, 0:8, cs]))
            S.dma(out=wb[:, 8:16, :], in_=mod_w.v(mwv[:, 8:16, cs]))
            for k in range(16):
                S.pe("matmul", out=ps[ct % 2][:], lhsT=sT[:, k, :], rhs=wb[:, k, :],
                     start=(k == 0), stop=(k == 15))
            S.dve("tensor_tensor", out=mv[:, cs], in0=ps[ct % 2][:], in1=mb[:, cs], op=ALU.add)
        S.dma(out=modv.v(modv.t), in_=mv[:], eng="pool")


def load_w_bf16(S, Wb, w_dram, stages, nk, ncols):
    for k in range(nk):
        st = stages[k % len(stages)]
        S.dma(out=st[:, :ncols], in_=w_dram.v(w_dram.t[k * 128:(k + 1) * 128, :]))
        S.I("dve" if k % 2 == 0 else "pool", "tensor_copy", out=Wb[:, k, :], in_=st[:, :ncols])


def rsqrt_col(S, out, in_, scale, eps):
    S.dve("tensor_scalar", out=out[:], in0=in_[:], scalar1=scale, scalar2=eps,
          op0=ALU.mult, op1=ALU.add)
    S.act("activation", out=out[:], in_=out[:], func=AF.Sqrt)
    S.dve("reciprocal", out=out[:], in_=out[:])


def rms_mod(S, x, A, Bv, hb, junk, ssq, rstd, width=2048):
    S.act("activation", out=junk[:], in_=x[:], func=AF.Square, accum_out=ssq[:])
    rsqrt_col(S, rstd, ssq, 1.0 / width, 1e-6)
    S.dve("scalar_tensor_tensor", out=hb[:], in0=x[:], scalar=rstd[:], in1=A[:],
          op0=ALU.mult, op1=ALU.mult)
    S.pool("tensor_tensor", out=hb[:], in0=hb[:], in1=Bv[:], op=ALU.add)


def transpose_tile(S, src, dstT, idt, pst, nk=16, base=0):
    ng = (nk + 3) // 4
    for g in range(ng):
        p = pst[(base + g) % len(pst)]
        n = min(4, nk - g * 4)
        for j in range(n):
            k = g * 4 + j
            S.pe("transpose", out=p[:, j * 128:(j + 1) * 128], in_=src[:, k * 128:(k + 1) * 128],
                 identity=idt[:])
        eng = "act" if g % 2 else "dve"
        dv = dstT.v(dstT.t[:, g * 4:g * 4 + n, :].rearrange("p a b -> p (a b)"))
        if eng == "act":
            S.act("activation", out=dv, in_=p[:, :n * 128], func=AF.Copy)
        else:
            S.dve("tensor_copy", out=dv, in_=p[:, :n * 128])


def phase_in(S, xin, modv, nw, w_in, z, ident):
    with S.scope():
        idt = S.sbuf([128, 128])
        S.dma(out=idt[:], in_=ident.v(ident.t))
        Wb = S.sbuf([128, 16, ZC], BF16)
        zst = [S.sbuf([128, ZC]) for _ in range(2)]
        load_w_bf16(S, Wb, w_in, zst, 16, ZC)
        nwb = S.sbuf([128, 2048])
        S.dma(out=nwb[:], in_=nw.v(bc(nw.t[0:1, :])))
        A = S.sbuf([128, 2048])
        Bv = S.sbuf([128, 2048])
        xr = [S.sbuf([128, 2048]) for _ in range(2)]
        hb = S.sbuf([128, 2048])
        junk = S.sbuf([128, 2048], BF16)
        hT = [S.sbuf([128, 16, 128], BF16) for _ in range(2)]
        ssq = S.sbuf([128, 1])
        rstd = S.sbuf([128, 1])
        pst = [S.psum([128, 512]) for _ in range(2)]
        psz = [S.psum([128, 512]) for _ in range(3)]
        for t in range(NT):
            if t == 0 or t == 2:
                r = 1 if t == 0 else 0
                S.dma(out=A[:], in_=modv.v(bc(modv.t[r:r + 1, 2048:4096])))
                S.dma(out=Bv[:], in_=modv.v(bc(modv.t[r:r + 1, 0:2048])))
                S.dve("scalar_tensor_tensor", out=A[:], in0=A[:], scalar=1.0, in1=nwb[:],
                      op0=ALU.add, op1=ALU.mult)
            x = xr[t % 2]
            S.dma(out=x[:], in_=xin.v(xin.t[t * 128:(t + 1) * 128, :]))
            rms_mod(S, x, A, Bv, hb, junk, ssq, rstd)
            transpose_tile(S, hb, hT[t % 2], idt, pst)
            zs = zst[t % 2]
            for ct in range((ZC + 511) // 512):
                w = min(512, ZC - ct * 512)
                p = psz[ct % 3]
                for k in range(16):
                    S.pe("matmul", out=p[:, :w], lhsT=hT[t % 2][:, k, :],
                         rhs=Wb[:, k, ct * 512:ct * 512 + w], start=(k == 0), stop=(k == 15))
                if ct % 2:
                    S.act("activation", out=zs[:, ct * 512:ct * 512 + w], in_=p[:, :w], func=AF.Copy)
                else:
                    S.dve("tensor_copy", out=zs[:, ct * 512:ct * 512 + w], in_=p[:, :w])
            S.dma(out=z.v(z.t[t * 128:(t + 1) * 128, :]), in_=zs[:], eng="pool")


def rope_tables():
    out = {}
    t = np.arange(4096)
    pos = {0: (t // 64).astype(np.float64), 1: (t % 64).astype(np.float64)}
    for d in (128, 64):
        half = d // 2
        nf = half // 2
        inv = 10000.0 ** (-np.arange(0, half, 2, dtype=np.float64) / half)
        C = np.ones((T, d), np.float32)
        Sg = np.zeros((T, d), np.float32)
        for ax in range(2):
            ang = (pos[ax][:, None].astype(np.float32) * inv[None, :].astype(np.float32)).astype(np.float32)
            cs, sn = np.cos(ang), np.sin(ang)
            o = ax * half
            C[NCTX:, o:o + nf] = cs
            C[NCTX:, o + nf:o + 2 * nf] = cs
            Sg[NCTX:, o:o + nf] = -sn
            Sg[NCTX:, o + nf:o + 2 * nf] = sn
        out[f'ropeC{d}'] = C
        out[f'ropeS{d}'] = Sg
    return out


def rope_apply(S, x, xo, t1, Ct, St, nvec, d, c0):
    nf = d // 4
    cs = slice(c0, c0 + nvec * d)
    v3 = lambda b: b.v(b.t[:, cs].rearrange("p (v d) -> p v d", d=d))
    cb = Ct.v(Ct.t[:, :].unsqueeze(1).to_broadcast([128, nvec, d]))
    S.dve("tensor_tensor", out=v3(t1), in0=v3(x), in1=cb, op=ALU.mult)
    v4 = lambda b: b.t[:, cs].rearrange("p (v a h f) -> p v a h f", a=2, h=2, f=nf)
    s4 = St.t[:, :].rearrange("p (a h f) -> p a h f", a=2, h=2)
    for hf in range(2):
        sb = St.v(s4[:, :, hf, :].unsqueeze(1).to_broadcast([128, nvec, 2, nf]))
        S.dve("tensor_tensor", out=xo.v(v4(xo)[:, :, :, hf, :]), in0=x.v(v4(x)[:, :, :, 1 - hf, :]),
               in1=sb, op=ALU.mult)
    S.dve("tensor_tensor", out=xo[:, cs], in0=xo[:, cs], in1=t1[:, cs], op=ALU.add)


DEBUG = {}


def phase_attn(S, z, consts, prm, ocat, need_ctx=True):
    gq0 = ZOFF['gqa_q'][0]
    df0 = ZOFF['diff_q'][0]
    with S.scope():
        idt = S.sbuf([128, 128])
        S.dma(out=idt[:], in_=consts['ident'].v(consts['ident'].t))
        ones = S.sbuf([128, 128], BF16)
        S.dve("memset", out=ones[:], constant=1.0) if False else S.I("dve", "memset", ones.t[:], 1.0, w=[ones])
        wn = S.sbuf([128, 3, 128])
        S.dma(out=wn[:, 0, :], in_=prm['gqa_qn'].v(bc(prm['gqa_qn'].t[0:1, :])))
        S.dma(out=wn[:, 1, :], in_=prm['gqa_qn'].v(bc(prm['gqa_qn'].t[0:1, :])))
        S.dma(out=wn[:, 2, :], in_=prm['gqa_kn'].v(bc(prm['gqa_kn'].t[0:1, :])))
        dnw = S.sbuf([128, 128])
        S.dma(out=dnw[:], in_=prm['diff_nw'].v(bc(prm['diff_nw'].t[0:1, :])))
        lamc = S.sbuf([128, 2])
        S.dma(out=lamc[:], in_=prm['lamc'].v(bc(prm['lamc'].t[0:1, :])))
        S.dve("tensor_scalar", out=dnw[:], in0=dnw[:], scalar1=lamc[:, 0:1], scalar2=None, op0=ALU.mult)
        lamt = S.sbuf([128, 4, 64])
        S.dma(out=lamt.v(lamt.t[:, :, :].rearrange("p a b -> p (a b)")),
              in_=prm['diff_lambda'].v(bc(prm['diff_lambda'].t[0:1, :])))
        lj = S.sbuf([128, 2, 64])
        lam2 = S.sbuf([128, 2])
        S.dve("tensor_tensor", out=lj[:, 0, :], in0=lamt[:, 0, :], in1=lamt[:, 1, :], op=ALU.mult)
        S.dve("tensor_tensor", out=lj[:, 1, :], in0=lamt[:, 2, :], in1=lamt[:, 3, :], op=ALU.mult)
        S.dve("tensor_reduce", out=lam2[:], in_=lj[:], axis=AX.X, op=ALU.add)
        S.act("activation", out=lam2[:], in_=lam2[:], func=AF.Exp)
        nlam = S.sbuf([128, 1])
        S.dve("tensor_tensor", out=nlam[:], in0=lam2[:, 1:2], in1=lam2[:, 0:1], op=ALU.subtract)
        S.dve("tensor_scalar", out=nlam[:], in0=nlam[:], scalar1=lamc[:, 1:2], scalar2=None, op0=ALU.add)

        if DEBUG.get('setup_only'):
            S.dma(out=ocat.v(ocat.t[0:128, 0:384]), in_=wn.v(wn.t[:, :, :].rearrange("p a b -> p (a b)")), eng="pool")
            S.dma(out=ocat.v(ocat.t[0:128, 384:386]), in_=lam2[:, 0:2], eng="pool")
            S.dma(out=ocat.v(ocat.t[0:128, 512:640]), in_=dnw[:], eng="pool")
            return
        gqT = [S.sbuf([128, T], BF16) for _ in range(2)]
        gkT = S.sbuf([128, T], BF16)
        gv = S.sbuf([128, NT, 128], BF16)
        dqT = [S.sbuf([128, T], BF16) for _ in range(2)]
        dkT = [S.sbuf([128, T], BF16) for _ in range(2)]
        dv = S.sbuf([128, NT, 256], BF16)

        pst = [S.psum([128, 512]) for _ in range(2)]
        pS = [S.psum([128, 512]) for _ in range(2)]
        pO = [S.psum([128, 512]) for _ in range(2)]
        pR = [S.psum([128, 512]) for _ in range(2)]

        with S.scope():
            zin = [S.sbuf([128, 512 + 768]) for _ in range(2)]
            xo = [S.sbuf([128, 512 + 768]) for _ in range(2)]
            t1 = S.sbuf([128, 512 + 768])
            C128 = [S.sbuf([128, 128]) for _ in range(2)]
            S128 = [S.sbuf([128, 128]) for _ in range(2)]
            C64 = [S.sbuf([128, 64]) for _ in range(2)]
            S64 = [S.sbuf([128, 64]) for _ in range(2)]
            sq = S.sbuf([128, 384])
            ss = S.sbuf([128, 3])
            for t in range(DEBUG.get('ntl', NT)):
                rs_ = slice(t * 128, (t + 1) * 128)
                zi = zin[t % 2]
                x2 = xo[t % 2]
                S.dma(out=zi[:, 0:512], in_=z.v(z.t[rs_, gq0:gq0 + 512]))
                S.dma(out=zi[:, 512:1280], in_=z.v(z.t[rs_, df0:df0 + 768]))
                for nm, tl in (('ropeC128', C128), ('ropeS128', S128), ('ropeC64', C64), ('ropeS64', S64)):
                    S.dma(out=tl[t % 2][:], in_=consts[nm].v(consts[nm].t[rs_, :]))
                stop = DEBUG.get('stop', 99)
                if stop == 1:
                    S.dma(out=ocat.v(ocat.t[rs_, 0:1024]), in_=zi[:, 0:1024], eng="pool")
                    S.dma(out=ocat.v(ocat.t[rs_, 0:128]), in_=C128[t % 2][:], eng="pool")
                    S.dma(out=ocat.v(ocat.t[rs_, 128:256]), in_=S128[t % 2][:], eng="pool")
                    S.dma(out=ocat.v(ocat.t[rs_, 256:320]), in_=C64[t % 2][:], eng="pool")
                    S.dma(out=ocat.v(ocat.t[rs_, 320:384]), in_=S64[t % 2][:], eng="pool")
                    continue
                if DEBUG.get('skip_norm'):
                    pass
                else:
                  S.dve("tensor_tensor", out=sq[:], in0=zi[:, 0:384], in1=zi[:, 0:384], op=ALU.mult)
                  S.dve("tensor_reduce", out=ss[:], in_=sq.v(sq.t[:, :].rearrange("p (v d) -> p v d", d=128)),
                      axis=AX.X, op=ALU.add)
                  rsqrt_col(S, ss, ss, 1.0 / 128, 1e-6)
                  z3 = zi.v(zi.t[:, 0:384].rearrange("p (v d) -> p v d", d=128))
                  if not DEBUG.get('skip_bc'):
                    S.dve("tensor_tensor", out=z3, in0=z3,
                      in1=ss.v(ss.t[:, :].unsqueeze(2).to_broadcast([128, 3, 128])), op=ALU.mult)
                  S.dve("tensor_tensor", out=z3, in0=z3, in1=wn[:, :, :], op=ALU.mult)
                if DEBUG.get('skip_rope'):
                    S.dve("tensor_copy", out=x2[:, 0:1024], in_=zi[:, 0:1024])
                else:
                    rope_apply(S, zi, x2, t1, C128[t % 2], S128[t % 2], 3, 128, 0)
                    rope_apply(S, zi, x2, t1, C64[t % 2], S64[t % 2], 8, 64, 512)
                if stop == 3:
                    S.dma(out=ocat.v(ocat.t[rs_, 0:384]), in_=x2[:, 0:384], eng="pool")
                    S.dma(out=ocat.v(ocat.t[rs_, 512:1024]), in_=x2[:, 512:1024], eng="pool")
                    continue
                em = DEBUG.get('em', 0)
                p = pst[0]
                for j in range(3):
                    S.pe("transpose", out=p[:, j * 128:(j + 1) * 128], in_=x2[:, j * 128:(j + 1) * 128], identity=idt[:])
                if em != 1:
                    S.act("activation", out=gqT[0][:, rs_], in_=p[:, 0:128], func=AF.Copy)
                    S.act("activation", out=gqT[1][:, rs_], in_=p[:, 128:256], func=AF.Copy)
                if em != 2:
                    S.act("activation", out=gkT[:, rs_], in_=p[:, 256:384], func=AF.Copy)
                p = pst[1]
                if em != 3:
                  for j in range(4):
                    S.pe("transpose", out=p[:, j * 128:(j + 1) * 128], in_=x2[:, 512 + j * 128:512 + (j + 1) * 128], identity=idt[:])
                  if em != 1:
                    S.dve("tensor_copy", out=dqT[0][:, rs_], in_=p[:, 0:128])
                    S.dve("tensor_copy", out=dqT[1][:, rs_], in_=p[:, 128:256])
                  if em != 2:
                    S.dve("tensor_copy", out=dkT[0][:, rs_], in_=p[:, 256:384])
                    S.dve("tensor_copy", out=dkT[1][:, rs_], in_=p[:, 384:512])
                if stop == 4:
                    S.dma(out=ocat.v(ocat.t[rs_, 0:384]), in_=x2[:, 0:384], eng="pool")
                    S.dma(out=ocat.v(ocat.t[rs_, 512:1024]), in_=x2[:, 512:1024], eng="pool")
                    continue
                S.pool("tensor_copy", out=gv[:, t, :], in_=zi[:, 384:512])
                S.pool("tensor_copy", out=dv[:, t, :], in_=zi[:, 1024:1280])
                if DEBUG.get('pre_only'):
                    S.dma(out=ocat.v(ocat.t[rs_, 0:384]), in_=x2[:, 0:384], eng="pool")
                    S.dma(out=ocat.v(ocat.t[rs_, 512:1024]), in_=x2[:, 512:1024], eng="pool")
        if DEBUG.get('pre_only'):
            return

        pT = [S.sbuf([128, 512], BF16) for _ in range(3)]
        oTs = [S.sbuf([128, 512]) for _ in range(2)]
        oT2 = [S.sbuf([128, 512]) for _ in range(2)]
        rinv = [S.sbuf([128, 512]) for _ in range(2)]
        otok = [S.sbuf([128, 4, 128]) for _ in range(2)]
        sq2 = S.sbuf([128, 4, 128])
        ss2 = S.sbuf([128, 4])
        S.I("dve", "memset", ss2.t[:], 1.0, w=[ss2])
        cnt = [0]

        def one_map(qT, kT, prow, vv, vcol, q0, nq, nkt, scale):
            i = cnt[0]
            cnt[0] += 1
            po, pr = pO[i % 2], pR[i % 2]

            def s_step(kt):
                ps = pS[kt % 2]
                S.pe("matmul", out=ps[:, :nq], lhsT=kT[prow, kt * 128:(kt + 1) * 128], rhs=qT[prow, q0:q0 + nq],
                     start=True, stop=True)
                pt = pT[kt % 3]
                S.act("activation", out=pt[:, :nq], in_=ps[:, :nq], func=AF.Exp, scale=scale)

            def pv_step(kt):
                pt = pT[kt % 3]
                S.pe("matmul", out=po[:, :nq], lhsT=vv[:, kt, vcol], rhs=pt[:, :nq], start=(kt == 0), stop=(kt == nkt - 1))
                S.pe("matmul", out=pr[:, :nq], lhsT=ones[:, :], rhs=pt[:, :nq], start=(kt == 0), stop=(kt == nkt - 1))

            s_step(0)
            for kt in range(nkt):
                if kt + 1 < nkt:
                    s_step(kt + 1)
                pv_step(kt)
            return po, pr

        blocks = ([(0, 256, 2)] if need_ctx else []) + [(NCTX + i * 512, 512, NT) for i in range(8)]
        bi = 0
        for (q0, nq, nkt) in blocks:
            for hd in range(2):
                po, pr = one_map(gqT[hd], gkT, slice(0, 128), gv, slice(0, 128), q0, nq, nkt, 128 ** -0.5)
                ri = rinv[bi % 2]
                ot = oTs[bi % 2]
                S.dve("reciprocal", out=ri[:, :nq], in_=pr[:, :nq])
                S.dve("tensor_tensor", out=ot[:, :nq], in0=po[:, :nq], in1=ri[:, :nq], op=ALU.mult)
                ok = otok[bi % 2]
                p = pst[bi % 2]
                for j in range(nq // 128):
                    S.pe("transpose", out=p[:, j * 128:(j + 1) * 128], in_=ot[:, j * 128:(j + 1) * 128], identity=idt[:])
                S.act("activation", out=ok.v(ok.t[:, 0:nq // 128, :].rearrange("p a b -> p (a b)")), in_=p[:, :nq], func=AF.Copy)
                S.dma(out=ocat.v(ocat.t[q0:q0 + nq, 512 + hd * 128:512 + (hd + 1) * 128].rearrange("(a p) d -> p a d", p=128)),
                      in_=ok[:, 0:nq // 128, :], eng="pool")
                bi += 1
            for hd in range(2):
                po, pr = one_map(dqT[hd], dkT[hd], slice(0, 64), dv, slice(hd * 128, (hd + 1) * 128), q0, nq, nkt, 64 ** -0.5)
                ri = rinv[bi % 2]
                ot = oTs[bi % 2]
                S.dve("reciprocal", out=ri[:, :nq], in_=pr[:, :nq])
                S.dve("tensor_tensor", out=ot[:, :nq], in0=po[:, :nq], in1=ri[:, :nq], op=ALU.mult)
                po, pr = one_map(dqT[hd], dkT[hd], slice(64, 128), dv, slice(hd * 128, (hd + 1) * 128), q0, nq, nkt, 64 ** -0.5)
                o2 = oT2[bi % 2]
                S.dve("reciprocal", out=ri[:, :nq], in_=pr[:, :nq])
                S.dve("tensor_tensor", out=o2[:, :nq], in0=po[:, :nq], in1=ri[:, :nq], op=ALU.mult)
                S.dve("scalar_tensor_tensor", out=ot[:, :nq], in0=o2[:, :nq], scalar=nlam[:, 0:1], in1=ot[:, :nq],
                      op0=ALU.mult, op1=ALU.add)
                ok = otok[bi % 2]
                p = pst[bi % 2]
                na = nq // 128
                for j in range(na):
                    S.pe("transpose", out=p[:, j * 128:(j + 1) * 128], in_=ot[:, j * 128:(j + 1) * 128], identity=idt[:])
                okv = ok.v(ok.t[:, 0:na, :].rearrange("p a b -> p (a b)"))
                S.act("activation", out=okv, in_=p[:, :nq], func=AF.Copy)
                S.dve("tensor_tensor", out=sq2[:, 0:na, :], in0=ok[:, 0:na, :], in1=ok[:, 0:na, :], op=ALU.mult)
                S.dve("tensor_reduce", out=ss2[:, 0:na], in_=sq2[:, 0:na, :], axis=AX.X, op=ALU.add)
                rsqrt_col(S, ss2, ss2, 1.0 / 128, 1e-6)
                S.dve("tensor_tensor", out=ok[:, 0:na, :], in0=ok[:, 0:na, :],
                      in1=ss2.v(ss2.t[:, 0:na].unsqueeze(2).to_broadcast([128, na, 128])), op=ALU.mult)
                S.dve("tensor_tensor", out=ok[:, 0:na, :], in0=ok[:, 0:na, :],
                      in1=dnw.v(dnw.t[:, :].unsqueeze(1).to_broadcast([128, na, 128])), op=ALU.mult)
                S.dma(out=ocat.v(ocat.t[q0:q0 + nq, 768 + hd * 128:768 + (hd + 1) * 128].rearrange("(a p) d -> p a d", p=128)),
                      in_=ok[:, 0:na, :], eng="pool")
                bi += 1


def tri_consts():
    j = np.arange(128)[:, None]
    i = np.arange(128)[None, :]
    c = {}
    c['mincl_f'] = (j <= i).astype(np.float32)
    c['mincl_r'] = (j >= i).astype(np.float32)
    c['mstr_f'] = (j > i).astype(np.float32)
    c['mstr_r'] = (j < i).astype(np.float32)
    c['ones1'] = np.ones((1, 128), np.float32)
    hm = np.zeros((128, 2), np.float32); hm[:64, 0] = 1; hm[64:, 1] = 1
    c['hmask'] = hm
    c['ones128'] = np.ones((128, 128), np.float32)
    return c


def block_order(d):
    if d == 0:
        return list(range(NT))
    return [1, 0] + list(range(NT - 1, 1, -1))


def phase_gla(S, z, consts, prm, ocat):
    c0 = ZOFF['gla_q'][0]
    with S.scope():
        idt = S.sbuf([128, 128])
        S.dma(out=idt[:], in_=consts['ident'].v(consts['ident'].t))
        msk = {}
        for nm in ('mincl_f', 'mincl_r', 'mstr_f', 'mstr_r'):
            msk[nm] = S.sbuf([128, 128], name="gla_" + nm)
            S.dma(out=msk[nm][:], in_=consts[nm].v(consts[nm].t))
        mS = {}
        for nm in ('mincl_f', 'mincl_r', 'mstr_f', 'mstr_r'):
            mS[nm] = S.sbuf([128, 128], name="glaS_" + nm)
            S.dve("tensor_scalar", out=mS[nm][:], in0=msk[nm][:], scalar1=-1.0 / 16, scalar2=None, op0=ALU.mult)
        ones1 = S.sbuf([1, 128])
        S.dma(out=ones1[:], in_=consts['ones1'].v(consts['ones1'].t))
        w2p = [S.sbuf([32, 128]) for _ in range(2)]
        gb = [S.sbuf([1, 128]) for _ in range(2)]
        for d in range(2):
            S.dma(out=w2p[d][:], in_=prm[f'w2pad{d}'].v(prm[f'w2pad{d}'].t))
            S.dma(out=gb[d][:], in_=prm[f'gb{d}'].v(prm[f'gb{d}'].t))
        nwb = S.sbuf([128, 128])
        S.dma(out=nwb[:], in_=prm['gla_nw'].v(bc(prm['gla_nw'].t[0:1, :])))
        oacc = S.sbuf([128, NT, 256])
        Sst = S.sbuf([128, 128])
        R = 2
        zin = [S.sbuf([128, 800]) for _ in range(R)]
        lrT = [S.sbuf([32, 128]) for _ in range(R)]
        ee = [S.sbuf([128, 128]) for _ in range(R)]
        sp = [S.sbuf([128, 128]) for _ in range(R)]
        EbT = [S.sbuf([128, 128]) for _ in range(R)]
        EnbT = [S.sbuf([128, 128]) for _ in range(R)]
        Ebm = [S.sbuf([128, 128]) for _ in range(R)]
        qgT = [S.sbuf([128, 128]) for _ in range(R)]
        qgTh = [S.sbuf([128, 2, 128]) for _ in range(R)]
        kinvT = [S.sbuf([128, 2, 128]) for _ in range(R)]
        hm = S.sbuf([128, 2])
        S.dma(out=hm[:], in_=consts['hmask'].v(consts['hmask'].t))
        kd = [S.sbuf([128, 128]) for _ in range(R)]
        scm = [S.sbuf([128, 2, 128]) for _ in range(R)]
        osum = [S.sbuf([128, 2, 128]) for _ in range(R)]
        sg = [S.sbuf([128, 256]) for _ in range(R)]
        sq = S.sbuf([128, 2, 128])
        ss = S.sbuf([128, 2])
        pA, pB, pC, pD, pE, pF, pG, pH = [S.psum([128, 512]) for _ in range(8)]
        for d in range(2):
            sfx = '_f' if d == 0 else '_r'
            Mincl, Mstr, MinclS, MstrS = msk['mincl' + sfx], msk['mstr' + sfx], mS['mincl' + sfx], mS['mstr' + sfx]
            last = 127 if d == 0 else 0
            S.I("dve", "memset", Sst.t[:], 0.0, w=[Sst])
            for n, blk in enumerate(block_order(d)[:DEBUG.get('gnb', NT)]):
                i = n % R
                rs_ = slice(blk * 128, (blk + 1) * 128)
                zi = zin[i]
                S.dma(out=zi[:], in_=z.v(z.t[rs_, c0:c0 + 800]))
                q2, k2, v2, g2, lr = (zi[:, 0:128], zi[:, 128:256], zi[:, 256:512], zi[:, 512:768], zi[:, 768:800])
                S.pe("transpose", out=pA[0:32, 0:128], in_=lr, identity=idt[:])
                S.act("activation", out=lrT[i][:], in_=pA[0:32, 0:128], func=AF.Copy)
                if DEBUG.get('gstop') == 1:
                    continue
                S.pe("matmul", out=pB[:, 0:128], lhsT=lrT[i][:], rhs=w2p[d][:], start=True, stop=False)
                S.pe("matmul", out=pB[:, 0:128], lhsT=ones1[:], rhs=gb[d][:], start=False, stop=True)
                if DEBUG.get('gstop') == 2:
                    continue
                S.act("activation", out=ee[i][:], in_=pB[:, 0:128], func=AF.Exp, scale=-1.0)
                S.act("activation", out=sp[i][:], in_=ee[i][:], func=AF.Ln, bias=1.0)
                if DEBUG.get('gstop') == 3:
                    continue
                S.pe("matmul", out=pC[:, 0:128], lhsT=sp[i][:], rhs=MinclS[:], start=True, stop=True)
                S.pe("matmul", out=pD[:, 0:128], lhsT=MstrS[:], rhs=sp[i][:], start=True, stop=True)
                S.act("activation", out=EbT[i][:], in_=pC[:, 0:128], func=AF.Exp)
                S.act("activation", out=EnbT[i][:], in_=pC[:, 0:128], func=AF.Exp, scale=-1.0)
                S.act("activation", out=Ebm[i][:], in_=pD[:, 0:128], func=AF.Exp)
                if DEBUG.get('gstop') == 4:
                    continue
                S.pe("transpose", out=pE[:, 0:128], in_=q2, identity=idt[:])
                S.pe("transpose", out=pE[:, 128:256], in_=k2, identity=idt[:])
                S.dve("scalar_tensor_tensor", out=qgT[i][:], in0=pE[:, 0:128], scalar=0.125, in1=EbT[i][:],
                      op0=ALU.mult, op1=ALU.mult)
                for hh in range(2):
                    S.dve("scalar_tensor_tensor", out=kinvT[i][:, hh, :], in0=pE[:, 128:256], scalar=hm[:, hh:hh + 1],
                          in1=EnbT[i][:], op0=ALU.mult, op1=ALU.mult)
                    S.dve("tensor_scalar", out=qgTh[i][:, hh, :], in0=qgT[i][:], scalar1=hm[:, hh:hh + 1], scalar2=None,
                          op0=ALU.mult)
                S.dve("tensor_tensor", out=kd[i][:], in0=k2, in1=Ebm[i][:], op=ALU.mult)
                if DEBUG.get('gstop') == 6:
                    continue
                for hh in range(2):
                    r = slice(64 * hh, 64 * hh + 64)
                    S.pe("matmul", out=pF[:, hh * 128:(hh + 1) * 128], lhsT=kinvT[i][:, hh, :], rhs=qgT[i][:],
                         start=True, stop=True)
                S.dve("tensor_tensor", out=scm[i][:, :, :],
                      in0=pF.v(pF.t[:, 0:256].rearrange("p (a b) -> p a b", a=2)),
                      in1=Mincl.v(Mincl.t[:, :].unsqueeze(1).to_broadcast([128, 2, 128])), op=ALU.mult)
                if DEBUG.get('gstop') == 7:
                    continue
                for hh in range(2):
                    r = slice(64 * hh, 64 * hh + 64)
                    S.pe("matmul", out=pG[:, hh * 128:(hh + 1) * 128], lhsT=scm[i][:, hh, :],
                         rhs=zi[:, 256 + hh * 128:256 + (hh + 1) * 128], start=True, stop=False)
                    S.pe("matmul", out=pG[:, hh * 128:(hh + 1) * 128], lhsT=qgTh[i][:, hh, :], rhs=Sst[:],
                         start=False, stop=True)
                S.pe("matmul", out=pH[:, 0:256], lhsT=kd[i][:], rhs=v2, start=True, stop=True)
                if DEBUG.get('gstop') == 8:
                    continue
                for hh in range(2):
                    r = slice(64 * hh, 64 * hh + 64)
                    S.dve("scalar_tensor_tensor", out=Sst[r, :], in0=Sst[r, :], scalar=EbT[i][r, last:last + 1],
                          in1=pH[r, hh * 128:(hh + 1) * 128], op0=ALU.mult, op1=ALU.add)
                if d == 0:
                    S.act("activation", out=oacc[:, blk, :], in_=pG[:, 0:256], func=AF.Copy)
                else:
                    os_ = osum[i]
                    osv = os_.v(os_.t[:, :, :].rearrange("p a b -> p (a b)"))
                    S.dve("tensor_tensor", out=osv, in0=pG[:, 0:256], in1=oacc[:, blk, :], op=ALU.add)
                    S.act("activation", out=sg[i][:], in_=g2, func=AF.Silu)
                    S.dve("tensor_tensor", out=sq[:, :, :], in0=os_[:, :, :], in1=os_[:, :, :], op=ALU.mult)
                    S.dve("tensor_reduce", out=ss[:], in_=sq[:, :, :], axis=AX.X, op=ALU.add)
                    rsqrt_col(S, ss, ss, 1.0 / 128, 1e-6)
                    S.dve("tensor_tensor", out=os_[:, :, :], in0=os_[:, :, :],
                          in1=ss.v(ss.t[:, :].unsqueeze(2).to_broadcast([128, 2, 128])), op=ALU.mult)
                    S.dve("tensor_tensor", out=os_[:, :, :], in0=os_[:, :, :],
                          in1=nwb.v(nwb.t[:, :].unsqueeze(1).to_broadcast([128, 2, 128])), op=ALU.mult)
                    S.dve("tensor_tensor", out=osv, in0=osv, in1=sg[i][:], op=ALU.mult)
                    S.dma(out=ocat.v(ocat.t[rs_, 0:256]), in_=osv, eng="pool")
        if DEBUG.get('gstop'):
            S.dma(out=ocat.v(ocat.t[0:128, 0:128]), in_=nwb[:], eng="pool")


def dn_consts():
    p = np.arange(128)[:, None]
    f = np.arange(128)[None, :]
    same = (p // 64) == (f // 64)
    c = {}
    for d, le in (('f', lambda a, b: a <= b), ('r', lambda a, b: a >= b)):
        lt = (lambda a, b: a < b) if d == 'f' else (lambda a, b: a > b)
        c[f'dn_MTi_{d}'] = le(p, f).astype(np.float32)
        c[f'dn_MTSn_{d}'] = -(lt(p, f) & same).astype(np.float32)
        c[f'dn_MSn_{d}'] = -(lt(f, p) & same).astype(np.float32)
        c[f'dn_MSo_{d}'] = (lt(f, p) & ~same).astype(np.float32)
        c[f'dn_Mc_{d}'] = le(p, f).astype(np.float32)
    return c


def phase_dn_prep(S, z, prm, dnp):
    q0 = ZOFF['dn_q'][0]
    a0 = ZOFF['dn_a'][0]
    with S.scope():
        wk = S.sbuf([128, 5, 768])
        for k in range(5):
            S.dma(out=wk[:, k, :], in_=prm['dn_convw'].v(bc(prm['dn_convw'].t[k:k + 1, :])))
        nea = S.sbuf([128, 4])
        dtb = S.sbuf([128, 4])
        S.dma(out=nea[:], in_=prm['dn_alog'].v(bc(prm['dn_alog'].t[0:1, :])))
        S.dma(out=dtb[:], in_=prm['dn_dtb'].v(bc(prm['dn_dtb'].t[0:1, :])))
        S.act("activation", out=nea[:], in_=nea[:], func=AF.Exp)
        S.dve("tensor_scalar", out=nea[:], in0=nea[:], scalar1=-1.0, scalar2=None, op0=ALU.mult)
        xs = [[S.sbuf([128, 768]) for _ in range(5)] for _ in range(2)]
        ab = [S.sbuf([128, 8]) for _ in range(2)]
        tmp = [S.sbuf([128, 768]) for _ in range(2)]
        acc = S.sbuf([128, 768])
        yo = [S.sbuf([128, 776]) for _ in range(2)]
        sq = S.sbuf([128, 512])
        ss = S.sbuf([128, 4])
        e4 = S.sbuf([128, 4])
        e5 = S.sbuf([128, 4])
        for t in range(NT):
            t0 = t * 128
            lo, hi = (0, NCTX) if t < 2 else (NCTX, T)
            x5 = xs[t % 2]
            for k in range(5):
                s = k - 2
                a = max(t0 + s, lo)
                b = min(t0 + s + 128, hi)
                if a != t0 + s or b != t0 + s + 128:
                    S.I("dve", "memset", x5[k].t[:], 0.0, w=[x5[k]])
                S.dma(out=x5[k][a - (t0 + s):b - (t0 + s), :], in_=z.v(z.t[a:b, q0:q0 + 768]))
            S.dma(out=ab[t % 2][:], in_=z.v(z.t[t0:t0 + 128, a0:a0 + 8]))
            S.dve("tensor_tensor", out=acc[:], in0=x5[0][:], in1=wk[:, 0, :], op=ALU.mult)
            for k in range(1, 5):
                tm = tmp[k % 2]
                S.pool("tensor_tensor", out=tm[:], in0=x5[k][:], in1=wk[:, k, :], op=ALU.mult)
                S.dve("tensor_tensor", out=acc[:], in0=acc[:], in1=tm[:], op=ALU.add)
            y = yo[t % 2]
            S.act("activation", out=y[:, 0:768], in_=acc[:], func=AF.Silu)
            S.dve("tensor_tensor", out=sq[:], in0=y[:, 0:512], in1=y[:, 0:512], op=ALU.mult)
            S.dve("tensor_reduce", out=ss[:], in_=sq.v(sq.t[:, :].rearrange("p (v d) -> p v d", d=128)),
                  axis=AX.X, op=ALU.add)
            rsqrt_col(S, ss, ss, 1.0, 1e-6)
            S.dve("tensor_scalar", out=ss[:, 0:2], in0=ss[:, 0:2], scalar1=128 ** -0.5, scalar2=None, op0=ALU.mult)
            y3 = y.v(y.t[:, 0:512].rearrange("p (v d) -> p v d", d=128))
            S.dve("tensor_tensor", out=y3, in0=y3, in1=ss.v(ss.t[:, :].unsqueeze(2).to_broadcast([128, 4, 128])),
                  op=ALU.mult)
            S.dve("tensor_tensor", out=e4[:], in0=ab[t % 2][:, 0:4], in1=dtb[:], op=ALU.add)
            S.act("activation", out=e4[:], in_=e4[:], func=AF.Exp)
            S.act("activation", out=e4[:], in_=e4[:], func=AF.Ln, bias=1.0)
            S.dve("tensor_tensor", out=y[:, 768:772], in0=e4[:], in1=nea[:], op=ALU.mult)
            S.act("activation", out=e5[:], in_=ab[t % 2][:, 4:8], func=AF.Exp, scale=-1.0)
            S.dve("tensor_scalar", out=e5[:], in0=e5[:], scalar1=1.0, scalar2=None, op0=ALU.add)
            S.dve("reciprocal", out=y[:, 772:776], in_=e5[:])
            S.dma(out=dnp.v(dnp.t[t0:t0 + 128, :]), in_=y[:], eng="pool")


def phase_dn(S, z, dnp, consts, prm, ocat):
    g0 = ZOFF['dn_g'][0]
    with S.scope():
        idt = S.sbuf([128, 128])
        S.dma(out=idt[:], in_=consts['ident'].v(consts['ident'].t))
        ones = S.sbuf([128, 128])
        S.dma(out=ones[:], in_=consts['ones128'].v(consts['ones128'].t))
        M = {}
        for d in ('f', 'r'):
            for nm in ('MTi', 'MTSn', 'MSn', 'MSo', 'Mc'):
                key = f'dn_{nm}_{d}'
                M[key] = S.sbuf([128, 128], name='sb_' + key)
                S.dma(out=M[key][:], in_=consts[key].v(consts[key].t))
        nwb = S.sbuf([128, 128])
        S.dma(out=nwb[:], in_=prm['dn_nw'].v(bc(prm['dn_nw'].t[0:1, :])))
        oacc = S.sbuf([128, NT, 256])
        Sst = [S.sbuf([128, 128], name=f"dnS{h_}") for h_ in range(2)]
        R = 2
        zin = [S.sbuf([128, 776]) for _ in range(R)]
        gin = [S.sbuf([128, 256]) for _ in range(R)]
        sc = [S.sbuf([128, 4]) for _ in range(R)]
        egc = [S.sbuf([128, 2]) for _ in range(R)]
        edl = [S.sbuf([128, 2]) for _ in range(R)]
        dl = [S.sbuf([128, 2]) for _ in range(R)]
        bgc = [S.sbuf([128, 2]) for _ in range(R)]
        tdl = [S.sbuf([128, 2]) for _ in range(R)]

        def mk(n=128):
            return [[S.sbuf([128, n]) for _ in range(2)] for _ in range(R)]
        DD, kT, qT, qgT, E3, t1, E1, t2, E2 = mk(256), mk(), mk(), mk(), mk(), mk(), mk(), mk(), mk()
        DTm, attnT, bm1, bm2, XT, e2a, X, e2b, Lo = mk(), mk(), mk(), mk(), mk(), mk(), mk(), mk(), mk()
        P_ = [[[S.sbuf([128, 128]) for _ in range(2)] for _ in range(2)] for _ in range(R)]
        PT = [[[S.sbuf([128, 128]) for _ in range(2)] for _ in range(2)] for _ in range(R)]
        Rm = [[[S.sbuf([128, 128]) for _ in range(2)] for _ in range(2)] for _ in range(R)]
        RT = [[[S.sbuf([128, 128]) for _ in range(2)] for _ in range(2)] for _ in range(R)]
        A1, TmT, vb, kbg, kd, usb, wT, vnew = mk(), mk(), mk(), mk(), mk(), mk(), mk(), mk()
        osum = [S.sbuf([128, 2, 128]) for _ in range(R)]
        sg = [S.sbuf([128, 256]) for _ in range(R)]
        sq = S.sbuf([128, 2, 128])
        ss = S.sbuf([128, 2])
        bA = [S.psum([128, 512]) for _ in range(2)]
        bB = [S.psum([128, 512]) for _ in range(2)]
        bC = [S.psum([128, 512]) for _ in range(2)]
        bD = [S.psum([128, 512]) for _ in range(2)]
        for d in range(2):
            dn = 'f' if d == 0 else 'r'
            MTi, MTSn, MSn, MSo, Mc = (M[f'dn_{nm}_{dn}'] for nm in ('MTi', 'MTSn', 'MSn', 'MSo', 'Mc'))
            for hh in range(2):
                S.I("dve", "memset", Sst[hh].t[:], 0.0, w=[Sst[hh]])
            for n, blk in enumerate(block_order(d)[:DEBUG.get('dnb', NT)]):
                i = n % R
                rs_ = slice(blk * 128, (blk + 1) * 128)
                zi = zin[i]
                S.dma(out=zi[:], in_=dnp.v(dnp.t[rs_, :]))
                if d == 1:
                    S.dma(out=gin[i][:], in_=z.v(z.t[rs_, g0:g0 + 256]))
                g2 = zi[:, 768 + 2 * d:768 + 2 * d + 2]
                be = lambda hh: zi[:, 772 + 2 * d + hh:772 + 2 * d + hh + 1]
                S.pe("matmul", out=bD[0][:, 384:386], lhsT=Mc[:], rhs=g2, start=True, stop=True)
                S.pe("matmul", out=bD[0][:, 386:388], lhsT=ones[:], rhs=g2, start=True, stop=True)
                S.dve("tensor_copy", out=sc[i][:], in_=bD[0][:, 384:388])
                S.act("activation", out=egc[i][:], in_=sc[i][:, 0:2], func=AF.Exp)
                S.dve("tensor_tensor", out=tdl[i][:], in0=sc[i][:, 2:4], in1=sc[i][:, 0:2], op=ALU.subtract)
                S.act("activation", out=edl[i][:], in_=tdl[i][:], func=AF.Exp)
                S.act("activation", out=dl[i][:], in_=sc[i][:, 2:4], func=AF.Exp)
                S.dve("tensor_tensor", out=bgc[i][:], in0=egc[i][:], in1=zi[:, 772 + 2 * d:772 + 2 * d + 2], op=ALU.mult)
                def head_steps(hh, i=i, zi=zi, be=be, blk=blk, d=d):
                    pRB = pT = bA[hh]
                    pK = pU = bB[hh]
                    pI = bC[hh]
                    pO = pS = bD[hh]
                    q_h = zi[:, hh * 128:(hh + 1) * 128]
                    k_h = zi[:, 256 + hh * 128:256 + (hh + 1) * 128]
                    v_h = zi[:, 512 + hh * 128:512 + (hh + 1) * 128]
                    gcc = sc[i][:, hh:hh + 1]
                    c2 = slice(0, 256)
                    ca = slice(0, 128)
                    cb = slice(128, 256)
                    ta = slice(256, 384)
                    tb = slice(384, 512)
                    S.dve("tensor_scalar", out=DD[i][hh][:, 0:128], in0=idt[:], scalar1=gcc, scalar2=None, op0=ALU.mult)
                    S.dve("tensor_scalar", out=DD[i][hh][:, 128:256], in0=idt[:], scalar1=be(hh), scalar2=None, op0=ALU.mult)
                    S.pe("matmul", out=pRB[:, c2], lhsT=ones[:], rhs=DD[i][hh][:], start=True, stop=True)
                    S.pe("transpose", out=pT[:, ta], in_=q_h, identity=idt[:])
                    S.pe("transpose", out=pT[:, tb], in_=k_h, identity=idt[:])
                    yield
                    S.dve("tensor_copy", out=qT[i][hh][:], in_=pT[:, ta])
                    S.dve("tensor_copy", out=kT[i][hh][:], in_=pT[:, tb])
                    S.act("activation", out=E3[i][hh][:], in_=pRB[:, ca], func=AF.Exp)
                    S.dve("tensor_tensor", out=qgT[i][hh][:], in0=qT[i][hh][:], in1=E3[i][hh][:], op=ALU.mult)
                    S.pe("matmul", out=pK[:, ca], lhsT=kT[i][hh][:], rhs=kT[i][hh][:], start=True, stop=True)
                    S.pe("matmul", out=pK[:, cb], lhsT=kT[i][hh][:], rhs=qT[i][hh][:], start=True, stop=True)
                    yield
                    S.dve("tensor_scalar", out=t1[i][hh][:], in0=pRB[:, ca], scalar1=gcc, scalar2=0.0,
                          op0=ALU.subtract, op1=ALU.min)
                    S.dve("tensor_scalar", out=t2[i][hh][:], in0=pRB[:, ca], scalar1=gcc, scalar2=0.0,
                          op0=ALU.subtract, op1=ALU.max)
                    S.dve("tensor_tensor", out=bm1[i][hh][:], in0=pRB[:, cb], in1=MTSn[:], op=ALU.mult)
                    S.act("activation", out=E1[i][hh][:], in_=t1[i][hh][:], func=AF.Exp)
                    S.act("activation", out=E2[i][hh][:], in_=t2[i][hh][:], func=AF.Exp, scale=-1.0)
                    yield
                    S.dve("tensor_tensor", out=DTm[i][hh][:], in0=E1[i][hh][:], in1=MTi[:], op=ALU.mult)
                    S.dve("tensor_tensor", out=attnT[i][hh][:], in0=pK[:, cb], in1=DTm[i][hh][:], op=ALU.mult)
                    S.dve("tensor_tensor", out=bm2[i][hh][:], in0=bm1[i][hh][:], in1=E1[i][hh][:], op=ALU.mult)
                    S.dve("tensor_tensor", out=XT[i][hh][:], in0=pK[:, ca], in1=bm2[i][hh][:], op=ALU.mult)
                    S.dve("scalar_tensor_tensor", out=e2a[i][hh][:], in0=E2[i][hh][:], scalar=be(hh), in1=MSn[:],
                          op0=ALU.mult, op1=ALU.mult)
                    S.dve("tensor_tensor", out=X[i][hh][:], in0=pK[:, ca], in1=e2a[i][hh][:], op=ALU.mult)
                    S.dve("scalar_tensor_tensor", out=e2b[i][hh][:], in0=E2[i][hh][:], scalar=be(hh), in1=MSo[:],
                          op0=ALU.mult, op1=ALU.mult)
                    S.dve("tensor_tensor", out=Lo[i][hh][:], in0=pK[:, ca], in1=e2b[i][hh][:], op=ALU.mult)
                    Pc, PTc, Rc, RTc = X[i][hh], XT[i][hh], Rm[i][hh][0], RT[i][hh][0]
                    S.dve("tensor_tensor", out=Rc[:], in0=X[i][hh][:], in1=idt[:], op=ALU.add)
                    S.dve("tensor_tensor", out=RTc[:], in0=XT[i][hh][:], in1=idt[:], op=ALU.add)
                    yield
                    for k in range(1, 6):
                        Pn, PTn = P_[i][hh][k % 2], PT[i][hh][k % 2]
                        Rn, RTn = Rm[i][hh][k % 2], RT[i][hh][k % 2]
                        S.pe("matmul", out=pI[:, 0:128], lhsT=PTc[:], rhs=Pc[:], start=True, stop=True)
                        S.pe("matmul", out=pI[:, 128:256], lhsT=Pc[:], rhs=PTc[:], start=True, stop=True)
                        S.act("activation", out=Pn[:], in_=pI[:, 0:128], func=AF.Copy)
                        S.act("activation", out=PTn[:], in_=pI[:, 128:256], func=AF.Copy)
                        yield
                        S.pe("matmul", out=pI[:, 256:384], lhsT=PTn[:], rhs=Rc[:], start=True, stop=True)
                        S.pe("matmul", out=pI[:, 384:512], lhsT=Pn[:], rhs=RTc[:], start=True, stop=True)
                        S.act("activation", out=Rn[:], in_=pI[:, 256:384], func=AF.Copy) if False else None
                        S.dve("tensor_tensor", out=Rn[:], in0=pI[:, 256:384], in1=Rc[:], op=ALU.add)
                        S.dve("tensor_tensor", out=RTn[:], in0=pI[:, 384:512], in1=RTc[:], op=ALU.add)
                        Pc, PTc, Rc, RTc = Pn, PTn, Rn, RTn
                        yield
                    Td, TdT = Rc, RTc
                    S.pe("matmul", out=pI[:, 0:128], lhsT=Lo[i][hh][:], rhs=TdT[:], start=True, stop=True)
                    yield
                    S.act("activation", out=A1[i][hh][:], in_=pI[:, 0:128], func=AF.Copy)
                    S.pe("matmul", out=pI[:, 128:256], lhsT=Td[:], rhs=A1[i][hh][:], start=True, stop=True)
                    S.dve("tensor_tensor", out=TmT[i][hh][:], in0=TdT[:], in1=pI[:, 128:256], op=ALU.subtract)
                    S.pool("tensor_scalar", out=vb[i][hh][:], in0=v_h, scalar1=be(hh), scalar2=None, op0=ALU.mult)
                    S.pool("tensor_scalar", out=kbg[i][hh][:], in0=k_h, scalar1=bgc[i][:, hh:hh + 1], scalar2=None, op0=ALU.mult)
                    S.pool("tensor_scalar", out=kd[i][hh][:], in0=k_h, scalar1=edl[i][:, hh:hh + 1], scalar2=None, op0=ALU.mult)
                    cu = slice(256, 384)
                    cw = slice(384, 512)
                    yield
                    S.pe("matmul", out=pU[:, cu], lhsT=TmT[i][hh][:], rhs=vb[i][hh][:], start=True, stop=True)
                    S.pe("matmul", out=pU[:, cw], lhsT=kbg[i][hh][:], rhs=TmT[i][hh][:], start=True, stop=True)
                    S.act("activation", out=usb[i][hh][:], in_=pU[:, cu], func=AF.Copy)
                    S.act("activation", out=wT[i][hh][:], in_=pU[:, cw], func=AF.Copy)
                    yield
                    S.pe("matmul", out=pO[:, 0:128], lhsT=wT[i][hh][:], rhs=Sst[hh][:], start=True, stop=True)
                    yield
                    S.dve("tensor_tensor", out=vnew[i][hh][:], in0=usb[i][hh][:], in1=pO[:, 0:128], op=ALU.subtract)
                    S.pe("matmul", out=pO[:, 128:256], lhsT=qgT[i][hh][:], rhs=Sst[hh][:], start=True, stop=False)
                    S.pe("matmul", out=pO[:, 128:256], lhsT=attnT[i][hh][:], rhs=vnew[i][hh][:], start=False, stop=True)
                    S.pe("matmul", out=pS[:, 256:384], lhsT=kd[i][hh][:], rhs=vnew[i][hh][:],
                         start=True, stop=True)
                    yield
                    S.dve("scalar_tensor_tensor", out=Sst[hh][:], in0=Sst[hh][:], scalar=dl[i][:, hh:hh + 1],
                          in1=pS[:, 256:384], op0=ALU.mult, op1=ALU.add)
                    if d == 0:
                        S.dve("tensor_copy", out=oacc[:, blk, hh * 128:(hh + 1) * 128], in_=pO[:, 128:256])
                    else:
                        S.dve("tensor_tensor", out=osum[i][:, hh, :], in0=pO[:, 128:256],
                              in1=oacc[:, blk, hh * 128:(hh + 1) * 128], op=ALU.add)

                gens = [head_steps(0), head_steps(1)]
                while gens:
                    for gn in list(gens):
                        try:
                            next(gn)
                        except StopIteration:
                            gens.remove(gn)
                if d == 1:
                    os_ = osum[i]
                    osv = os_.v(os_.t[:, :, :].rearrange("p a b -> p (a b)"))
                    S.act("activation", out=sg[i][:], in_=gin[i][:], func=AF.Silu)
                    S.dve("tensor_tensor", out=sq[:, :, :], in0=os_[:, :, :], in1=os_[:, :, :], op=ALU.mult)
                    S.dve("tensor_reduce", out=ss[:], in_=sq[:, :, :], axis=AX.X, op=ALU.add)
                    rsqrt_col(S, ss, ss, 1.0 / 128, 1e-6)
                    S.dve("tensor_tensor", out=os_[:, :, :], in0=os_[:, :, :],
                          in1=ss.v(ss.t[:, :].unsqueeze(2).to_broadcast([128, 2, 128])), op=ALU.mult)
                    S.dve("tensor_tensor", out=os_[:, :, :], in0=os_[:, :, :],
                          in1=nwb.v(nwb.t[:, :].unsqueeze(1).to_broadcast([128, 2, 128])), op=ALU.mult)
                    S.dve("tensor_tensor", out=osv, in0=osv, in1=sg[i][:], op=ALU.mult)
                    S.dma(out=ocat.v(ocat.t[rs_, 256:512]), in_=osv, eng="pool")


CTX_TILES = (0, 17)
NOWN = 17


def phase_c1(S, ofull, xin, modv, w_out, xmid, ident, ctx_tiles=CTX_TILES, gathered=False, rpc=512):
    with S.scope():
        idt = S.sbuf([128, 128])
        S.dma(out=idt[:], in_=ident.v(ident.t))
        Wb = S.sbuf([128, 16, 2048], BF16)
        ot = [S.sbuf([128, 2048]) for _ in range(2)]
        xt = [S.sbuf([128, 2048]) for _ in range(2)]
        load_w_bf16(S, Wb, w_out, ot + xt, 16, 2048)
        G1 = S.sbuf([128, 2048])
        oT = [S.sbuf([128, 16, 128], BF16) for _ in range(2)]
        xm = [S.sbuf([128, 2048]) for _ in range(2)]
        pst = [S.psum([128, 512]) for _ in range(2)]
        py = [S.psum([128, 512]) for _ in range(4)]
        order = list(ctx_tiles) + [t for t in range(NT) if t not in ctx_tiles]
        for n, t in enumerate(order):
            if n == 0 or n == 2:
                r = 1 if n == 0 else 0
                S.dma(out=G1[:], in_=modv.v(bc(modv.t[r:r + 1, 4096:6144])))
            rs_ = slice(t * 128, (t + 1) * 128)
            o_, x_ = ot[n % 2], xt[n % 2]
            if gathered:
                for r in range(2):
                    g0 = gat_row(t, r, rpc)
                    S.dma(out=o_[:, r * 1024:(r + 1) * 1024], in_=ofull.v(ofull.t[g0:g0 + 128, :]))
            else:
                S.dma(out=o_[:], in_=ofull.v(ofull.t[rs_, :]))
            S.dma(out=x_[:], in_=xin.v(xin.t[rs_, :]))
            transpose_tile(S, o_, oT[n % 2], idt, pst)
            for ct in range(4):
                cs = slice(ct * 512, (ct + 1) * 512)
                for k in range(16):
                    S.pe("matmul", out=py[ct][:], lhsT=oT[n % 2][:, k, :], rhs=Wb[:, k, cs], start=(k == 0), stop=(k == 15))
                S.dve("tensor_tensor", out=xm[n % 2][:, cs], in0=py[ct][:], in1=G1[:, cs], op=ALU.mult)
                S.pool("tensor_tensor", out=xm[n % 2][:, cs], in0=xm[n % 2][:, cs], in1=x_[:, cs], op=ALU.add)
            S.dma(out=xmid.v(xmid.t[rs_, :]), in_=xm[n % 2][:], eng="pool")


def phase_c2(S, xmid, modv, nw2, router_w, h2T, wm, consts, ctx_tiles=CTX_TILES, nown=NOWN, natural=False):
    ident = consts['ident']
    with S.scope():
        idt = S.sbuf([128, 128])
        S.dma(out=idt[:], in_=ident.v(ident.t))
        nwb = S.sbuf([128, 2048])
        S.dma(out=nwb[:], in_=nw2.v(bc(nw2.t[0:1, :])))
        rw = S.sbuf([128, 16, 16])
        S.dma(out=rw[:], in_=router_w.v(router_w.t.rearrange("(k p) e -> p k e", p=128)))
        A = S.sbuf([128, 2048])
        Bv = S.sbuf([128, 2048])
        xt = [S.sbuf([128, 2048]) for _ in range(2)]
        hb = S.sbuf([128, 2048])
        junk = S.sbuf([128, 2048], BF16)
        hT32 = [S.sbuf([128, 16, 128]) for _ in range(2)]
        hTb = [S.sbuf([128, 16, 128], BF16) for _ in range(2)]
        ssq = S.sbuf([128, 1])
        rstd = S.sbuf([128, 1])
        mx = S.sbuf([128, 1])
        sm = S.sbuf([128, 1])
        ex = S.sbuf([128, 16])
        aff = S.sbuf([128, 16])
        affT = S.sbuf([16, T])
        pst = [S.psum([128, 512]) for _ in range(2)]
        pl = S.psum([128, 512])
        pa = S.psum([128, 512])
        order = list(ctx_tiles) + [t for t in range(NT) if t not in ctx_tiles]
        for n, t in enumerate(order):
            if n == 0 or n == 2:
                r = 1 if n == 0 else 0
                S.dma(out=A[:], in_=modv.v(bc(modv.t[r:r + 1, 4 * 2048:5 * 2048])))
                S.dma(out=Bv[:], in_=modv.v(bc(modv.t[r:r + 1, 3 * 2048:4 * 2048])))
                S.dve("scalar_tensor_tensor", out=A[:], in0=A[:], scalar=1.0, in1=nwb[:], op0=ALU.add, op1=ALU.mult)
            rs_ = slice(t * 128, (t + 1) * 128)
            x_ = xt[n % 2]
            S.dma(out=x_[:], in_=xmid.v(xmid.t[rs_, :]))
            rms_mod(S, x_, A, Bv, hb, junk, ssq, rstd)
            transpose_tile(S, hb, hT32[n % 2], idt, pst)
            if t < nown:
                S.pool("tensor_copy", out=hTb[n % 2][:, :, :], in_=hT32[n % 2][:, :, :])
                S.dma(out=h2T.v(h2T.t[:, :, t * 128:(t + 1) * 128]), in_=hTb[n % 2][:, :, :], eng="pool")
            for k in range(16):
                S.pe("matmul", out=pl[:, 0:16], lhsT=hT32[n % 2][:, k, :], rhs=rw[:, k, :], start=(k == 0), stop=(k == 15))
            S.dve("tensor_reduce", out=mx[:], in_=pl[:, 0:16], axis=AX.X, op=ALU.max)
            S.dve("tensor_scalar", out=mx[:], in0=mx[:], scalar1=-1.0, scalar2=None, op0=ALU.mult)
            S.act("activation", out=ex[:], in_=pl[:, 0:16], func=AF.Exp, bias=mx[:], accum_out=sm[:])
            S.dve("reciprocal", out=sm[:], in_=sm[:])
            S.dve("tensor_scalar", out=aff[:], in0=ex[:], scalar1=sm[:], scalar2=None, op0=ALU.mult)
            S.pe("transpose", out=pa[0:16, 0:128], in_=aff[:], identity=idt[:])
            S.act("activation", out=affT[:, rs_], in_=pa[0:16, 0:128], func=AF.Copy)
        work = S.sbuf([16, T])
        m8 = S.sbuf([16, 8])
        S.dve("tensor_copy", out=work[:], in_=affT[:])
        if natural:
            lat = work[:, NCTX:T]
            ctxv = work[:, 0:NCTX]
        else:
            lat = work.v(bass.AP(work.t, 128, [[T, 16], [2176, 2], [1, 2048]]))
            ctxv = work.v(bass.AP(work.t, 0, [[T, 16], [2176, 2], [1, 128]]))
        for (view, kk) in ((lat, 512), (ctxv, 32)):
            for _ in range(kk // 8):
                S.dve("max", out=m8[:], in_=view)
                S.dve("match_replace", out=view, in_to_replace=m8[:], in_values=view, imm_value=0.0)
        S.dve("tensor_tensor", out=work[:], in0=affT[:], in1=work[:], op=ALU.subtract)
        wmt = [S.sbuf([128, 16]) for _ in range(2)]
        for t in range(NT):
            S.pe("transpose", out=pa[:, 0:16], in_=work[:, t * 128:(t + 1) * 128], identity=idt[0:16, 0:16])
            S.act("activation", out=wmt[t % 2][:], in_=pa[:, 0:16], func=AF.Copy)
            S.dma(out=wm.v(wm.t[t * 128:(t + 1) * 128, :]), in_=wmt[t % 2][:], eng="pool")


def phase_moe(S, h2T, wm, xmid, modv, wg, wu, wd, xout, final_nw=None):
    groups = [(0, 6), (6, 6), (12, 5)]
    with S.scope():
        G2c = S.sbuf([128, 2048])
        G2l = None
        hT = S.sbuf([128, 16, 768], BF16)
        yacc = S.sbuf([128, 6, 2048])
        hid = S.sbuf([128, 8, 768], BF16)
        wmt = S.sbuf([128, 6, 16])
        sgu = [[S.sbuf([128, 16, 128]) for _ in range(2)] for _ in range(2)]
        bgu = [[S.sbuf([128, 16, 128], BF16) for _ in range(2)] for _ in range(2)]
        sd = [S.sbuf([128, 8, 256]) for _ in range(2)]
        bd = [S.sbuf([128, 8, 256], BF16) for _ in range(2)]
        sgt = [S.sbuf([128, 512], BF16) for _ in range(2)]
        xm = S.sbuf([128, 2048])
        nwf = None
        if final_nw is not None:
            nwf = S.sbuf([128, 2048])
            S.dma(out=nwf[:], in_=final_nw.v(bc(final_nw.t[0:1, :])))
            junk = S.sbuf([128, 2048], BF16)
            ssq = S.sbuf([128, 1])
            rstd = S.sbuf([128, 1])
        pg = [S.psum([128, 512]) for _ in range(2)]
        pu = [S.psum([128, 512]) for _ in range(2)]
        pd = [S.psum([128, 512]) for _ in range(4)]
        cnt = 0
        dcnt = 0
        for (t0, ntl) in groups:
            ntok = ntl * 128
            S.dma(out=hT[:, :, 0:ntok], in_=h2T.v(h2T.t[:, :, t0 * 128:t0 * 128 + ntok]))
            S.dma(out=wmt[:, 0:ntl, :], in_=wm.v(wm.t[t0 * 128:t0 * 128 + ntok, :].rearrange("(a p) e -> p a e", p=128)))
            S.I("pool", "memset", yacc.t[:], 0.0, w=[yacc])
            subs = [(s0, min(512, ntok - s0)) for s0 in range(0, ntok, 512)]
            for e in range(16):
                for fc in range(8):
                    rg = cnt % 2
                    cnt += 1
                    fs = slice(fc * 128, (fc + 1) * 128)
                    for j, wsrc in enumerate((wg, wu)):
                        S.dma(out=sgu[rg][j][:, :, :], in_=wsrc.v(wsrc.t[e].rearrange("(k p) f -> p k f", p=128)[:, :, fs]))
                        S.I("pool" if j == 0 else "dve", "tensor_copy", out=bgu[rg][j][:, :, :], in_=sgu[rg][j][:, :, :])
                    for si, (s0, sn) in enumerate(subs):
                        for k in range(16):
                            S.pe("matmul", out=pg[si % 2][:, :sn], lhsT=bgu[rg][0][:, k, :], rhs=hT[:, k, s0:s0 + sn],
                                 start=(k == 0), stop=(k == 15))
                        for k in range(16):
                            S.pe("matmul", out=pu[si % 2][:, :sn], lhsT=bgu[rg][1][:, k, :], rhs=hT[:, k, s0:s0 + sn],
                                 start=(k == 0), stop=(k == 15))
                        S.act("activation", out=sgt[si % 2][:, :sn], in_=pg[si % 2][:, :sn], func=AF.Silu)
                        S.dve("tensor_tensor", out=hid[:, fc, s0:s0 + sn], in0=pu[si % 2][:, :sn], in1=sgt[si % 2][:, :sn], op=ALU.mult)
                for dc in range(8):
                    rg = dcnt % 2
                    dcnt += 1
                    ds_ = slice(dc * 256, (dc + 1) * 256)
                    S.dma(out=sd[rg][:, :, :], in_=wd.v(wd.t[e].rearrange("(k p) d -> p k d", p=128)[:, :, ds_]))
                    S.act("activation", out=bd[rg][:, :, :], in_=sd[rg][:, :, :], func=AF.Copy)
                    for tl in range(ntl):
                        pp = pd[(dc * ntl + tl) % 4]
                        for k in range(8):
                            S.pe("matmul", out=pp[:, 0:256], lhsT=hid[:, k, tl * 128:(tl + 1) * 128], rhs=bd[rg][:, k, :],
                                 start=(k == 0), stop=(k == 7))
                        S.dve("scalar_tensor_tensor", out=yacc[:, tl, ds_], in0=pp[:, 0:256], scalar=wmt[:, tl, e:e + 1],
                              in1=yacc[:, tl, ds_], op0=ALU.mult, op1=ALU.add)
            for tl in range(ntl):
                t = t0 + tl
                if t == 0:
                    S.dma(out=G2c[:], in_=modv.v(bc(modv.t[1:2, 5 * 2048:6 * 2048])))
                if t == 1:
                    S.dma(out=G2c[:], in_=modv.v(bc(modv.t[0:1, 5 * 2048:6 * 2048])))
                S.dma(out=xm[:], in_=xmid.v(xmid.t[t * 128:(t + 1) * 128, :]))
                S.dve("tensor_tensor", out=yacc[:, tl, :], in0=yacc[:, tl, :], in1=G2c[:], op=ALU.mult)
                S.pool("tensor_tensor", out=yacc[:, tl, :], in0=yacc[:, tl, :], in1=xm[:], op=ALU.add)
                if nwf is not None:
                    S.act("activation", out=junk[:], in_=yacc[:, tl, :], func=AF.Square, accum_out=ssq[:])
                    rsqrt_col(S, rstd, ssq, 1.0 / 2048, 1e-6)
                    S.dve("scalar_tensor_tensor", out=yacc[:, tl, :], in0=yacc[:, tl, :], scalar=rstd[:], in1=nwf[:],
                          op0=ALU.mult, op1=ALU.mult)
                S.dma(out=xout.v(xout.t[t * 128:(t + 1) * 128, :]), in_=yacc[:, tl, :], eng="pool")


def phase_moe_part(S, h2T, wm, wg, wu, wd, ypart, nexp=8):
    groups = [(t0, min(6, NT - t0)) for t0 in range(0, NT, 6)]
    with S.scope():
        hT = S.sbuf([128, 16, 768], BF16)
        yacc = S.sbuf([128, 6, 2048])
        hid = S.sbuf([128, 8, 768], BF16)
        wmt = S.sbuf([128, 6, 16])
        sgu = [[S.sbuf([128, 16, 128]) for _ in range(2)] for _ in range(2)]
        bgu = [[S.sbuf([128, 16, 128], BF16) for _ in range(2)] for _ in range(2)]
        sd = [S.sbuf([128, 8, 256]) for _ in range(2)]
        bd = [S.sbuf([128, 8, 256], BF16) for _ in range(2)]
        sgt = [S.sbuf([128, 512], BF16) for _ in range(2)]
        pg = [S.psum([128, 512]) for _ in range(2)]
        pu = [S.psum([128, 512]) for _ in range(2)]
        pd = [S.psum([128, 512]) for _ in range(4)]
        cnt = 0
        dcnt = 0
        for (t0, ntl) in groups:
            ntok = ntl * 128
            S.dma(out=hT[:, :, 0:ntok], in_=h2T.v(h2T.t[:, :, t0 * 128:t0 * 128 + ntok]))
            S.dma(out=wmt[:, 0:ntl, :], in_=wm.v(wm.t[t0 * 128:t0 * 128 + ntok, :].rearrange("(a p) e -> p a e", p=128)))
            S.I("pool", "memset", yacc.t[:], 0.0, w=[yacc])
            subs = [(s0, min(512, ntok - s0)) for s0 in range(0, ntok, 512)]
            for e in range(nexp):
                for fc in range(8):
                    rg = cnt % 2
                    cnt += 1
                    fs = slice(fc * 128, (fc + 1) * 128)
                    for j, wsrc in enumerate((wg, wu)):
                        S.dma(out=sgu[rg][j][:, :, :], in_=wsrc.v(wsrc.t[e].rearrange("(k p) f -> p k f", p=128)[:, :, fs]))
                        S.I("pool" if j == 0 else "dve", "tensor_copy", out=bgu[rg][j][:, :, :], in_=sgu[rg][j][:, :, :])
                    for si, (s0, sn) in enumerate(subs):
                        for k in range(16):
                            S.pe("matmul", out=pg[si % 2][:, :sn], lhsT=bgu[rg][0][:, k, :], rhs=hT[:, k, s0:s0 + sn],
                                 start=(k == 0), stop=(k == 15))
                        for k in range(16):
                            S.pe("matmul", out=pu[si % 2][:, :sn], lhsT=bgu[rg][1][:, k, :], rhs=hT[:, k, s0:s0 + sn],
                                 start=(k == 0), stop=(k == 15))
                        S.act("activation", out=sgt[si % 2][:, :sn], in_=pg[si % 2][:, :sn], func=AF.Silu)
                        S.dve("tensor_tensor", out=hid[:, fc, s0:s0 + sn], in0=pu[si % 2][:, :sn], in1=sgt[si % 2][:, :sn], op=ALU.mult)
                for dc in range(8):
                    rg = dcnt % 2
                    dcnt += 1
                    ds_ = slice(dc * 256, (dc + 1) * 256)
                    S.dma(out=sd[rg][:, :, :], in_=wd.v(wd.t[e].rearrange("(k p) d -> p k d", p=128)[:, :, ds_]))
                    S.act("activation", out=bd[rg][:, :, :], in_=sd[rg][:, :, :], func=AF.Copy)
                    for tl in range(ntl):
                        pp = pd[(dc * ntl + tl) % 4]
                        for k in range(8):
                            S.pe("matmul", out=pp[:, 0:256], lhsT=hid[:, k, tl * 128:(tl + 1) * 128], rhs=bd[rg][:, k, :],
                                 start=(k == 0), stop=(k == 7))
                        S.dve("scalar_tensor_tensor", out=yacc[:, tl, ds_], in0=pp[:, 0:256], scalar=wmt[:, tl, e:e + 1],
                              in1=yacc[:, tl, ds_], op0=ALU.mult, op1=ALU.add)
            S.dma(out=ypart.v(ypart.t[t0 * 128:t0 * 128 + ntok, :].rearrange("(a p) d -> p a d", p=128)),
                  in_=yacc[:, 0:ntl, :], eng="pool")


def phase_fin(S, ygat, xmid, modv, xnext, final_nw=None, out_lat=None, rpc=256):
    with S.scope():
        G2 = S.sbuf([128, 2048])
        y0 = [S.sbuf([128, 2048]) for _ in range(2)]
        y1 = [S.sbuf([128, 2048]) for _ in range(2)]
        xm = [S.sbuf([128, 2048]) for _ in range(2)]
        if final_nw is not None:
            nwf = S.sbuf([128, 2048])
            S.dma(out=nwf[:], in_=final_nw.v(bc(final_nw.t[0:1, :])))
            junk = S.sbuf([128, 2048], BF16)
            ssq = S.sbuf([128, 1])
            rstd = S.sbuf([128, 1])
        for t in range(NT):
            if final_nw is not None and t < 2:
                continue
            if t == 0 or t == 2 or (final_nw is not None and t == 2):
                r = 1 if t == 0 else 0
                S.dma(out=G2[:], in_=modv.v(bc(modv.t[r:r + 1, 5 * 2048:6 * 2048])))
            rs_ = slice(t * 128, (t + 1) * 128)
            a, b, x_ = y0[t % 2], y1[t % 2], xm[t % 2]
            ga, gb_ = gat_row(t, 0, rpc), gat_row(t, 1, rpc)
            S.dma(out=a[:], in_=ygat.v(ygat.t[ga:ga + 128, :]))
            S.dma(out=b[:], in_=ygat.v(ygat.t[gb_:gb_ + 128, :]))
            S.dma(out=x_[:], in_=xmid.v(xmid.t[rs_, :]))
            S.dve("tensor_tensor", out=a[:], in0=a[:], in1=b[:], op=ALU.add)
            S.pool("tensor_tensor", out=a[:], in0=a[:], in1=G2[:], op=ALU.mult)
            S.dve("tensor_tensor", out=a[:], in0=a[:], in1=x_[:], op=ALU.add)
            if final_nw is not None:
                S.act("activation", out=junk[:], in_=a[:], func=AF.Square, accum_out=ssq[:])
                rsqrt_col(S, rstd, ssq, 1.0 / 2048, 1e-6)
                S.dve("scalar_tensor_tensor", out=a[:], in0=a[:], scalar=rstd[:], in1=nwf[:], op0=ALU.mult, op1=ALU.mult)
                S.dma(out=out_lat.v(out_lat.t[(t - 2) * 128:(t - 1) * 128, :]), in_=a[:], eng="pool")
            else:
                S.dma(out=xnext.v(xnext.t[rs_, :]), in_=a[:], eng="pool")


def pair_gather(S, src, dst, rpc):
    for r0 in range(0, T, rpc):
        r1 = min(T, r0 + rpc)
        S.I("pool", "collective_compute", "AllGather", ALU.bypass, replica_groups=[[0, 1], [2, 3], [4, 5], [6, 7]],
            ins=[src.t[r0:r1, :]], outs=[dst.t[2 * r0:2 * r1, :]], r=[src], w=[dst])


def gat_row(t, r, rpc):
    r0 = (t * 128 // rpc) * rpc
    rows_c = min(rpc, T - r0)
    return 2 * r0 + r * rows_c + (t * 128 - r0)


U32 = mybir.dt.uint32
NSLOT = 640


def sparse_consts():
    return {'dummy_rows': np.tile(np.arange(256, 352, dtype=np.float32)[None], (16, 1))}


def phase_c2s(S, xmid, modv, nw2, router_w, h2d, selI, selW, consts):
    ident = consts['ident']
    with S.scope():
        idt = S.sbuf([128, 128])
        S.dma(out=idt[:], in_=ident.v(ident.t))
        nwb = S.sbuf([128, 2048])
        S.dma(out=nwb[:], in_=nw2.v(bc(nw2.t[0:1, :])))
        rw = S.sbuf([128, 16, 16])
        S.dma(out=rw[:], in_=router_w.v(router_w.t.rearrange("(k p) e -> p k e", p=128)))
        A = S.sbuf([128, 2048])
        Bv = S.sbuf([128, 2048])
        xt = [S.sbuf([128, 2048]) for _ in range(2)]
        hb = [S.sbuf([128, 2048]) for _ in range(2)]
        junk = S.sbuf([128, 2048], BF16)
        hT32 = [S.sbuf([128, 16, 128]) for _ in range(2)]
        ssq = S.sbuf([128, 1])
        rstd = S.sbuf([128, 1])
        mx = S.sbuf([128, 1])
        sm = S.sbuf([128, 1])
        ex = S.sbuf([128, 16])
        aff = S.sbuf([128, 16])
        affT = S.sbuf([16, T])
        pst = [S.psum([128, 512]) for _ in range(2)]
        pl = S.psum([128, 512])
        pa = S.psum([128, 512])
        for t in range(NT):
            if t == 0 or t == 2:
                r = 1 if t == 0 else 0
                S.dma(out=A[:], in_=modv.v(bc(modv.t[r:r + 1, 4 * 2048:5 * 2048])))
                S.dma(out=Bv[:], in_=modv.v(bc(modv.t[r:r + 1, 3 * 2048:4 * 2048])))
                S.dve("scalar_tensor_tensor", out=A[:], in0=A[:], scalar=1.0, in1=nwb[:], op0=ALU.add, op1=ALU.mult)
            rs_ = slice(t * 128, (t + 1) * 128)
            x_ = xt[t % 2]
            h_ = hb[t % 2]
            S.dma(out=x_[:], in_=xmid.v(xmid.t[rs_, :]))
            rms_mod(S, x_, A, Bv, h_, junk, ssq, rstd)
            S.dma(out=h2d.v(h2d.t[rs_, :]), in_=h_[:], eng="pool")
            transpose_tile(S, h_, hT32[t % 2], idt, pst)
            for k in range(16):
                S.pe("matmul", out=pl[:, 0:16], lhsT=hT32[t % 2][:, k, :], rhs=rw[:, k, :], start=(k == 0), stop=(k == 15))
            S.dve("tensor_reduce", out=mx[:], in_=pl[:, 0:16], axis=AX.X, op=ALU.max)
            S.dve("tensor_scalar", out=mx[:], in0=mx[:], scalar1=-1.0, scalar2=None, op0=ALU.mult)
            S.act("activation", out=ex[:], in_=pl[:, 0:16], func=AF.Exp, bias=mx[:], accum_out=sm[:])
            S.dve("reciprocal", out=sm[:], in_=sm[:])
            S.dve("tensor_scalar", out=aff[:], in0=ex[:], scalar1=sm[:], scalar2=None, op0=ALU.mult)
            S.pe("transpose", out=pa[0:16, 0:128], in_=aff[:], identity=idt[:])
            S.act("activation", out=affT[:, rs_], in_=pa[0:16, 0:128], func=AF.Copy)
        vals = S.sbuf([16, NSLOT])
        idxs = S.sbuf([16, NSLOT], U32)
        idxf = S.sbuf([16, NSLOT])
        S.I("dve", "memset", vals.t[:], 0.0, w=[vals])
        S.I("dve", "memset", idxs.t[:], 0, w=[idxs])
        for (view, c0, kk) in ((affT[:, NCTX:T], 0, 512), (affT[:, 0:NCTX], 512, 32)):
            for r in range(kk // 8):
                cs = slice(c0 + 8 * r, c0 + 8 * r + 8)
                S.dve("max", out=vals[:, cs], in_=view)
                S.dve("max_index", out=idxs[:, cs], in_max=vals[:, cs], in_values=view)
                S.dve("match_replace", out=view, in_to_replace=vals[:, cs], in_values=view, imm_value=0.0)
        S.dve("tensor_copy", out=idxf[:], in_=idxs[:])
        S.dve("tensor_scalar", out=idxf[:, 0:512], in0=idxf[:, 0:512], scalar1=float(NCTX), scalar2=None, op0=ALU.add)
        S.dma(out=idxf[:, 544:640], in_=consts['dummy_rows'].v(consts['dummy_rows'].t))
        it = [S.sbuf([128, 16], U32) for _ in range(2)]
        wt = [S.sbuf([128, 16]) for _ in range(2)]
        for j in range(NSLOT // 128):
            S.pe("transpose", out=pa[:, 0:16], in_=idxf[:, j * 128:(j + 1) * 128], identity=idt[0:16, 0:16])
            S.pe("transpose", out=pa[:, 16:32], in_=vals[:, j * 128:(j + 1) * 128], identity=idt[0:16, 0:16])
            S.dve("tensor_copy", out=it[j % 2][:], in_=pa[:, 0:16])
            S.dve("tensor_copy", out=wt[j % 2][:], in_=pa[:, 16:32])
            S.dma(out=selI.v(selI.t[j]), in_=it[j % 2][:], eng="pool")
            S.dma(out=selW.v(selW.t[j]), in_=wt[j % 2][:], eng="pool")


def phase_moe_sparse(S, h2d, selI, selW, wg, wu, wd, ypart, ident, nexp=8):
    NJ = NSLOT // 128
    with S.scope():
        idt = S.sbuf([128, 128])
        S.dma(out=idt[:], in_=ident.v(ident.t))
        it = S.sbuf([128, NJ, 16], U32)
        wt = S.sbuf([128, NJ, 16])
        S.dma(out=it[:], in_=selI.v(selI.t.rearrange("j p e -> p j e")))
        S.dma(out=wt[:], in_=selW.v(selW.t.rearrange("j p e -> p j e")))
        yv = [S.sbuf([128, 2048]) for _ in range(NJ)]
        S.I("pool", "memset", yv[0].t[:], 0.0, w=[yv[0]])
        for t in range(NT):
            S.dma(out=ypart.v(ypart.t[t * 128:(t + 1) * 128, :]), in_=yv[0][:], eng="pool")
        xs = [S.sbuf([128, 2048]) for _ in range(2)]
        xsT = S.sbuf([128, 16, NSLOT], BF16)
        hid = S.sbuf([128, 8, NSLOT], BF16)
        sgu = [[S.sbuf([128, 16, 128]) for _ in range(2)] for _ in range(2)]
        bgu = [[S.sbuf([128, 16, 128], BF16) for _ in range(2)] for _ in range(2)]
        sd = [S.sbuf([128, 8, 256]) for _ in range(2)]
        bd = [S.sbuf([128, 8, 256], BF16) for _ in range(2)]
        sgt = [S.sbuf([128, 512], BF16) for _ in range(2)]
        pst = [S.psum([128, 512]) for _ in range(2)]
        pg = [S.psum([128, 512]) for _ in range(2)]
        pu = [S.psum([128, 512]) for _ in range(2)]
        pd = [S.psum([128, 512]) for _ in range(2)]
        subs = [(0, 512), (512, 128)]
        cnt = 0
        dcnt = 0
        for e in range(nexp):
            for j in range(NJ):
                x_ = xs[j % 2]
                S.I("pool", "indirect_dma_start", r=[h2d, it], w=[x_], out=x_.t[:], out_offset=None, in_=h2d.t,
                    in_offset=bass.IndirectOffsetOnAxis(ap=it.t[:, j, e:e + 1], axis=0))
                for g in range(4):
                    p = pst[g % 2]
                    for jj in range(4):
                        k = g * 4 + jj
                        S.pe("transpose", out=p[:, jj * 128:(jj + 1) * 128], in_=x_[:, k * 128:(k + 1) * 128], identity=idt[:])
                    dv = xsT[:, g * 4:g * 4 + 4, j * 128:(j + 1) * 128]
                    pv = p.v(p.t[:, :].rearrange("p (a b) -> p a b", a=4))
                    if g % 2:
                        S.act("activation", out=dv, in_=pv, func=AF.Copy)
                    else:
                        S.dve("tensor_copy", out=dv, in_=pv)
            for fc in range(8):
                rg = cnt % 2
                cnt += 1
                fs = slice(fc * 128, (fc + 1) * 128)
                for jw, wsrc in enumerate((wg, wu)):
                    S.dma(out=sgu[rg][jw][:, :, :], in_=wsrc.v(wsrc.t[e].rearrange("(k p) f -> p k f", p=128)[:, :, fs]))
                    S.I("pool" if jw == 0 else "dve", "tensor_copy", out=bgu[rg][jw][:, :, :], in_=sgu[rg][jw][:, :, :])
                for si, (s0, sn) in enumerate(subs):
                    for k in range(16):
                        S.pe("matmul", out=pg[si % 2][:, :sn], lhsT=bgu[rg][0][:, k, :], rhs=xsT[:, k, s0:s0 + sn],
                             start=(k == 0), stop=(k == 15))
                    for k in range(16):
                        S.pe("matmul", out=pu[si % 2][:, :sn], lhsT=bgu[rg][1][:, k, :], rhs=xsT[:, k, s0:s0 + sn],
                             start=(k == 0), stop=(k == 15))
                    S.act("activation", out=sgt[si % 2][:, :sn], in_=pg[si % 2][:, :sn], func=AF.Silu)
                    S.dve("tensor_tensor", out=hid[:, fc, s0:s0 + sn], in0=pu[si % 2][:, :sn], in1=sgt[si % 2][:, :sn], op=ALU.mult)
            for dc in range(8):
                rg = dcnt % 2
                dcnt += 1
                ds_ = slice(dc * 256, (dc + 1) * 256)
                S.dma(out=sd[rg][:, :, :], in_=wd.v(wd.t[e].rearrange("(k p) d -> p k d", p=128)[:, :, ds_]))
                S.act("activation", out=bd[rg][:, :, :], in_=sd[rg][:, :, :], func=AF.Copy)
                for j in range(NJ):
                    pp = pd[(dc * NJ + j) % 2]
                    for k in range(8):
                        S.pe("matmul", out=pp[:, 0:256], lhsT=hid[:, k, j * 128:(j + 1) * 128], rhs=bd[rg][:, k, :],
                             start=(k == 0), stop=(k == 7))
                    S.dve("tensor_scalar", out=yv[j][:, ds_], in0=pp[:, 0:256], scalar1=wt[:, j, e:e + 1], scalar2=None,
                          op0=ALU.mult)
            for j in range(NJ):
                S.I("pool", "indirect_dma_start", r=[yv[j], it], w=[ypart], out=ypart.t,
                    out_offset=bass.IndirectOffsetOnAxis(ap=it.t[:, j, e:e + 1], axis=0), in_=yv[j].t[:], in_offset=None,
                    compute_op=ALU.add, oob_is_err=True)


import math

DEPTH = 2
_CONSTS = None
_PROGS = {}


def host_consts():
    global _CONSTS
    if _CONSTS is None:
        _CONSTS = {'ident': np.eye(128, dtype=np.float32), **rope_tables(), **tri_consts(), **dn_consts(), **sparse_consts()}
    return _CONSTS


A_PRM_SHAPES = {'w2pad0': [32, 128], 'w2pad1': [32, 128], 'gb0': [1, 128], 'gb1': [1, 128], 'gla_nw': [1, 128],
                'dn_convw': [5, 768], 'dn_alog': [1, 4], 'dn_dtb': [1, 4], 'dn_nw': [1, 128],
                'gqa_qn': [1, 128], 'gqa_kn': [1, 128], 'diff_lambda': [1, 256], 'diff_nw': [1, 128], 'lamc': [1, 2]}


def layer_params(inp, l, h):
    p = {}
    w2 = inp['gla_gate_w2'][l]
    gbias = inp['gla_gate_b'][l]
    for d in range(2):
        wp = np.zeros((32, 128), np.float32)
        wp[d * 16:(d + 1) * 16] = w2[d][:, 128 * h:128 * h + 128]
        p[f'w2pad{d}'] = wp
        p[f'gb{d}'] = np.ascontiguousarray(gbias[d][None, 128 * h:128 * h + 128])
    p['gla_nw'] = np.ascontiguousarray(inp['gla_norm_w'][l][None])
    cw = inp['dn_conv_w'][l]
    p['dn_convw'] = np.ascontiguousarray(np.concatenate(
        [cw[:, 256 * h:256 * h + 256], cw[:, 512 + 256 * h:512 + 256 * h + 256], cw[:, 1024 + 256 * h:1024 + 256 * h + 256]], 1))
    p['dn_alog'] = np.ascontiguousarray(inp['dn_a_log'][l][:, 2 * h:2 * h + 2].reshape(1, 4))
    p['dn_dtb'] = np.ascontiguousarray(inp['dn_dt_bias'][l][:, 2 * h:2 * h + 2].reshape(1, 4))
    p['dn_nw'] = np.ascontiguousarray(inp['dn_norm_w'][l][None])
    p['gqa_qn'] = np.ascontiguousarray(inp['gqa_q_norm'][l][None])
    p['gqa_kn'] = np.ascontiguousarray(inp['gqa_k_norm'][l][None])
    p['diff_lambda'] = np.ascontiguousarray(inp['diff_lambda'][l].reshape(1, 256))
    p['diff_nw'] = np.ascontiguousarray(inp['diff_norm_w'][l][None])
    lam_init = 0.8 - 0.6 * math.exp(-0.3 * l)
    p['lamc'] = np.array([[1.0 - lam_init, -lam_init]], np.float32)
    return p


def prog_F():
    if 'F' in _PROGS:
        return _PROGS['F']
    nc = bass.Bass("TRN2", target_bir_lowering=False)
    C = host_consts()
    with ExitStack() as st:
        S = Sched(nc, st)
        xin0 = S.dram("xin", [T, 2048], kind="ExternalInput")
        c2 = S.dram("c2", [2, 2048], kind="ExternalInput")
        consts = {k: S.dram(k, list(v.shape), kind="ExternalInput") for k, v in C.items()}
        fnw = S.dram("fnw", [1, 2048], kind="ExternalInput")
        yout = S.dram("yout", [4096, 2048], kind="ExternalOutput")
        z = S.dram("z", [T, ZC])
        dnp = S.dram("dnp", [T, 776])
        xcur = xin0
        for l in range(DEPTH):
            last = l == DEPTH - 1
            E = lambda nm, shp, dt=F32: S.dram(f"{nm}_{l}", shp, dt, kind="ExternalInput")
            mod_w = E("mod_w", [2048, 12288])
            mod_b = E("mod_b", [1, 12288])
            nw = E("nw", [1, 2048])
            w_in = E("w_in", [2048, ZC])
            prm = {k: E(k, shp) for k, shp in A_PRM_SHAPES.items()}
            w_out = E("w_out", [2048, 2048])
            nw2 = E("nw2", [1, 2048])
            router_w = E("router_w", [2048, 16])
            wg = E("wg", [8, 2048, 1024])
            wu = E("wu", [8, 2048, 1024])
            wd = E("wd", [8, 1024, 2048])
            I_ = lambda nm, shp, dt=F32: S.dram(f"{nm}_{l}", shp, dt)
            modv = I_("modv", [2, 12288])
            ocat = I_("ocat", [T, 1024])
            ogat = I_("ogat", [2 * T, 1024])
            xmid = I_("xmid", [T, 2048])
            h2d = I_("h2d", [T, 2048])
            selI = I_("selI", [5, 128, 16], U32)
            selW = I_("selW", [5, 128, 16])
            ypart = I_("ypart", [T, 2048])
            ygat = I_("ygat", [2 * T, 2048])
            xnext = None if last else I_("xnext", [T, 2048])
            phase_mod(S, c2, mod_w, mod_b, modv)
            phase_in(S, xcur, modv, nw, w_in, z, consts['ident'])
            phase_gla(S, z, consts, prm, ocat)
            phase_dn_prep(S, z, prm, dnp)
            phase_dn(S, z, dnp, consts, prm, ocat)
            phase_attn(S, z, consts, prm, ocat)
            pair_gather(S, ocat, ogat, 512)
            phase_c1(S, ogat, xcur, modv, w_out, xmid, consts['ident'], ctx_tiles=(0, 1), gathered=True)
            phase_c2s(S, xmid, modv, nw2, router_w, h2d, selI, selW, consts)
            phase_moe_sparse(S, h2d, selI, selW, wg, wu, wd, ypart, consts['ident'], nexp=8)
            pair_gather(S, ypart, ygat, 256)
            phase_fin(S, ygat, xmid, modv, xnext, final_nw=fnw if last else None, out_lat=yout if last else None)
            xcur = xnext
        outs = [o for o in S.all_ops if o.is_dma and o.eng == "pool" and o.fn[0] == "dma_start"]
        S.emit(final_waits=outs[-40:])
        _PROGS['F_info'] = (S.nsem, {e: len(v) for e, v in S.ops.items()})
    _PROGS['F'] = nc
    return nc


def kernel(**inp):
    inp = {k: np.asarray(v) for k, v in inp.items()}
    C = host_consts()
    B = 4
    cores = [(b, h) for b in range(B) for h in range(2)]
    nc = prog_F()
    perm_o = np.array([g * 512 + h * 256 + j for h in range(2) for g in range(4) for j in range(256)])
    shared = {}
    per_h = [dict(), dict()]
    for l in range(DEPTH):
        shared[f"mod_w_{l}"] = np.ascontiguousarray(inp['mod_w'][l])
        shared[f"mod_b_{l}"] = np.ascontiguousarray(inp['mod_b'][l][None])
        shared[f"nw_{l}"] = np.ascontiguousarray(inp['norm1_w'][l][None])
        shared[f"w_out_{l}"] = np.ascontiguousarray(inp['w_out'][l][perm_o])
        shared[f"nw2_{l}"] = np.ascontiguousarray(inp['norm2_w'][l][None])
        for h in range(2):
            d = per_h[h]
            d[f"w_in_{l}"] = np.ascontiguousarray(inp['w_in'][l][:, wcols(h)])
            for k, v in layer_params(inp, l, h).items():
                d[f"{k}_{l}"] = v
            ecols = np.concatenate([np.arange(8 * h, 8 * h + 8), np.arange(8 * (1 - h), 8 * (1 - h) + 8)])
            d[f"router_w_{l}"] = np.ascontiguousarray(inp['router_w'][l][:, ecols])
            d[f"wg_{l}"] = np.ascontiguousarray(inp['exp_w_gate'][l][8 * h:8 * h + 8])
            d[f"wu_{l}"] = np.ascontiguousarray(inp['exp_w_up'][l][8 * h:8 * h + 8])
            d[f"wd_{l}"] = np.ascontiguousarray(inp['exp_w_down'][l][8 * h:8 * h + 8])
    fnw = np.ascontiguousarray(inp['final_norm_w'][None])
    in_maps = []
    for (b, h) in cores:
        m = {"xin": np.concatenate([inp['ctx'][b], inp['x'][b]], 0),
             "c2": np.ascontiguousarray(np.stack([inp['c'][b], inp['c_ctx']])), "fnw": fnw, **C, **shared, **per_h[h]}
        in_maps.append(m)
    res = run_bass_kernel_spmd(nc, in_maps, core_ids=list(range(8)))
    return np.stack([np.asarray(res.results[2 * b]["yout"]) for b in range(B)], 0).astype(np.float32)
```
